# Optimizing a Trainium2 kernel written in Bass

```python
import jax
import jax.numpy as jnp
from jax import lax
import numpy as np

D_MODEL = 2048
BATCH = 4
SEQ = 4096
DEPTH = 2

GRID_W = 64
CTX_LEN = 256
EPS = 1e-6
ROPE_THETA = 10000.0
Q_BLOCK = 128

MLA_HEADS = 8
MLA_Q_RANK = 512
MLA_KV_RANK = 512
MLA_NOPE = 128
MLA_ROPE = 64
MLA_V = 128
MLA_SCALE = (MLA_NOPE + MLA_ROPE) ** -0.5

GQA_HEADS = 8
GQA_KV_HEADS = 2
HEAD_DIM = 128
HEAD_SCALE = HEAD_DIM ** -0.5

NA_HEADS = 8
NA_KH = 8
NA_KW = 16

N_BRANCH = 3
BRANCH_W = 1024

N_EXPERTS = 32
TOP_K = 4
D_FF = 2048
SWIGLU_ALPHA = 1.702
SWIGLU_LIMIT = 7.0
MOE_BLOCK = 128

MOD_SCALE = 0.5

IN_SIZES = (MLA_Q_RANK, MLA_KV_RANK, MLA_ROPE,
            GQA_HEADS * HEAD_DIM, GQA_KV_HEADS * HEAD_DIM, GQA_KV_HEADS * HEAD_DIM,
            NA_HEADS * HEAD_DIM, NA_HEADS * HEAD_DIM, NA_HEADS * HEAD_DIM,
            N_BRANCH * D_MODEL)
IN_COLS = sum(IN_SIZES)
IN_SPLITS = tuple(sum(IN_SIZES[:i + 1]) for i in range(len(IN_SIZES) - 1))

kernel_name = 'hybrid_gated_mla_gqa_natten_moe'


def rms_norm(x, g):
    xf = x.astype(jnp.float32)
    y = xf * lax.rsqrt(jnp.mean(xf * xf, axis=-1, keepdims=True) + EPS)
    return (y * g.astype(jnp.float32)).astype(x.dtype)


def modulate(x, shift, scale):
    return x * (1 + scale) + shift


def rope_1d(x, pos):
    dim = x.shape[-1]
    freqs = ROPE_THETA ** (-jnp.arange(0, dim, 2, dtype=jnp.float32) / dim)
    ang = pos[:, None, None] * freqs
    cos, sin = jnp.cos(ang), jnp.sin(ang)
    xf = x.astype(jnp.float32)
    x1, x2 = xf[..., : dim // 2], xf[..., dim // 2:]
    return jnp.concatenate([x1 * cos - x2 * sin, x2 * cos + x1 * sin], axis=-1).astype(x.dtype)


def axial_rope(x, pos_r, pos_c):
    half = x.shape[-1] // 2
    return jnp.concatenate([rope_1d(x[..., :half], pos_r), rope_1d(x[..., half:], pos_c)], axis=-1)


def block_attention(q, k, v, scale):
    B, Sq, Hq, dk = q.shape
    Hkv, dv = k.shape[2], v.shape[-1]
    G = Hq // Hkv
    nb = Sq // Q_BLOCK
    qb = q.reshape(B, nb, Q_BLOCK, Hkv, G, dk).transpose(1, 0, 2, 3, 4, 5)

    def one_block(q_blk):
        s = jnp.einsum('bqhgd,bkhd->bhgqk', q_blk, k, preferred_element_type=jnp.float32) * scale
        p = jax.nn.softmax(s, axis=-1).astype(v.dtype)
        return jnp.einsum('bhgqk,bkhe->bqhge', p, v)

    out = lax.map(one_block, qb)
    return out.transpose(1, 0, 2, 3, 4, 5).reshape(B, Sq, Hq, dv)


def neighbourhood_attention(q, k, v, k_ctx, v_ctx, rpb, scale):
    B, S, H, d = q.shape
    rows = S // GRID_W
    kh = min(NA_KH, rows)
    n_loc = kh * NA_KW
    qg = q.reshape(B, rows, GRID_W, H, d).transpose(1, 0, 2, 3, 4)
    kg = k.reshape(B, rows, GRID_W, H, d)
    vg = v.reshape(B, rows, GRID_W, H, d)
    col = jnp.arange(GRID_W, dtype=jnp.int32)
    col0 = jnp.clip(col - NA_KW // 2, 0, GRID_W - NA_KW)
    col_idx = col0[:, None] + jnp.arange(NA_KW, dtype=jnp.int32)
    rpb_cols = rpb[:, :, col_idx - col[:, None] + NA_KW - 1]

    def one_row(args):
        r, q_row = args
        r0 = jnp.clip(r - kh // 2, 0, rows - kh)
        k_nb = lax.dynamic_slice_in_dim(kg, r0, kh, axis=1)[:, :, col_idx]
        v_nb = lax.dynamic_slice_in_dim(vg, r0, kh, axis=1)[:, :, col_idx]
        dr = r0 + jnp.arange(kh, dtype=jnp.int32) - r + NA_KH - 1
        bias = jnp.take(rpb_cols, dr, axis=1).transpose(0, 2, 1, 3)
        s_loc = jnp.einsum('bqhd,bkqwhd->bhqkw', q_row, k_nb, preferred_element_type=jnp.float32) * scale
        s_loc = s_loc + bias.astype(jnp.float32)[None]
        s_ctx = jnp.einsum('bqhd,bchd->bhqc', q_row, k_ctx, preferred_element_type=jnp.float32) * scale
        s = jnp.concatenate([s_loc.reshape(B, H, GRID_W, n_loc), s_ctx], axis=-1)
        p = jax.nn.softmax(s, axis=-1).astype(v.dtype)
        p_loc = p[..., :n_loc].reshape(B, H, GRID_W, kh, NA_KW)
        return (jnp.einsum('bhqkw,bkqwhe->bqhe', p_loc, v_nb)
                + jnp.einsum('bhqc,bche->bqhe', p[..., n_loc:], v_ctx))

    out = lax.map(one_row, (jnp.arange(rows, dtype=jnp.int32), qg))
    return out.transpose(1, 0, 2, 3, 4).reshape(B, S, H, d)


def gated_merge(branches, gate_logits, w_branch, w_out):
    B, T, _ = gate_logits.shape
    gates = jax.nn.sigmoid(gate_logits.astype(jnp.float32)).astype(gate_logits.dtype)
    gates = gates.reshape(B, T, N_BRANCH, D_MODEL)
    merged = None
    for i, o in enumerate(branches):
        term = gates[:, :, i] * (o.reshape(B, T, BRANCH_W) @ w_branch[i])
        merged = term if merged is None else merged + term
    return merged @ w_out


def hybrid_mixer(n_ctx, n_lat, pos_r, pos_c, w_in, g_q_a, w_uq, g_kv_a, w_ukv, g_qn, g_kn,
                 rpb, w_branch, w_out, need_ctx):
    B, L, _ = n_ctx.shape
    T = L + n_lat.shape[1]
    h = jnp.concatenate([n_ctx, n_lat], axis=1)
    proj = h @ w_in
    (p_dq, p_dkv, p_kr, p_qb, p_kb, p_vb, p_qc, p_kc, p_vc, p_gate) = jnp.split(proj, IN_SPLITS, axis=-1)

    q_a = (rms_norm(p_dq, g_q_a) @ w_uq).reshape(B, T, MLA_HEADS, MLA_NOPE + MLA_ROPE)
    q_a = jnp.concatenate([q_a[..., :MLA_NOPE], axial_rope(q_a[..., MLA_NOPE:], pos_r, pos_c)], axis=-1)
    kv_a = (rms_norm(p_dkv, g_kv_a) @ w_ukv).reshape(B, T, MLA_HEADS, MLA_NOPE + MLA_V)
    k_pe = axial_rope(p_kr[:, :, None, :], pos_r, pos_c)
    k_a = jnp.concatenate([kv_a[..., :MLA_NOPE],
                           jnp.broadcast_to(k_pe, (B, T, MLA_HEADS, MLA_ROPE))], axis=-1)
    v_a = kv_a[..., MLA_NOPE:]

    q_b = axial_rope(rms_norm(p_qb.reshape(B, T, GQA_HEADS, HEAD_DIM), g_qn), pos_r, pos_c)
    k_b = axial_rope(rms_norm(p_kb.reshape(B, T, GQA_KV_HEADS, HEAD_DIM), g_kn), pos_r, pos_c)
    v_b = p_vb.reshape(B, T, GQA_KV_HEADS, HEAD_DIM)

    q_c = p_qc.reshape(B, T, NA_HEADS, HEAD_DIM)
    k_c = p_kc.reshape(B, T, NA_HEADS, HEAD_DIM)
    v_c = p_vc.reshape(B, T, NA_HEADS, HEAD_DIM)

    o_lat = (
        block_attention(q_a[:, L:], k_a, v_a, MLA_SCALE),
        block_attention(q_b[:, L:], k_b, v_b, HEAD_SCALE),
        neighbourhood_attention(q_c[:, L:], k_c[:, L:], v_c[:, L:], k_c[:, :L], v_c[:, :L], rpb, HEAD_SCALE),
    )
    y_lat = gated_merge(o_lat, p_gate[:, L:], w_branch, w_out)
    if not need_ctx:
        return None, y_lat
    o_ctx = (
        block_attention(q_a[:, :L], k_a[:, :L], v_a[:, :L], MLA_SCALE),
        block_attention(q_b[:, :L], k_b[:, :L], v_b[:, :L], HEAD_SCALE),
        block_attention(q_c[:, :L], k_c[:, :L], v_c[:, :L], HEAD_SCALE),
    )
    y_ctx = gated_merge(o_ctx, p_gate[:, :L], w_branch, w_out)
    return y_ctx, y_lat


def clamped_swiglu(hdn):
    x_glu = jnp.minimum(hdn[..., ::2], SWIGLU_LIMIT)
    x_lin = jnp.clip(hdn[..., 1::2], -SWIGLU_LIMIT, SWIGLU_LIMIT)
    return x_glu * jax.nn.sigmoid(SWIGLU_ALPHA * x_glu) * (x_lin + 1)


def moe_ffn(h, w_router, b_router, w1, b1, w2, b2):
    N, D = h.shape
    logits = (h @ w_router).astype(jnp.float32) + b_router.astype(jnp.float32)
    top_val, top_idx = lax.top_k(logits, TOP_K)
    top_w = jax.nn.softmax(top_val, axis=-1)
    A = N * TOP_K
    flat_e = top_idx.reshape(A)
    flat_t = jnp.repeat(jnp.arange(N, dtype=jnp.int32), TOP_K)
    flat_w = top_w.reshape(A)
    order = jnp.argsort(flat_e)
    se, st, sw = flat_e[order], flat_t[order], flat_w[order]
    counts = jnp.bincount(flat_e, length=N_EXPERTS)
    padded = (counts + MOE_BLOCK - 1) // MOE_BLOCK * MOE_BLOCK
    pend = jnp.cumsum(padded)
    dest = (pend - padded)[se] + jnp.arange(A, dtype=jnp.int32) - (jnp.cumsum(counts) - counts)[se]
    n_blocks = -(-A // MOE_BLOCK) + N_EXPERTS
    P = n_blocks * MOE_BLOCK
    buf_tok = jnp.full((P,), N, dtype=jnp.int32).at[dest].set(st)
    buf_w = jnp.zeros((P,), jnp.float32).at[dest].set(sw)
    starts = jnp.arange(n_blocks, dtype=jnp.int32) * MOE_BLOCK
    blk_e = jnp.minimum(jnp.sum(pend[None, :] <= starts[:, None], axis=1), N_EXPERTS - 1)
    h_pad = jnp.concatenate([h, jnp.zeros((1, D), h.dtype)], axis=0)

    def one_block(acc, blk):
        tok, wt, e = blk
        a = clamped_swiglu(h_pad[tok] @ w1[e] + b1[e])
        y = (a @ w2[e] + b2[e]).astype(jnp.float32) * wt[:, None]
        return acc.at[tok].add(y), None

    acc, _ = lax.scan(one_block, jnp.zeros((N + 1, D), jnp.float32),
                      (buf_tok.reshape(n_blocks, MOE_BLOCK), buf_w.reshape(n_blocks, MOE_BLOCK), blk_e))
    return acc[:N].astype(h.dtype)


def setup_inputs(seed: int = 0) -> dict:
    key = jax.random.key(seed)
    ks = jax.random.split(key, 25)
    f32 = jnp.float32
    D = D_MODEL
    L = DEPTH

    def nrm(k, shape, scale):
        return jax.random.normal(k, shape, f32) * scale

    def gain(k, shape):
        return 1.0 + 0.05 * jax.random.normal(k, shape, f32)

    return {
        'x': nrm(ks[0], (BATCH, SEQ, D), 1.0),
        'c': nrm(ks[1], (BATCH, D), 1.0),
        'ctx': nrm(ks[2], (BATCH, CTX_LEN, D), 1.0),
        'c_ctx': nrm(ks[3], (D,), 1.0),
        'w_mod': nrm(ks[4], (L, D, 6 * D), MOD_SCALE * D ** -0.5),
        'b_mod': nrm(ks[5], (L, 6 * D), 0.02),
        'g_mix': gain(ks[6], (L, D)),
        'w_in': nrm(ks[7], (L, D, IN_COLS), D ** -0.5),
        'g_q_a': gain(ks[8], (L, MLA_Q_RANK)),
        'w_uq': nrm(ks[9], (L, MLA_Q_RANK, MLA_HEADS * (MLA_NOPE + MLA_ROPE)), MLA_Q_RANK ** -0.5),
        'g_kv_a': gain(ks[10], (L, MLA_KV_RANK)),
        'w_ukv': nrm(ks[11], (L, MLA_KV_RANK, MLA_HEADS * (MLA_NOPE + MLA_V)), MLA_KV_RANK ** -0.5),
        'g_qn': gain(ks[12], (L, HEAD_DIM)),
        'g_kn': gain(ks[13], (L, HEAD_DIM)),
        'rpb': nrm(ks[14], (L, NA_HEADS, 2 * NA_KH - 1, 2 * NA_KW - 1), 0.05),
        'w_branch': nrm(ks[15], (L, N_BRANCH, BRANCH_W, D), BRANCH_W ** -0.5),
        'w_out': nrm(ks[16], (L, D, D), D ** -0.5),
        'g_ffn': gain(ks[17], (L, D)),
        'w_router': nrm(ks[18], (L, D, N_EXPERTS), D ** -0.5),
        'b_router': nrm(ks[19], (L, N_EXPERTS), 0.01),
        'w_exp1': nrm(ks[20], (L, N_EXPERTS, D, 2 * D_FF), D ** -0.5),
        'b_exp1': nrm(ks[21], (L, N_EXPERTS, 2 * D_FF), 0.02),
        'w_exp2': nrm(ks[22], (L, N_EXPERTS, D_FF, D), D_FF ** -0.5),
        'b_exp2': nrm(ks[23], (L, N_EXPERTS, D), 0.02),
        'g_final': gain(ks[24], (D,)),
    }


def reference(x, c, ctx, c_ctx, w_mod, b_mod, g_mix, w_in, g_q_a, w_uq, g_kv_a, w_ukv, g_qn, g_kn,
              rpb, w_branch, w_out, g_ffn, w_router, b_router, w_exp1, b_exp1, w_exp2, b_exp2, g_final):
    B, S, D = x.shape
    L = ctx.shape[1]
    t = jnp.arange(S, dtype=jnp.int32)
    zeros_ctx = jnp.zeros((L,), jnp.float32)
    pos_r = jnp.concatenate([zeros_ctx, (t // GRID_W).astype(jnp.float32)])
    pos_c = jnp.concatenate([zeros_ctx, (t % GRID_W).astype(jnp.float32)])
    x_lat, x_ctx = x, ctx
    for l in range(DEPTH):
        last = l == DEPTH - 1
        mod_lat = jax.nn.silu(c) @ w_mod[l] + b_mod[l]
        mod_ctx = jax.nn.silu(c_ctx) @ w_mod[l] + b_mod[l]
        sh1, sc1, gt1, sh2, sc2, gt2 = jnp.split(mod_lat[:, None, :], 6, axis=-1)
        csh1, csc1, cgt1, csh2, csc2, cgt2 = jnp.split(mod_ctx, 6, axis=-1)

        n_lat = modulate(rms_norm(x_lat, g_mix[l]), sh1, sc1)
        n_ctx = modulate(rms_norm(x_ctx, g_mix[l]), csh1, csc1)
        y_ctx, y_lat = hybrid_mixer(n_ctx, n_lat, pos_r, pos_c, w_in[l], g_q_a[l], w_uq[l], g_kv_a[l],
                                    w_ukv[l], g_qn[l], g_kn[l], rpb[l], w_branch[l], w_out[l],
                                    need_ctx=not last)
        x_lat = x_lat + gt1 * y_lat
        n_lat = modulate(rms_norm(x_lat, g_ffn[l]), sh2, sc2)
        if last:
            f_lat = moe_ffn(n_lat.reshape(B * S, D), w_router[l], b_router[l],
                            w_exp1[l], b_exp1[l], w_exp2[l], b_exp2[l]).reshape(B, S, D)
        else:
            x_ctx = x_ctx + cgt1 * y_ctx
            n_ctx = modulate(rms_norm(x_ctx, g_ffn[l]), csh2, csc2)
            f_all = moe_ffn(jnp.concatenate([n_ctx, n_lat], axis=1).reshape(B * (L + S), D),
                            w_router[l], b_router[l], w_exp1[l], b_exp1[l],
                            w_exp2[l], b_exp2[l]).reshape(B, L + S, D)
            x_ctx = x_ctx + cgt2 * f_all[:, :L]
            f_lat = f_all[:, L:]
        x_lat = x_lat + gt2 * f_lat
    return rms_norm(x_lat, g_final)
```

```python
import contextlib
import numpy as np
import concourse.bass as bass
import concourse.mybir as mybir
from concourse.bass_utils import run_bass_kernel_spmd

F32 = mybir.dt.float32
BF16 = mybir.dt.bfloat16
ALU = mybir.AluOpType
AF = mybir.ActivationFunctionType
AX = mybir.AxisListType

NCORES = 8
SEM_G = 8192
NDMA = 40
NDMA_SW = 8


class AS:
    def __init__(self, nc, stack):
        self.nc = nc
        self.stack = stack
        self.streams = {k: [] for k in ('pe', 'act', 'dve', 'pool', 'sp')}
        self.count = {k: 0 for k in self.streams}
        self.waited = {k: {} for k in self.streams}
        self.last_w = {}
        self.readers = {}
        self.dma_i = 0
        self.dma_sw = 0
        self.csem = {k: [] for k in self.streams}
        self.dsem = [stack.enter_context(nc.semaphore(f"dq{i}")) for i in range(NDMA + NDMA_SW)]
        self.dma_final = {}

    def _deps(self, reads, writes):
        toks = {}

        def add(t):
            k = (t[0], t[1])
            if toks.get(k, 0) < t[2]:
                toks[k] = t[2]
        for b in reads:
            if b in self.last_w:
                add(self.last_w[b])
        for b in writes:
            if b in self.last_w:
                add(self.last_w[b])
            for k, v in self.readers.get(b, {}).items():
                add((k[0], k[1], v))
        return toks

    def _emit_waits(self, eng, toks):
        for k, val in toks.items():
            if k[0] == 'c' and k[1] == 'pe' and eng == 'pe':
                continue
            if self.waited[eng].get(k, 0) >= val:
                continue
            self.waited[eng][k] = val
            self.streams[eng].append(('wait', k, val))

    def _record(self, tok, reads, writes):
        k = (tok[0], tok[1])
        for b in reads:
            r = self.readers.setdefault(b, {})
            if r.get(k, 0) < tok[2]:
                r[k] = tok[2]
        for b in writes:
            self.last_w[b] = tok
            self.readers[b] = {}

    def op(self, eng, fn, reads=(), writes=()):
        toks = self._deps(reads, writes)
        self._emit_waits(eng, toks)
        self.count[eng] += 1
        idx = self.count[eng]
        self.streams[eng].append(('op', fn, idx))
        self._record(('c', eng, idx), reads, writes)

    def dma(self, eng, out, in_, reads=(), writes=(), **kw):
        if eng == 'pool':
            i = self.dma_sw
            self.dma_sw += 1
            s = NDMA + i % NDMA_SW
            prev = 16 * (i // NDMA_SW)
        else:
            i = self.dma_i
            self.dma_i += 1
            s = i % NDMA
            prev = 16 * (i // NDMA)
        toks = self._deps(reads, writes)
        if prev > 0:
            k = ('d', s)
            if toks.get(k, 0) < prev:
                toks[k] = prev
        self._emit_waits(eng, toks)
        self.streams[eng].append(('dma', out, in_, s, kw))
        self.dma_final[s] = prev + 16
        self._record(('d', s, prev + 16), reads, writes)

    def _sem_for(self, k, val):
        if k[0] == 'd':
            return self.dsem[k[1]], val
        eng = k[1]
        g = (val - 1) // SEM_G
        return self.csem[eng][g], (val - 1) % SEM_G + 1

    def finish(self):
        nc = self.nc
        for s, v in self.dma_final.items():
            self.streams['sp'].append(('wait', ('d', s), v))
        for eng in self.streams:
            ng = (self.count[eng] + SEM_G - 1) // SEM_G
            self.csem[eng] = [self.stack.enter_context(nc.semaphore(f"c_{eng}{g}")) for g in range(max(ng, 1))]
        engmap = {'pe': 'tensor', 'act': 'scalar', 'dve': 'vector', 'pool': 'gpsimd', 'sp': 'sync'}
        with nc.Block() as block:
            for eng, attr in engmap.items():
                stream = self.streams[eng]
                if not stream:
                    continue

                def body(e, stream=stream, eng=eng):
                    for it in stream:
                        if it[0] == 'wait':
                            sem, val = self._sem_for(it[1], it[2])
                            e.wait_ge(sem, val)
                        elif it[0] == 'op':
                            idx = it[2]
                            sem = self.csem[eng][(idx - 1) // SEM_G]
                            it[1](e).then_inc(sem, 1)
                        else:
                            _, out, in_, s, kw = it
                            e.dma_start(out=out, in_=in_, **kw).then_inc(self.dsem[s], 16)
                getattr(block, attr)(body)


def new_nc():
    return bass.Bass("TRN2", target_bir_lowering=False)


def run_spmd(nc, in_maps):
    res = run_bass_kernel_spmd(nc, in_maps, core_ids=list(range(len(in_maps))))
    return res.results


def build_mod(ncol):
    nc = new_nc()
    cT = nc.dram_tensor("cT", [2048, 8], F32, kind="ExternalInput").ap()
    w = nc.dram_tensor("w", [2048, ncol], F32, kind="ExternalInput").ap()
    b = nc.dram_tensor("b", [1, ncol], F32, kind="ExternalInput").ap()
    o = nc.dram_tensor("o", [8, ncol], F32, kind="ExternalOutput").ap()
    NT = ncol // 512
    with contextlib.ExitStack() as st:
        a = AS(nc, st)
        sb = lambda name, shape, dt: st.enter_context(nc.sbuf_tensor(name, shape, dt))
        ct = sb("ct", [128, 16, 8], F32)
        cs = sb("cs", [128, 16, 8], F32)
        wt = [sb(f"wt{i}", [128, 16, 512], F32) for i in range(2)]
        bt = sb("bt", [8, ncol], F32)
        ot = sb("ot", [8, ncol], F32)
        ps = [st.enter_context(nc.psum_tensor(f"ps{i}", [128, 512], F32)) for i in range(2)]
        a.dma('sp', ct[:], cT.rearrange("(k p) c -> p k c", p=128), writes=['ct'])
        a.dma('sp', bt[:], b.broadcast_to([8, ncol]), writes=['bt'])
        a.op('act', lambda e: e.activation(out=cs[:], in_=ct[:], func=AF.Silu), reads=['ct'], writes=['cs'])
        for t in range(NT):
            wb = wt[t % 2]
            a.dma('sp' if t % 2 == 0 else 'act', wb[:], w[:, t * 512:(t + 1) * 512].rearrange("(k p) n -> p k n", p=128),
                  writes=[f'wt{t % 2}'])
            p = ps[t % 2]
            for k in range(16):
                a.op('pe', lambda e, k=k, p=p, wb=wb: e.matmul(p[0:8, :], lhsT=cs[:, k, :], rhs=wb[:, k, :],
                                                               start=(k == 0), stop=(k == 15)),
                     reads=['cs', f'wt{t % 2}'], writes=[f'ps{t % 2}'])
            a.op('dve', lambda e, p=p, t=t: e.tensor_tensor(out=ot[:, t * 512:(t + 1) * 512], in0=p[0:8, :],
                                                            in1=bt[:, t * 512:(t + 1) * 512], op=ALU.add),
                 reads=[f'ps{t % 2}', 'bt'], writes=['ot'])
        a.dma('sp', o, ot[:], reads=['ot'], writes=['o'])
        a.finish()
    return nc


class Ctx:
    def __init__(self):
        self.nc = new_nc()
        self.st = contextlib.ExitStack()
        self.a = AS(self.nc, self.st)
        self.nps = 0

    def din(self, name, shape, dt=F32):
        return self.nc.dram_tensor(name, list(shape), dt, kind="ExternalInput").ap()

    def dout(self, name, shape, dt=F32):
        return self.nc.dram_tensor(name, list(shape), dt, kind="ExternalOutput").ap()

    def dscratch(self, name, shape, dt=F32):
        return self.nc.dram_tensor(name, list(shape), dt).ap()

    def sb(self, name, shape, dt=F32):
        return self.st.enter_context(self.nc.sbuf_tensor(name, list(shape), dt))

    def ps(self, name):
        self.nps += 1
        assert self.nps <= 8
        return self.st.enter_context(self.nc.psum_tensor(name, [128, 512], F32))

    def done(self):
        self.a.finish()
        self.st.close()
        return self.nc


class Rot:
    def __init__(self, items):
        self.items = items
        self.i = 0

    def next(self):
        it = self.items[self.i % len(self.items)]
        self.i += 1
        return it


def rope_ops(a, eng, x, C, S, t1, t2, out, H, Wd, keys):
    kx, kC, kS, k1, k2, ko = keys
    hw = Wd // 4
    xv = x.rearrange("p (h a f w) -> p h a f w", h=H, a=2, f=2, w=hw)
    t2v = t2.rearrange("p (h a f w) -> p h a f w", h=H, a=2, f=2, w=hw)
    Sv = S.rearrange("p (a f w) -> p a f w", a=2, f=2, w=hw)
    x3 = x.rearrange("p (h d) -> p h d", h=H)
    t13 = t1.rearrange("p (h d) -> p h d", h=H)
    Cb = C.unsqueeze(1).broadcast_to([128, H, Wd])
    a.op(eng, lambda e: e.tensor_tensor(out=t13, in0=x3, in1=Cb, op=ALU.mult), reads=[kx, kC], writes=[k1])
    for f in range(2):
        Sb = Sv[:, :, f, :].unsqueeze(1).broadcast_to([128, H, 2, hw])
        a.op(eng, lambda e, f=f, Sb=Sb: e.tensor_tensor(out=t2v[:, :, :, f, :], in0=xv[:, :, :, 1 - f, :], in1=Sb,
                                                        op=ALU.mult), reads=[kx, kS], writes=[k2])
    a.op(eng, lambda e: e.tensor_tensor(out=out, in0=t1, in1=t2, op=ALU.add), reads=[k1, k2], writes=[ko])


A_COLS = [
    ('dq', 0, 512, 'dq'), ('dkv', 512, 512, 'dkv'), ('kr', 1024, 64, 'kr'),
    ('qb0', 1088, 512, 'qb'), ('qb1', 1600, 512, 'qb'), ('kb', 2112, 256, 'kb'), ('vb', 2368, 256, 'copy'),
    ('qc0', 2624, 512, 'copy'), ('qc1', 3136, 512, 'copy'), ('kc0', 3648, 512, 'copy'), ('kc1', 4160, 512, 'copy'),
    ('vc0', 4672, 512, 'copy'), ('vc1', 5184, 512, 'copy'),
] + [(f'gt{i}', 5696 + 512 * i, 512, 'sig') for i in range(12)]
A_OUT = {'qa': 1536, 'kva': 2048, 'kpe': 64, 'qb': 1024, 'kb': 256, 'vb': 256, 'qc': 1024, 'kc': 1024, 'vc': 1024,
         'gate': 6144}
A_DEST = {'vb': ('vb', 0), 'qc0': ('qc', 0), 'qc1': ('qc', 512), 'kc0': ('kc', 0), 'kc1': ('kc', 512),
          'vc0': ('vc', 0), 'vc1': ('vc', 512), 'qb0': ('qb', 0), 'qb1': ('qb', 512), 'kb': ('kb', 0), 'kr': ('kpe', 0)}
EPS = 1e-6


def build_A(NT, g0_tiles):
    c = Ctx()
    a = c.a
    nc = c.nc
    NTT = NT // 128
    x = c.din("x", [NT, 2048])
    g = c.din("g", [1, 2048])
    sc = c.din("sc", [2, 2048])
    sh = c.din("sh", [2, 2048])
    w_in = c.din("w_in", [2048, 11840])
    g_q = c.din("g_q", [1, 512])
    g_kv = c.din("g_kv", [1, 512])
    w_uq = c.din("w_uq", [512, 1536])
    w_ukv = c.din("w_ukv", [512, 2048])
    g_qn = c.din("g_qn", [1, 128])
    g_kn = c.din("g_kn", [1, 128])
    ident = c.din("ident", [128, 128])
    ropeb = c.din("ropeb", [NT, 256])
    ropea = c.din("ropea", [NT, 128])
    outs = {k: c.dout(k, [NT, w], BF16) for k, w in A_OUT.items()}

    idf = c.sb("idf", [128, 128])
    idb = c.sb("idb", [128, 128], BF16)
    At = c.sb("At", [128, 2048])
    St = c.sb("St", [128, 2048])
    gt = c.sb("gt", [128, 2048])
    gq = c.sb("gq", [128, 512])
    gkv = c.sb("gkv", [128, 512])
    gqn = c.sb("gqn", [128, 128])
    gkn = c.sb("gkn", [128, 128])
    nT = c.sb("nT", [128, 16, NT], BF16)
    wuq = c.sb("wuq", [128, 4, 1536], BF16)
    wukv = c.sb("wukv", [128, 4, 2048], BF16)
    xt = Rot([(c.sb(f"xt{i}", [128, 2048]), f"xt{i}") for i in range(1)])
    nb = Rot([(c.sb(f"nb{i}", [128, 2048], BF16), f"nb{i}") for i in range(1)])
    scr = c.sb("scr", [128, 2048])
    small = Rot([(c.sb(f"sm{i}", [128, 16]), f"sm{i}") for i in range(4)])
    wt = Rot([(c.sb(f"wt{i}", [128, 16, 512], BF16), f"wt{i}") for i in range(2)])
    ob = Rot([(c.sb(f"ob{i}", [128, 512], BF16), f"ob{i}") for i in range(3)])
    w1 = Rot([(c.sb(f"w1_{i}", [128, 1024]), f"w1_{i}") for i in range(2)])
    w2 = c.sb("w2", [128, 1024])
    w3 = c.sb("w3", [128, 1024])
    ynb = c.sb("ynb", [128, 512], BF16)
    ynT = c.sb("ynT", [128, 4, 128], BF16)
    qab = c.sb("qab", [128, 2048], BF16)
    rb = Rot([(c.sb(f"rb{i}", [128, 256]), f"rb{i}") for i in range(2)])
    ra = Rot([(c.sb(f"ra{i}", [128, 128]), f"ra{i}") for i in range(2)])
    pst = Rot([(c.ps(f"pst{i}"), f"pst{i}") for i in range(2)])
    psm = Rot([(c.ps(f"psm{i}"), f"psm{i}") for i in range(3)])
    psu = Rot([(c.ps(f"psu{i}"), f"psu{i}") for i in range(3)])

    epsb = c.sb("epsb", [128, 1])
    a.op('dve', lambda e: e.memset(epsb[:], EPS), writes=['epsb'])
    a.dma('sp', idf[:], ident, writes=['idf'])
    a.op('dve', lambda e: e.tensor_copy(out=idb[:], in_=idf[:]), reads=['idf'], writes=['idb'])
    a.dma('sp', gt[:], g.broadcast_to([128, 2048]), writes=['gt'])
    a.dma('sp', gq[:], g_q.broadcast_to([128, 512]), writes=['gq'])
    a.dma('sp', gkv[:], g_kv.broadcast_to([128, 512]), writes=['gkv'])
    a.dma('sp', gqn[:], g_qn.broadcast_to([128, 128]), writes=['gqn'])
    a.dma('sp', gkn[:], g_kn.broadcast_to([128, 128]), writes=['gkn'])
    a.dma('pool', wuq[:], w_uq.rearrange("(k p) n -> p k n", p=128), writes=['wuq'])
    a.dma('pool', wukv[:], w_ukv.rearrange("(k p) n -> p k n", p=128), writes=['wukv'])

    def load_group(gi):
        a.dma('sp', At[:], sc[gi:gi + 1, :].broadcast_to([128, 2048]), writes=['At'])
        a.dma('sp', St[:], sh[gi:gi + 1, :].broadcast_to([128, 2048]), writes=['St'])
        a.op('dve', lambda e: e.scalar_tensor_tensor(out=At[:], in0=At[:], scalar=1.0, in1=gt[:], op0=ALU.add,
                                                     op1=ALU.mult), reads=['At', 'gt'], writes=['At'])

    def rstd_from(ssap, sskey, n, outap, outkey):
        a.op('act', lambda e: e.activation(out=outap, in_=ssap, func=AF.Sqrt, bias=epsb[:, 0:1], scale=1.0 / n),
             reads=[sskey, 'epsb'], writes=[outkey])
        a.op('dve', lambda e: e.reciprocal(out=outap, in_=outap), reads=[outkey], writes=[outkey])

    for tt in range(NTT):
        if tt == 0:
            load_group(0)
        elif tt == g0_tiles:
            load_group(1)
        xtile, xk = xt.next()
        a.dma('sp', xtile[:], x[tt * 128:(tt + 1) * 128, :], writes=[xk])
        sm, smk = small.next()
        a.op('dve', lambda e, sm=sm: e.memset(sm[:], 0.0), writes=[smk])
        a.op('act', lambda e, xtile=xtile, sm=sm: e.activation(out=scr[:], in_=xtile[:], func=AF.Square,
                                                               accum_out=sm[:, 0:1]),
             reads=[xk, smk], writes=['scr', smk])
        rstd_from(sm[:, 0:1], smk, 2048, sm[:, 1:2], smk)
        a.op('dve', lambda e, xtile=xtile, sm=sm: e.scalar_tensor_tensor(out=xtile[:], in0=xtile[:], scalar=sm[:, 1:2],
                                                                         in1=At[:], op0=ALU.mult, op1=ALU.mult),
             reads=[xk, smk, 'At'], writes=[xk])
        nbt, nbk = nb.next()
        a.op('pool', lambda e, xtile=xtile, nbt=nbt: e.tensor_tensor(out=nbt[:], in0=xtile[:], in1=St[:], op=ALU.add),
             reads=[xk, 'St'], writes=[nbk])
        for q4 in range(4):
            p, pk = pst.next()
            for j in range(4):
                kc = q4 * 4 + j
                a.op('pe', lambda e, p=p, j=j, kc=kc, nbt=nbt: e.matmul(p[:, j * 128:(j + 1) * 128],
                                                                          lhsT=nbt[:, kc * 128:(kc + 1) * 128],
                                                                          rhs=idb[:], start=True, stop=True),
                     reads=[nbk, 'idb'], writes=[pk])
            eng = 'act' if q4 % 2 == 0 else 'dve'
            dst = nT[:, q4 * 4:(q4 + 1) * 4, tt * 128:(tt + 1) * 128]
            src = p[:].rearrange("p (j t) -> p j t", j=4)
            if eng == 'act':
                a.op('act', lambda e, dst=dst, src=src: e.copy(out=dst, in_=src), reads=[pk], writes=[('nT', tt)])
            else:
                a.op('dve', lambda e, dst=dst, src=src: e.tensor_copy(out=dst, in_=src), reads=[pk], writes=[('nT', tt)])

    def store(src_tile, src_key, name, col0, width, tt):
        a.dma('act', outs[name][tt * 128:(tt + 1) * 128, col0:col0 + width], src_tile[:, 0:width],
              reads=[src_key], writes=[('out', name, col0, tt)])

    def headnorm_rope(p, pk, cw, gtile, gkey, H, tt, name, col0):
        rbt, rbk = rb.next()
        a.dma('sp', rbt[:], ropeb[tt * 128:(tt + 1) * 128, :], writes=[rbk])
        xa, xak = w1.next()
        a.op('act', lambda e: e.copy(out=xa[:, 0:cw], in_=p[:, 0:cw]), reads=[pk], writes=[xak])
        a.op('pool', lambda e: e.tensor_tensor(out=w2[:, 0:cw], in0=xa[:, 0:cw], in1=xa[:, 0:cw], op=ALU.mult),
             reads=[xak], writes=['w2'])
        sm, smk = small.next()
        a.op('dve', lambda e: e.tensor_reduce(out=sm[:, 0:H], in_=w2[:, 0:cw].rearrange("p (h d) -> p h d", h=H),
                                              axis=AX.X, op=ALU.add), reads=['w2'], writes=[smk])
        rstd_from(sm[:, 0:H], smk, 128, sm[:, 8:8 + H], smk)
        x3 = xa[:, 0:cw].rearrange("p (h d) -> p h d", h=H)
        a.op('dve', lambda e: e.tensor_tensor(out=x3, in0=x3, in1=sm[:, 8:8 + H].unsqueeze(2).broadcast_to([128, H, 128]),
                                              op=ALU.mult), reads=[xak, smk], writes=[xak])
        a.op('dve', lambda e: e.tensor_tensor(out=x3, in0=x3, in1=gtile[:].unsqueeze(1).broadcast_to([128, H, 128]),
                                              op=ALU.mult), reads=[xak, gkey], writes=[xak])
        obt, obk = ob.next()
        rope_ops(a, 'dve', xa[:, 0:cw], rbt[:, 0:128], rbt[:, 128:256], w2[:, 0:cw], w3[:, 0:cw], obt[:, 0:cw], H, 128,
                 (xak, rbk, rbk, 'w2', 'w3', obk))
        store(obt, obk, name, col0, cw, tt)

    def upproj(p, pk, gtile, gkey, wres, wkey, nout, oname, tt, do_rope):
        sm, smk = small.next()
        a.op('dve', lambda e: e.memset(sm[:], 0.0), writes=[smk])
        a.op('act', lambda e: e.activation(out=w2[:, 0:512], in_=p[:, 0:512], func=AF.Square, accum_out=sm[:, 0:1]),
             reads=[pk, smk], writes=['w2', smk])
        rstd_from(sm[:, 0:1], smk, 512, sm[:, 1:2], smk)
        a.op('dve', lambda e: e.scalar_tensor_tensor(out=ynb[:], in0=p[:, 0:512], scalar=sm[:, 1:2], in1=gtile[:],
                                                     op0=ALU.mult, op1=ALU.mult), reads=[pk, smk, gkey], writes=['ynb'])
        pt, ptk = pst.next()
        for j in range(4):
            a.op('pe', lambda e, j=j: e.matmul(pt[:, j * 128:(j + 1) * 128], lhsT=ynb[:, j * 128:(j + 1) * 128],
                                               rhs=idb[:], start=True, stop=True), reads=['ynb', 'idb'], writes=[ptk])
        a.op('act', lambda e: e.copy(out=ynT[:], in_=pt[:].rearrange("p (j t) -> p j t", j=4)), reads=[ptk],
             writes=['ynT'])
        for n0 in range(0, nout, 512):
            pu, puk = psu.next()
            for k in range(4):
                a.op('pe', lambda e, k=k, n0=n0, pu=pu: e.matmul(pu[:, 0:512], lhsT=ynT[:, k, :], rhs=wres[:, k, n0:n0 + 512],
                                                          start=(k == 0), stop=(k == 3)), reads=['ynT', wkey], writes=[puk])
            if do_rope:
                a.op('act', lambda e, n0=n0, pu=pu: e.copy(out=w3[:, 0:512], in_=pu[:, 0:512]), reads=[puk], writes=['w3'])
                a.op('pool', lambda e, n0=n0: e.tensor_copy(out=scr[:, n0:n0 + 512], in_=w3[:, 0:512]), reads=['w3'],
                     writes=['scr'])
            else:
                eng = 'act' if (n0 // 512) % 2 == 0 else 'dve'
                if eng == 'act':
                    a.op('act', lambda e, n0=n0, pu=pu: e.copy(out=qab[:, n0:n0 + 512], in_=pu[:, 0:512]), reads=[puk],
                         writes=['qab'])
                else:
                    a.op('dve', lambda e, n0=n0, pu=pu: e.tensor_copy(out=qab[:, n0:n0 + 512], in_=pu[:, 0:512]), reads=[puk],
                         writes=['qab'])
        if do_rope:
            rat, rak = ra.next()
            a.dma('sp', rat[:], ropea[tt * 128:(tt + 1) * 128, :], writes=[rak])
            q3 = scr[:, 0:1536].rearrange("p (h d) -> p h d", h=8)
            a.op('dve', lambda e: e.tensor_copy(out=w2[:, 0:512].rearrange("p (h d) -> p h d", h=8), in_=q3[:, :, 128:192]),
                 reads=['scr'], writes=['w2'])
            xa, xak = w1.next()
            rope_ops(a, 'dve', w2[:, 0:512], rat[:, 0:64], rat[:, 64:128], w3[:, 0:512], w3[:, 512:1024], xa[:, 0:512], 8, 64,
                     ('w2', rak, rak, 'w3', 'w3', xak))
            qv = qab[:, 0:1536].rearrange("p (h d) -> p h d", h=8)
            a.op('act', lambda e: e.copy(out=qv[:, :, 0:128], in_=q3[:, :, 0:128]), reads=['scr'], writes=['qab'])
            a.op('dve', lambda e: e.tensor_copy(out=qv[:, :, 128:192], in_=xa[:, 0:512].rearrange("p (h d) -> p h d", h=8)),
                 reads=[xak], writes=['qab'])
        a.dma('act', outs[oname][tt * 128:(tt + 1) * 128, :], qab[:, 0:nout], reads=['qab'], writes=[('out', oname, tt)])

    for (cname, c0, cw, kind) in A_COLS:
        wtile, wk = wt.next()
        a.dma('pool', wtile[:, :, 0:cw], w_in[:, c0:c0 + cw].rearrange("(k p) n -> p k n", p=128), writes=[wk])
        for tt in range(NTT):
            p, pk = psm.next()
            for k in range(16):
                a.op('pe', lambda e, p=p, k=k, tt=tt, wtile=wtile: e.matmul(p[:, 0:cw], lhsT=nT[:, k, tt * 128:(tt + 1) * 128],
                                                                             rhs=wtile[:, k, 0:cw], start=(k == 0),
                                                                             stop=(k == 15)),
                     reads=[('nT', tt), wk], writes=[pk])
            if kind in ('copy', 'sig'):
                obt, obk = ob.next()
                if kind == 'sig':
                    a.op('act', lambda e, p=p, obt=obt: e.activation(out=obt[:, 0:cw], in_=p[:, 0:cw], func=AF.Sigmoid),
                         reads=[pk], writes=[obk])
                    store(obt, obk, 'gate', c0 - 5696, cw, tt)
                else:
                    a.op('dve', lambda e, p=p, obt=obt: e.tensor_copy(out=obt[:, 0:cw], in_=p[:, 0:cw]), reads=[pk],
                         writes=[obk])
                    dn, dc = A_DEST[cname]
                    store(obt, obk, dn, dc, cw, tt)
            elif kind == 'qb':
                dn, dc = A_DEST[cname]
                headnorm_rope(p, pk, cw, gqn, 'gqn', 4, tt, dn, dc)
            elif kind == 'kb':
                headnorm_rope(p, pk, cw, gkn, 'gkn', 2, tt, 'kb', 0)
            elif kind == 'kr':
                rat, rak = ra.next()
                a.dma('sp', rat[:], ropea[tt * 128:(tt + 1) * 128, :], writes=[rak])
                xa, xak = w1.next()
                a.op('act', lambda e, p=p, xa=xa: e.copy(out=xa[:, 0:64], in_=p[:, 0:64]), reads=[pk], writes=[xak])
                obt, obk = ob.next()
                rope_ops(a, 'dve', xa[:, 0:64], rat[:, 0:64], rat[:, 64:128], w2[:, 0:64], w3[:, 0:64], obt[:, 0:64], 1, 64,
                         (xak, rak, rak, 'w2', 'w3', obk))
                store(obt, obk, 'kpe', 0, 64, tt)
            elif kind == 'dq':
                upproj(p, pk, gq, 'gq', wuq, 'wuq', 1536, 'qa', tt, True)
            elif kind == 'dkv':
                upproj(p, pk, gkv, 'gkv', wukv, 'wukv', 2048, 'kva', tt, False)
    return c.done()


NQ = 2176
NK = 4352
NKL = 256 + 40 * 64
NA_SLOTS = 60


def na_row_chunks(i):
    if i < 4:
        cs, ce = i // 2, 5
    elif i >= 28:
        cs, ce = 14, (i + 7) // 2
    else:
        cs, ce = i // 2, (i + 7) // 2
    typ = i if i < 4 else (6 + i - 28 if i >= 28 else 4 + (i % 2))
    return typ, list(range(cs, ce + 1))


def build_B(nheads=8, nrows=32, do=('a', 'b', 'c')):
    c = Ctx()
    a = c.a
    qaT = c.din("qaT", [8, 192, NQ], BF16)
    kaT = c.din("kaT", [8, 128, NK], BF16)
    kpeT = c.din("kpeT", [64, NK], BF16)
    va = c.din("va", [NK, 1024], BF16)
    qbT = c.din("qbT", [8, 128, NQ], BF16)
    kbT = c.din("kbT", [2, 128, NK], BF16)
    vb = c.din("vb", [NK, 256], BF16)
    qcT = c.din("qcT", [8, 128, NQ], BF16)
    kcT = c.din("kcT", [8, 128, NKL], BF16)
    vc = c.din("vc", [NKL, 1024], BF16)
    nab = c.din("nab", [8, 128, NA_SLOTS * 64])
    outs = {k: c.dout(k, [1024, NQ], BF16) for k in ('oaT', 'obT', 'ocT')}

    ones = c.sb("ones", [128, 128], BF16)
    a.op('dve', lambda e: e.memset(ones[:], 1.0), writes=['ones'])
    kt = Rot([(c.sb(f"kt{i}", [128, NK], BF16), f"kt{i}") for i in range(2)])
    kpe = c.sb("kpe", [64, NK], BF16)
    ktb = c.sb("ktb", [128, NK], BF16)
    vtb = c.sb("vtb", [128, 34, 128], BF16)
    vt = Rot([(c.sb(f"vt{i}", [128, 34, 128], BF16), f"vt{i}") for i in range(2)])
    qt = Rot([(c.sb(f"qt{i}", [128, NQ], BF16), f"qt{i}") for i in range(2)])
    qr = Rot([(c.sb(f"qr{i}", [64, NQ], BF16), f"qr{i}") for i in range(2)])
    pt = Rot([(c.sb(f"pt{i}", [128, 512], BF16), f"pt{i}") for i in range(3)])
    ot = Rot([(c.sb(f"ot{i}", [128, NQ], BF16), f"ot{i}") for i in range(2)])
    rs = Rot([(c.sb(f"rs{i}", [128, 512]), f"rs{i}") for i in range(2)])
    ef = c.sb("ef", [128, NA_SLOTS * 64])
    eb = Rot([(c.sb(f"eb{i}", [128, NA_SLOTS * 64], BF16), f"eb{i}") for i in range(2)])
    pss = Rot([(c.ps(f"pss{i}"), f"pss{i}") for i in range(3)])
    pso = Rot([(c.ps(f"pso{i}"), f"pso{i}") for i in range(2)])
    psr = Rot([(c.ps(f"psr{i}"), f"psr{i}") for i in range(2)])
    a.dma('sp', kpe[:], kpeT, writes=['kpe'])

    def attend(qparts, kparts, vtile, vk, q0, n, kchunks, scale, otile, ok, etab=None, kcol=None):
        po, pok = pso.next()
        pr, prk = psr.next()
        nk = len(kchunks)
        if etab is None:
            for ji, j in enumerate(kchunks):
                p, pk = pss.next()
                for pi, ((qtile, qk, nr), (ktile, kk, _)) in enumerate(zip(qparts, kparts)):
                    a.op('pe', lambda e, p=p, qtile=qtile, ktile=ktile, nr=nr, j=j, pi=pi: e.matmul(
                        p[:, 0:n], lhsT=ktile[0:nr, j * 128:(j + 1) * 128], rhs=qtile[0:nr, q0:q0 + n],
                        start=(pi == 0), stop=(pi == len(qparts) - 1)), reads=[qk, kk], writes=[pk])
                ptile, ptk = pt.next()
                a.op('act', lambda e, p=p, ptile=ptile: e.activation(out=ptile[:, 0:n], in_=p[:, 0:n], func=AF.Exp,
                                                                     scale=scale), reads=[pk], writes=[ptk])
                a.op('pe', lambda e, ptile=ptile, j=j, ji=ji: e.matmul(po[:, 0:n], lhsT=vtile[:, j, :], rhs=ptile[:, 0:n],
                                                                       start=(ji == 0), stop=(ji == nk - 1)),
                     reads=[vk, ptk], writes=[pok])
                a.op('pe', lambda e, ptile=ptile, ji=ji: e.matmul(pr[:, 0:n], lhsT=ones[:], rhs=ptile[:, 0:n],
                                                                  start=(ji == 0), stop=(ji == nk - 1)),
                     reads=['ones', ptk], writes=[prk])
        else:
            etile, ek, slot0, nloc = etab
            p, pk = pss.next()
            (qtile, qk, nr), (ktile, kk, _) = qparts[0], kparts[0]
            for ji, j in enumerate(kchunks):
                a.op('pe', lambda e, j=j, ji=ji: e.matmul(p[:, ji * 64:(ji + 1) * 64], lhsT=ktile[0:nr, j * 128:(j + 1) * 128],
                                                          rhs=qtile[0:nr, q0:q0 + 64], start=True, stop=True),
                     reads=[qk, kk], writes=[pk])
            ptile, ptk = pt.next()
            a.op('act', lambda e: e.activation(out=ptile[:, 0:nk * 64], in_=p[:, 0:nk * 64], func=AF.Exp, scale=scale),
                 reads=[pk], writes=[ptk])
            a.op('dve', lambda e: e.tensor_tensor(out=ptile[:, 0:nloc * 64], in0=ptile[:, 0:nloc * 64],
                                                  in1=etile[:, slot0 * 64:(slot0 + nloc) * 64], op=ALU.mult),
                 reads=[ptk, ek], writes=[ptk])
            for ji, j in enumerate(kchunks):
                a.op('pe', lambda e, j=j, ji=ji: e.matmul(po[:, 0:64], lhsT=vtile[:, j, :], rhs=ptile[:, ji * 64:(ji + 1) * 64],
                                                          start=(ji == 0), stop=(ji == nk - 1)), reads=[vk, ptk], writes=[pok])
            for ji, j in enumerate(kchunks):
                a.op('pe', lambda e, ji=ji: e.matmul(pr[:, 0:64], lhsT=ones[:], rhs=ptile[:, ji * 64:(ji + 1) * 64],
                                                     start=(ji == 0), stop=(ji == nk - 1)), reads=['ones', ptk], writes=[prk])
        rt, rk = rs.next()
        a.op('dve', lambda e: e.reciprocal(out=rt[:, 0:n], in_=pr[:, 0:n]), reads=[prk], writes=[rk])
        a.op('dve', lambda e: e.tensor_tensor(out=otile[:, q0:q0 + n], in0=po[:, 0:n], in1=rt[:, 0:n], op=ALU.mult),
             reads=[pok, rk], writes=[ok])

    ALLK = list(range(34))
    for h in range(nheads):
        if 'a' in do:
            ktile, kk = kt.next()
            a.dma('sp', ktile[:], kaT[h], writes=[kk])
            vtile, vk = vt.next()
            a.dma('act', vtile[:], va[:, h * 128:(h + 1) * 128].rearrange("(c p) d -> p c d", p=128), writes=[vk])
            qtile, qk = qt.next()
            a.dma('sp', qtile[:], qaT[h, 0:128, :], writes=[qk])
            qrt, qrk = qr.next()
            a.dma('sp', qrt[:], qaT[h, 128:192, :], writes=[qrk])
            otile, ok = ot.next()
            qp = [(qtile, qk, 128), (qrt, qrk, 64)]
            kp = [(ktile, kk, 128), (kpe, 'kpe', 64)]
            attend(qp, kp, vtile, vk, 0, 128, [0, 1], 192 ** -0.5, otile, ok)
            for t in range(4):
                attend(qp, kp, vtile, vk, 128 + 512 * t, 512, ALLK, 192 ** -0.5, otile, ok)
            a.dma('act', outs['oaT'][h * 128:(h + 1) * 128, :], otile[:], reads=[ok], writes=[('oa', h)])
        if 'b' in do:
            if h % 4 == 0:
                kbtile, kbk = ktb, 'ktb'
                a.dma('sp', kbtile[:], kbT[h // 4], writes=[kbk])
                vbtile, vbk = vtb, 'vtb'
                a.dma('act', vbtile[:], vb[:, (h // 4) * 128:(h // 4 + 1) * 128].rearrange("(c p) d -> p c d", p=128),
                      writes=[vbk])
            qtile, qk = qt.next()
            a.dma('sp', qtile[:], qbT[h], writes=[qk])
            otile, ok = ot.next()
            qp = [(qtile, qk, 128)]
            kp = [(kbtile, kbk, 128)]
            attend(qp, kp, vbtile, vbk, 0, 128, [0, 1], 128 ** -0.5, otile, ok)
            for t in range(4):
                attend(qp, kp, vbtile, vbk, 128 + 512 * t, 512, ALLK, 128 ** -0.5, otile, ok)
            a.dma('act', outs['obT'][h * 128:(h + 1) * 128, :], otile[:], reads=[ok], writes=[('ob', h)])
        if 'c' in do:
            ktile, kk = kt.next()
            a.dma('sp', ktile[:, 0:NKL], kcT[h], writes=[kk])
            vtile, vk = vt.next()
            a.dma('act', vtile[:, 0:22, :], vc[:, h * 128:(h + 1) * 128].rearrange("(c p) d -> p c d", p=128), writes=[vk])
            qtile, qk = qt.next()
            a.dma('sp', qtile[:], qcT[h], writes=[qk])
            a.dma('sp', ef[:], nab[h], writes=['ef'])
            etile, ek = eb.next()
            a.op('act', lambda e, etile=etile: e.activation(out=etile[:], in_=ef[:], func=AF.Exp), reads=['ef'], writes=[ek])
            otile, ok = ot.next()
            qp = [(qtile, qk, 128)]
            kp = [(ktile, kk, 128)]
            attend(qp, kp, vtile, vk, 0, 128, [0, 1], 128 ** -0.5, otile, ok)
            for i in range(nrows):
                typ, chunks = na_row_chunks(i)
                kch = [2 + cc for cc in chunks] + [0, 1]
                attend(qp, kp, vtile, vk, 128 + 64 * i, 64, kch, 128 ** -0.5, otile, ok,
                       etab=(etile, ek, typ * 6, len(chunks)))
            a.dma('act', outs['ocT'][h * 128:(h + 1) * 128, :], otile[:], reads=[ok], writes=[('oc', h)])
    return c.done()


def na_bias_table(rpb_l, half):
    tab = np.full((8, NA_SLOTS, 128, 64), -30000.0, np.float32)
    rep = {0: 0, 1: 1, 2: 2, 3: 3, 4: 4, 5: 5, 6: 28, 7: 29, 8: 30, 9: 31}
    cq = np.arange(64)
    c0 = np.clip(cq - 8, 0, 48)
    ck = np.arange(64)
    colvalid = (ck[:, None] >= c0[None, :]) & (ck[:, None] < c0[None, :] + 16)
    dc = np.clip(ck[:, None] - cq[None, :] + 15, 0, 30)
    for typ, i in rep.items():
        _, chunks = na_row_chunks(i)
        r = 32 * half + i
        r0 = min(max(r - 4, 0), 56)
        for j, cc in enumerate(chunks):
            for rl in range(2):
                rk = 2 * cc + rl + 32 * half - 4
                if rk < 0 or rk >= 64 or rk < r0 or rk >= r0 + 8:
                    continue
                vals = rpb_l[:, rk - r + 7][:, dc]
                blk = tab[:, typ * 6 + j, rl * 64:(rl + 1) * 64, :]
                blk[:, colvalid] = vals[:, colvalid]
    return np.ascontiguousarray(tab.transpose(0, 2, 1, 3).reshape(8, 128, NA_SLOTS * 64))


def build_C(NT, g0_tiles):
    c = Ctx()
    a = c.a
    NTT = NT // 128
    oT = c.din("oT", [3, 1024, NT], BF16)
    gate = c.din("gate", [NT, 6144], BF16)
    x = c.din("x", [NT, 2048])
    w_branch = c.din("w_branch", [3, 1024, 2048])
    w_out = c.din("w_out", [2048, 2048])
    gt1 = c.din("gt1", [2, 2048])
    g = c.din("g", [1, 2048])
    sc = c.din("sc", [2, 2048])
    sh = c.din("sh", [2, 2048])
    w_r = c.din("w_r", [2048, 32])
    b_r = c.din("b_r", [1, 32])
    ident = c.din("ident", [128, 128])
    xo = c.dout("xo", [NT, 2048])
    n2o = c.dout("n2", [NT, 2048], BF16)
    rwo = c.dout("rw", [NT, 32])
    mTd = c.dscratch("mTd", [2048, NT], BF16)

    idf = c.sb("idf", [128, 128])
    idb = c.sb("idb", [128, 128], BF16)
    epsb = c.sb("epsb", [128, 1])
    wbig = c.sb("wbig", [128, 16 * 2048], BF16)
    gtile = Rot([(c.sb(f"gtl{i}", [128, 3, 1024], BF16), f"gtl{i}") for i in range(2)])
    otile = Rot([(c.sb(f"otl{i}", [128, 24, 128], BF16), f"otl{i}") for i in range(2)])
    S1 = c.sb("S1", [128, 2048])
    S2 = c.sb("S2", [128, 2048])
    S3 = c.sb("S3", [128, 2048])
    S4 = c.sb("S4", [128, 2048])
    mb = c.sb("mb", [128, 1024], BF16)
    mT = Rot([(c.sb(f"mT{i}", [128, 16, 128], BF16), f"mT{i}") for i in range(2)])
    G1 = c.sb("G1", [128, 2048])
    A2 = c.sb("A2", [128, 2048])
    Sh2 = c.sb("Sh2", [128, 2048])
    gf = c.sb("gf", [128, 2048])
    n2b = c.sb("n2b", [128, 2048], BF16)
    n2T = c.sb("n2T", [128, 16, 128])
    wr = c.sb("wr", [128, 16, 32])
    br = c.sb("br", [128, 32])
    small = Rot([(c.sb(f"sm{i}", [128, 48]), f"sm{i}") for i in range(3)])
    lg = c.sb("lg", [128, 32])
    ee = c.sb("ee", [128, 32])
    psm = Rot([(c.ps(f"psm{i}"), f"psm{i}") for i in range(4)])
    pst = Rot([(c.ps(f"pst{i}"), f"pst{i}") for i in range(2)])
    psl = c.ps("psl")

    a.op('dve', lambda e: e.memset(epsb[:], EPS), writes=['epsb'])
    a.dma('sp', idf[:], ident, writes=['idf'])
    a.op('dve', lambda e: e.tensor_copy(out=idb[:], in_=idf[:]), reads=['idf'], writes=['idb'])
    a.dma('sp', gf[:], g.broadcast_to([128, 2048]), writes=['gf'])
    a.dma('sp', wr[:], w_r.rearrange("(k p) n -> p k n", p=128), writes=['wr'])
    a.dma('sp', br[:], b_r.broadcast_to([128, 32]), writes=['br'])

    for half in range(2):
        wv = wbig[:, 0:24 * 1024].rearrange("p (k n) -> p k n", k=24)
        for i in range(3):
            a.dma('pool', wv[:, i * 8:(i + 1) * 8, :],
                  w_branch[i, :, half * 1024:(half + 1) * 1024].rearrange("(k p) n -> p k n", p=128), writes=['wbig'])
        for tt in range(NTT):
            ot_, otk = otile.next()
            a.dma('sp', ot_[:].rearrange("p (i k) t -> p i k t", i=3),
                  oT[:, :, tt * 128:(tt + 1) * 128].rearrange("i (k p) t -> p i k t", p=128), writes=[otk])
            gt_, gtk = gtile.next()
            a.dma('act', gt_[:], gate[tt * 128:(tt + 1) * 128, :].rearrange("t (i n) -> t i n", i=3)[:, :, half * 1024:(half + 1) * 1024],
                  writes=[gtk])
            for n0 in range(0, 1024, 512):
                for i in range(3):
                    p, pk = psm.next()
                    for k in range(8):
                        a.op('pe', lambda e, p=p, k=k, i=i, n0=n0, ot_=ot_: e.matmul(p[:, :], lhsT=ot_[:, i * 8 + k, :],
                                                                                     rhs=wv[:, i * 8 + k, n0:n0 + 512],
                                                                                     start=(k == 0), stop=(k == 7)),
                             reads=[otk, 'wbig'], writes=[pk])
                    if i == 0:
                        a.op('dve', lambda e, p=p, n0=n0, gt_=gt_: e.tensor_tensor(out=S1[:, n0:n0 + 512], in0=p[:, :],
                                                                                   in1=gt_[:, 0, n0:n0 + 512], op=ALU.mult),
                             reads=[pk, gtk], writes=['S1'])
                    else:
                        a.op('dve', lambda e, p=p, n0=n0, i=i, gt_=gt_: e.tensor_tensor(out=S2[:, n0:n0 + 512], in0=p[:, :],
                                                                                        in1=gt_[:, i, n0:n0 + 512], op=ALU.mult),
                             reads=[pk, gtk], writes=['S2'])
                        if i == 1:
                            a.op('pool', lambda e, n0=n0: e.tensor_tensor(out=S1[:, n0:n0 + 512], in0=S1[:, n0:n0 + 512],
                                                                          in1=S2[:, n0:n0 + 512], op=ALU.add),
                                 reads=['S1', 'S2'], writes=['S1'])
                        else:
                            a.op('pool', lambda e, n0=n0: e.tensor_tensor(out=mb[:, n0:n0 + 512], in0=S1[:, n0:n0 + 512],
                                                                          in1=S2[:, n0:n0 + 512], op=ALU.add),
                                 reads=['S1', 'S2'], writes=['mb'])
            mt_, mtk = mT.next()
            for q4 in range(2):
                p, pk = pst.next()
                for j in range(4):
                    kc = q4 * 4 + j
                    a.op('pe', lambda e, p=p, j=j, kc=kc: e.matmul(p[:, j * 128:(j + 1) * 128], lhsT=mb[:, kc * 128:(kc + 1) * 128],
                                                                   rhs=idb[:], start=True, stop=True), reads=['mb', 'idb'], writes=[pk])
                a.op('act', lambda e, p=p, q4=q4, mt_=mt_: e.copy(out=mt_[:, q4 * 4:(q4 + 1) * 4, :],
                                                                  in_=p[:].rearrange("p (j t) -> p j t", j=4)), reads=[pk], writes=[mtk])
            a.dma('sp', mTd[half * 1024:(half + 1) * 1024, tt * 128:(tt + 1) * 128].rearrange("(k p) t -> p k t", p=128),
                  mt_[:, 0:8, :], reads=[mtk], writes=[('mTd', tt, half)])

    wv2 = wbig[:].rearrange("p (k n) -> p k n", k=16)
    a.dma('pool', wv2[:, 0:8, :], w_out[0:1024, :].rearrange("(k p) n -> p k n", p=128), writes=['wbig'])
    a.dma('pool', wv2[:, 8:16, :], w_out[1024:2048, :].rearrange("(k p) n -> p k n", p=128), writes=['wbig'])

    def load_group(gi):
        a.dma('sp', G1[:], gt1[gi:gi + 1, :].broadcast_to([128, 2048]), writes=['G1'])
        a.dma('sp', A2[:], sc[gi:gi + 1, :].broadcast_to([128, 2048]), writes=['A2'])
        a.dma('sp', Sh2[:], sh[gi:gi + 1, :].broadcast_to([128, 2048]), writes=['Sh2'])
        a.op('dve', lambda e: e.scalar_tensor_tensor(out=A2[:], in0=A2[:], scalar=1.0, in1=gf[:], op0=ALU.add,
                                                     op1=ALU.mult), reads=['A2', 'gf'], writes=['A2'])

    for tt in range(NTT):
        if tt == 0:
            load_group(0)
        elif tt == g0_tiles:
            load_group(1)
        mt_, mtk = mT.next()
        a.dma('sp', mt_[:], mTd[:, tt * 128:(tt + 1) * 128].rearrange("(k p) t -> p k t", p=128),
              reads=[('mTd', tt, 0), ('mTd', tt, 1)], writes=[mtk])
        a.dma('act', S1[:], x[tt * 128:(tt + 1) * 128, :], writes=['S1'])
        for n0 in range(0, 2048, 512):
            p, pk = psm.next()
            for k in range(16):
                a.op('pe', lambda e, p=p, k=k, n0=n0, mt_=mt_: e.matmul(p[:, :], lhsT=mt_[:, k, :], rhs=wv2[:, k, n0:n0 + 512],
                                                                        start=(k == 0), stop=(k == 15)),
                     reads=[mtk, 'wbig'], writes=[pk])
            a.op('dve', lambda e, p=p, n0=n0: e.tensor_tensor(out=S2[:, n0:n0 + 512], in0=p[:, :], in1=G1[:, n0:n0 + 512],
                                                              op=ALU.mult), reads=[pk, 'G1'], writes=['S2'])
        a.op('pool', lambda e: e.tensor_tensor(out=S1[:], in0=S1[:], in1=S2[:], op=ALU.add), reads=['S1', 'S2'], writes=['S1'])
        a.dma('sp', xo[tt * 128:(tt + 1) * 128, :], S1[:], reads=['S1'], writes=[('xo', tt)])
        sm, smk = small.next()
        a.op('dve', lambda e, sm=sm: e.memset(sm[:], 0.0), writes=[smk])
        a.op('act', lambda e, sm=sm: e.activation(out=S4[:], in_=S1[:], func=AF.Square, accum_out=sm[:, 0:1]),
             reads=['S1', smk], writes=['S4', smk])
        a.op('act', lambda e, sm=sm: e.activation(out=sm[:, 1:2], in_=sm[:, 0:1], func=AF.Sqrt, bias=epsb[:, 0:1], scale=1.0 / 2048),
             reads=[smk, 'epsb'], writes=[smk])
        a.op('dve', lambda e, sm=sm: e.reciprocal(out=sm[:, 1:2], in_=sm[:, 1:2]), reads=[smk], writes=[smk])
        a.op('dve', lambda e, sm=sm: e.scalar_tensor_tensor(out=S3[:], in0=S1[:], scalar=sm[:, 1:2], in1=A2[:], op0=ALU.mult,
                                                            op1=ALU.mult), reads=['S1', smk, 'A2'], writes=['S3'])
        a.op('pool', lambda e: e.tensor_tensor(out=S3[:], in0=S3[:], in1=Sh2[:], op=ALU.add), reads=['S3', 'Sh2'], writes=['S3'])
        a.op('act', lambda e: e.copy(out=n2b[:], in_=S3[:]), reads=['S3'], writes=['n2b'])
        a.dma('act', n2o[tt * 128:(tt + 1) * 128, :], n2b[:], reads=['n2b'], writes=[('n2o', tt)])
        for q4 in range(4):
            p, pk = pst.next()
            for j in range(4):
                kc = q4 * 4 + j
                a.op('pe', lambda e, p=p, j=j, kc=kc: e.matmul(p[:, j * 128:(j + 1) * 128], lhsT=S3[:, kc * 128:(kc + 1) * 128],
                                                               rhs=idf[:], start=True, stop=True), reads=['S3', 'idf'], writes=[pk])
            a.op('dve' if q4 % 2 else 'act',
                 (lambda e, p=p, q4=q4: e.tensor_copy(out=n2T[:, q4 * 4:(q4 + 1) * 4, :], in_=p[:].rearrange("p (j t) -> p j t", j=4)))
                 if q4 % 2 else
                 (lambda e, p=p, q4=q4: e.copy(out=n2T[:, q4 * 4:(q4 + 1) * 4, :], in_=p[:].rearrange("p (j t) -> p j t", j=4))),
                 reads=[pk], writes=['n2T'])
        for k in range(16):
            a.op('pe', lambda e, k=k: e.matmul(psl[:, 0:32], lhsT=n2T[:, k, :], rhs=wr[:, k, :], start=(k == 0), stop=(k == 15)),
                 reads=['n2T', 'wr'], writes=['psl'])
        a.op('dve', lambda e: e.tensor_tensor(out=lg[:], in0=psl[:, 0:32], in1=br[:], op=ALU.add), reads=['psl', 'br'], writes=['lg'])
        sm, smk = small.next()
        a.op('dve', lambda e, sm=sm: e.max(out=sm[:, 0:8], in_=lg[:]), reads=['lg'], writes=[smk])
        a.op('dve', lambda e, sm=sm: e.tensor_scalar(out=sm[:, 8:9], in0=sm[:, 0:1], scalar1=-1.0, scalar2=None, op0=ALU.mult),
             reads=[smk], writes=[smk])
        a.op('act', lambda e, sm=sm: e.activation(out=ee[:], in_=lg[:], func=AF.Exp, bias=sm[:, 8:9], scale=1.0),
             reads=['lg', smk], writes=['ee'])
        a.op('dve', lambda e, sm=sm: e.tensor_scalar(out=lg[:], in0=lg[:], scalar1=sm[:, 3:4], scalar2=None, op0=ALU.is_ge),
             reads=['lg', smk], writes=['lg'])
        a.op('dve', lambda e: e.tensor_tensor(out=ee[:], in0=ee[:], in1=lg[:], op=ALU.mult), reads=['ee', 'lg'], writes=['ee'])
        a.op('dve', lambda e, sm=sm: e.tensor_reduce(out=sm[:, 9:10], in_=ee[:], axis=AX.X, op=ALU.add), reads=['ee'], writes=[smk])
        a.op('dve', lambda e, sm=sm: e.reciprocal(out=sm[:, 9:10], in_=sm[:, 9:10]), reads=[smk], writes=[smk])
        a.op('dve', lambda e, sm=sm: e.tensor_scalar(out=sm[:, 16:48], in0=ee[:], scalar1=sm[:, 9:10], scalar2=None, op0=ALU.mult),
             reads=['ee', smk], writes=[smk])
        a.dma('sp', rwo[tt * 128:(tt + 1) * 128, :], sm[:, 16:48], reads=[smk], writes=[('rwo', tt)])
    return c.done()


def build_D(NTOK, NE=4, TB=1024):
    c = Ctx()
    a = c.a
    n2T = c.din("n2T", [2048, NTOK], BF16)
    rw = c.din("rw", [NTOK, NE])
    w1 = c.din("w1", [NE, 2048, 4096])
    b1 = c.din("b1", [NE, 4096])
    w2 = c.din("w2", [NE, 2048, 2048])
    b2 = c.din("b2", [NE, 2048])
    ident = c.din("ident", [128, 128])
    yo = c.dout("y", [NTOK, 2048], BF16)
    NTT = TB // 128

    idf = c.sb("idf", [128, 128])
    idb = c.sb("idb", [128, 128], BF16)
    a.dma('sp', idf[:], ident, writes=['idf'])
    a.op('dve', lambda e: e.tensor_copy(out=idb[:], in_=idf[:]), reads=['idf'], writes=['idb'])
    nt = Rot([(c.sb(f"nt{i}", [128, 16, TB], BF16), f"nt{i}") for i in range(1 if TB > 512 else 2)])
    yacc = c.sb("yacc", [128, NTT, 2048])
    aT = c.sb("aT", [128, 16, TB], BF16)
    wt = Rot([(c.sb(f"wt{i}", [128, 16, 512], BF16), f"wt{i}") for i in range(2)])
    bt = Rot([(c.sb(f"bt{i}", [128, 512]), f"bt{i}") for i in range(2)])
    rwt = Rot([(c.sb(f"rwt{i}", [128, NTT, NE]), f"rwt{i}") for i in range(2)])
    hS = Rot([(c.sb(f"hS{i}", [128, 512]), f"hS{i}") for i in range(2)])
    hG = Rot([(c.sb(f"hG{i}", [128, 256]), f"hG{i}") for i in range(2)])
    hL = Rot([(c.sb(f"hL{i}", [128, 256]), f"hL{i}") for i in range(2)])
    hZ = Rot([(c.sb(f"hZ{i}", [128, 256]), f"hZ{i}") for i in range(2)])
    hA = Rot([(c.sb(f"hA{i}", [128, 256], BF16), f"hA{i}") for i in range(2)])
    stg = Rot([(c.sb(f"stg{i}", [128, 2048], BF16), f"stg{i}") for i in range(2)])
    psm = Rot([(c.ps(f"psm{i}"), f"psm{i}") for i in range(4)])
    pst = Rot([(c.ps(f"pst{i}"), f"pst{i}") for i in range(2)])

    for blk in range(NTOK // TB):
        t0 = blk * TB
        ntile, ntk = nt.next()
        a.dma('sp', ntile[:], n2T[:, t0:t0 + TB].rearrange("(k p) t -> p k t", p=128), writes=[ntk])
        rwtile, rwk = rwt.next()
        a.dma('sp', rwtile[:], rw[t0:t0 + TB, :].rearrange("(j p) e -> p j e", p=128), writes=[rwk])
        for ex in range(NE):
            for ct in range(8):
                wtile, wk = wt.next()
                a.dma('pool', wtile[:], w1[ex, :, ct * 512:(ct + 1) * 512].rearrange("(k p) n -> p k n", p=128), writes=[wk])
                btile, bk = bt.next()
                a.dma('act', btile[:], b1[ex:ex + 1, ct * 512:(ct + 1) * 512].broadcast_to([128, 512]), writes=[bk])
                for tt in range(NTT):
                    p, pk = psm.next()
                    for k in range(16):
                        a.op('pe', lambda e, p=p, k=k, tt=tt, ntile=ntile, wtile=wtile: e.matmul(
                            p[:, :], lhsT=ntile[:, k, tt * 128:(tt + 1) * 128], rhs=wtile[:, k, :], start=(k == 0), stop=(k == 15)),
                            reads=[ntk, wk], writes=[pk])
                    S, Sk = hS.next()
                    G, Gk = hG.next()
                    L, Lk = hL.next()
                    Z, Zk = hZ.next()
                    A_, Ak = hA.next()
                    a.op('dve', lambda e, p=p, S=S, btile=btile: e.tensor_tensor(out=S[:], in0=p[:, :], in1=btile[:], op=ALU.add),
                         reads=[pk, bk], writes=[Sk])
                    Sv = S[:].rearrange("p (n two) -> p n two", two=2)
                    a.op('dve', lambda e, Sv=Sv, G=G: e.tensor_scalar(out=G[:], in0=Sv[:, :, 0], scalar1=7.0, scalar2=None, op0=ALU.min),
                         reads=[Sk], writes=[Gk])
                    a.op('act', lambda e, G=G, Z=Z: e.activation(out=Z[:], in_=G[:], func=AF.Sigmoid, scale=1.702),
                         reads=[Gk], writes=[Zk])
                    a.op('dve', lambda e, Sv=Sv, L=L: e.tensor_scalar(out=L[:], in0=Sv[:, :, 1], scalar1=7.0, scalar2=-7.0, op0=ALU.min,
                                                                      op1=ALU.max), reads=[Sk], writes=[Lk])
                    a.op('dve', lambda e, L=L, G=G: e.scalar_tensor_tensor(out=L[:], in0=L[:], scalar=1.0, in1=G[:], op0=ALU.add,
                                                                           op1=ALU.mult), reads=[Lk, Gk], writes=[Lk])
                    a.op('dve', lambda e, L=L, Z=Z, A_=A_: e.tensor_tensor(out=A_[:], in0=L[:], in1=Z[:], op=ALU.mult),
                         reads=[Lk, Zk], writes=[Ak])
                    pt_, ptk = pst.next()
                    for j in range(2):
                        a.op('pe', lambda e, pt_=pt_, j=j, A_=A_: e.matmul(pt_[:, j * 128:(j + 1) * 128], lhsT=A_[:, j * 128:(j + 1) * 128],
                                                                            rhs=idb[:], start=True, stop=True), reads=[Ak, 'idb'], writes=[ptk])
                    a.op('act', lambda e, pt_=pt_, ct=ct, tt=tt: e.copy(out=aT[:, ct * 2:ct * 2 + 2, tt * 128:(tt + 1) * 128],
                                                                        in_=pt_[:, 0:256].rearrange("p (j t) -> p j t", j=2)),
                         reads=[ptk], writes=[('aT', tt)])
            for ct in range(4):
                wtile, wk = wt.next()
                a.dma('pool', wtile[:], w2[ex, :, ct * 512:(ct + 1) * 512].rearrange("(k p) n -> p k n", p=128), writes=[wk])
                btile, bk = bt.next()
                a.dma('act', btile[:], b2[ex:ex + 1, ct * 512:(ct + 1) * 512].broadcast_to([128, 512]), writes=[bk])
                for tt in range(NTT):
                    p, pk = psm.next()
                    for k in range(16):
                        a.op('pe', lambda e, p=p, k=k, tt=tt, wtile=wtile: e.matmul(
                            p[:, :], lhsT=aT[:, k, tt * 128:(tt + 1) * 128], rhs=wtile[:, k, :], start=(k == 0), stop=(k == 15)),
                            reads=[('aT', tt), wk], writes=[pk])
                    S, Sk = hS.next()
                    a.op('dve', lambda e, p=p, S=S, btile=btile: e.tensor_tensor(out=S[:], in0=p[:, :], in1=btile[:], op=ALU.add),
                         reads=[pk, bk], writes=[Sk])
                    ys = yacc[:, tt, ct * 512:(ct + 1) * 512]
                    if ex == 0:
                        a.op('dve', lambda e, S=S, ys=ys, tt=tt, rwtile=rwtile: e.tensor_scalar(
                            out=ys, in0=S[:], scalar1=rwtile[:, tt, 0:1], scalar2=None, op0=ALU.mult),
                            reads=[Sk, rwk], writes=[('yacc', tt, ct)])
                    else:
                        a.op('dve', lambda e, S=S, ys=ys, tt=tt, ex=ex, rwtile=rwtile: e.scalar_tensor_tensor(
                            out=ys, in0=S[:], scalar=rwtile[:, tt, ex:ex + 1], in1=ys, op0=ALU.mult, op1=ALU.add),
                            reads=[Sk, rwk, ('yacc', tt, ct)], writes=[('yacc', tt, ct)])
        for tt in range(NTT):
            st_, stk = stg.next()
            a.op('act', lambda e, st_=st_, tt=tt: e.copy(out=st_[:], in_=yacc[:, tt, :]), reads=[('yacc', tt, ct) for ct in range(4)],
                 writes=[stk])
            a.dma('sp', yo[t0 + tt * 128:t0 + (tt + 1) * 128, :], st_[:], reads=[stk], writes=[('yo', blk, tt)])
    return c.done()


def build_F(NT, g0_tiles, NP=8):
    c = Ctx()
    a = c.a
    x = c.din("x", [NT, 2048])
    parts = c.din("parts", [NP, NT, 2048], BF16)
    gt2 = c.din("gt2", [2, 2048])
    gfin = c.din("gfin", [1, 2048])
    xo = c.dout("xo", [NT, 2048])
    fo = c.dout("fo", [NT, 2048])
    epsb = c.sb("epsb", [128, 1])
    a.op('dve', lambda e: e.memset(epsb[:], EPS), writes=['epsb'])
    G2 = c.sb("G2", [128, 2048])
    GF = c.sb("GF", [128, 2048])
    a.dma('sp', GF[:], gfin.broadcast_to([128, 2048]), writes=['GF'])
    xt = Rot([(c.sb(f"xt{i}", [128, 2048]), f"xt{i}") for i in range(2)])
    pt = Rot([(c.sb(f"pt{i}", [128, NP, 2048], BF16), f"pt{i}") for i in range(2)])
    acc = c.sb("acc", [128, 2048])
    scr = c.sb("scr", [128, 2048])
    fo_t = Rot([(c.sb(f"fo{i}", [128, 2048]), f"fo{i}") for i in range(2)])
    small = Rot([(c.sb(f"sm{i}", [128, 4]), f"sm{i}") for i in range(2)])
    for tt in range(NT // 128):
        if tt == 0 or tt == g0_tiles:
            gi = 0 if tt == 0 else 1
            a.dma('sp', G2[:], gt2[gi:gi + 1, :].broadcast_to([128, 2048]), writes=['G2'])
        xtile, xk = xt.next()
        a.dma('sp', xtile[:], x[tt * 128:(tt + 1) * 128, :], writes=[xk])
        ptile, pk = pt.next()
        a.dma('act', ptile[:], parts[:, tt * 128:(tt + 1) * 128, :].rearrange("c t d -> t c d"), writes=[pk])
        a.op('dve', lambda e, ptile=ptile: e.tensor_tensor(out=acc[:], in0=ptile[:, 0, :], in1=ptile[:, 1, :], op=ALU.add),
             reads=[pk], writes=['acc'])
        for j in range(2, NP):
            a.op('dve', lambda e, ptile=ptile, j=j: e.tensor_tensor(out=acc[:], in0=acc[:], in1=ptile[:, j, :], op=ALU.add),
                 reads=[pk, 'acc'], writes=['acc'])
        a.op('pool', lambda e: e.tensor_tensor(out=acc[:], in0=acc[:], in1=G2[:], op=ALU.mult), reads=['acc', 'G2'], writes=['acc'])
        a.op('pool', lambda e, xtile=xtile: e.tensor_tensor(out=xtile[:], in0=xtile[:], in1=acc[:], op=ALU.add),
             reads=[xk, 'acc'], writes=[xk])
        a.dma('sp', xo[tt * 128:(tt + 1) * 128, :], xtile[:], reads=[xk], writes=[('xo', tt)])
        sm, smk = small.next()
        a.op('dve', lambda e, sm=sm: e.memset(sm[:], 0.0), writes=[smk])
        a.op('act', lambda e, sm=sm, xtile=xtile: e.activation(out=scr[:], in_=xtile[:], func=AF.Square, accum_out=sm[:, 0:1]),
             reads=[xk, smk], writes=['scr', smk])
        a.op('act', lambda e, sm=sm: e.activation(out=sm[:, 1:2], in_=sm[:, 0:1], func=AF.Sqrt, bias=epsb[:, 0:1], scale=1.0 / 2048),
             reads=[smk, 'epsb'], writes=[smk])
        a.op('dve', lambda e, sm=sm: e.reciprocal(out=sm[:, 1:2], in_=sm[:, 1:2]), reads=[smk], writes=[smk])
        ft, fk = fo_t.next()
        a.op('dve', lambda e, sm=sm, xtile=xtile, ft=ft: e.scalar_tensor_tensor(out=ft[:], in0=xtile[:], scalar=sm[:, 1:2], in1=GF[:],
                                                                                op0=ALU.mult, op1=ALU.mult),
             reads=[xk, smk, 'GF'], writes=[fk])
        a.dma('act', fo[tt * 128:(tt + 1) * 128, :], ft[:], reads=[fk], writes=[('fo', tt)])
    return c.done()


def _rope_table(pos_r, pos_c, dim):
    half = dim // 2
    fr = (10000.0 ** (-np.arange(0, half, 2, dtype=np.float32) / np.float32(half))).astype(np.float32)
    ar = pos_r[:, None].astype(np.float32) * fr
    ac = pos_c[:, None].astype(np.float32) * fr
    C = np.concatenate([np.cos(ar), np.cos(ar), np.cos(ac), np.cos(ac)], 1)
    S = np.concatenate([-np.sin(ar), np.sin(ar), -np.sin(ac), np.sin(ac)], 1)
    return np.ascontiguousarray(np.concatenate([C, S], 1).astype(np.float32))


_PROGS = {}


def _prog(name, fn):
    if name not in _PROGS:
        _PROGS[name] = fn()
    return _PROGS[name]


def kernel(x, c, ctx, c_ctx, w_mod, b_mod, g_mix, w_in, g_q_a, w_uq, g_kv_a, w_ukv, g_qn, g_kn,
           rpb, w_branch, w_out, g_ffn, w_router, b_router, w_exp1, b_exp1, w_exp2, b_exp2, g_final):
    f32 = np.float32
    x = np.asarray(x, f32)
    ctx = np.asarray(ctx, f32)
    B, S, D = x.shape
    NT = 2176
    ident = np.eye(128, dtype=f32)
    cores = [(b, h) for b in range(4) for h in range(2)]

    cT = np.zeros((2048, 8), f32)
    cT[:, 0:4] = np.asarray(c, f32).T
    cT[:, 4] = np.asarray(c_ctx, f32)
    w_all = np.concatenate([np.asarray(w_mod[0]), np.asarray(w_mod[1])], axis=1)
    b_all = np.concatenate([np.asarray(b_mod[0]), np.asarray(b_mod[1])], axis=0)[None, :]
    ncm = _prog('M', lambda: build_mod(3072))
    rm = run_spmd(ncm, [dict(cT=cT, w=np.ascontiguousarray(w_all[:, 3072 * k:3072 * (k + 1)]),
                             b=np.ascontiguousarray(b_all[:, 3072 * k:3072 * (k + 1)])) for k in range(8)])
    mod_all = np.concatenate([rm[k]["o"] for k in range(8)], axis=1)
    del w_all

    def grp(l, j, b):
        m = mod_all[:, l * 12288 + j * 2048: l * 12288 + (j + 1) * 2048]
        return np.ascontiguousarray(np.stack([m[4], m[b]], 0))

    xs = [np.ascontiguousarray(np.concatenate([ctx[b, 128 * h:128 * h + 128], x[b, 2048 * h:2048 * h + 2048]], 0)) for b, h in cores]
    ropeb, ropea = [], []
    for b, h in cores:
        t = np.arange(2048 * h, 2048 * h + 2048)
        pr = np.concatenate([np.zeros(128), t // 64]).astype(f32)
        pc = np.concatenate([np.zeros(128), t % 64]).astype(f32)
        ropeb.append(_rope_table(pr, pc, 128))
        ropea.append(_rope_table(pr, pc, 64))

    ncA = _prog('A', lambda: build_A(NT, 1))
    ncB = _prog('B', lambda: build_B())
    ncC = _prog('C', lambda: build_C(NT, 1))
    ncD = _prog('D', lambda: build_D(8 * NT))
    ncF = _prog('F', lambda: build_F(NT, 1))
    fo = None
    for l in range(2):
        row = lambda v: np.ascontiguousarray(np.asarray(v, f32)[None, :])
        w_in_l = np.ascontiguousarray(np.asarray(w_in[l], f32))
        w_uq_l = np.ascontiguousarray(np.asarray(w_uq[l], f32))
        w_ukv_l = np.ascontiguousarray(np.asarray(w_ukv[l], f32))
        ra = run_spmd(ncA, [dict(x=xs[k], g=row(g_mix[l]), sc=grp(l, 1, b), sh=grp(l, 0, b), w_in=w_in_l, g_q=row(g_q_a[l]),
                                 g_kv=row(g_kv_a[l]), w_uq=w_uq_l, w_ukv=w_ukv_l, g_qn=row(g_qn[l]), g_kn=row(g_kn[l]),
                                 ident=ident, ropeb=ropeb[k], ropea=ropea[k]) for k, (b, h) in enumerate(cores)])
        del w_in_l
        inB = []
        rpb_l = np.asarray(rpb[l], f32)
        for k, (b, h) in enumerate(cores):
            k0, k1 = 2 * b, 2 * b + 1

            def full(name):
                return np.concatenate([ra[k0][name][:128], ra[k1][name][:128], ra[k0][name][128:], ra[k1][name][128:]], 0)
            kva = full('kva').reshape(NK, 8, 256)
            lrows = np.arange(40) + 32 * h - 4
            ltok = np.concatenate([np.arange(256)] + [256 + r * 64 + np.arange(64) if 0 <= r < 64 else np.full(64, -1) for r in lrows])
            msk = ltok >= 0

            def takek(v):
                o = np.zeros((len(ltok),) + v.shape[1:], v.dtype)
                o[msk] = v[ltok[msk]]
                return o
            kc_f, vc_f = full('kc'), full('vc')
            inB.append(dict(
                qaT=np.ascontiguousarray(ra[k]['qa'].reshape(NQ, 8, 192).transpose(1, 2, 0)),
                kaT=np.ascontiguousarray(kva[:, :, :128].transpose(1, 2, 0)),
                kpeT=np.ascontiguousarray(full('kpe').T),
                va=np.ascontiguousarray(kva[:, :, 128:].reshape(NK, 1024)),
                qbT=np.ascontiguousarray(ra[k]['qb'].reshape(NQ, 8, 128).transpose(1, 2, 0)),
                kbT=np.ascontiguousarray(full('kb').reshape(NK, 2, 128).transpose(1, 2, 0)),
                vb=np.ascontiguousarray(full('vb')),
                qcT=np.ascontiguousarray(ra[k]['qc'].reshape(NQ, 8, 128).transpose(1, 2, 0)),
                kcT=np.ascontiguousarray(takek(kc_f).reshape(NKL, 8, 128).transpose(1, 2, 0)),
                vc=np.ascontiguousarray(takek(vc_f)),
                nab=na_bias_table(rpb_l, h)))
        rb = run_spmd(ncB, inB)
        del inB
        w_b_l = np.ascontiguousarray(np.asarray(w_branch[l], f32))
        w_o_l = np.ascontiguousarray(np.asarray(w_out[l], f32))
        rc = run_spmd(ncC, [dict(oT=np.ascontiguousarray(np.stack([rb[k]['oaT'], rb[k]['obT'], rb[k]['ocT']], 0)), gate=ra[k]['gate'],
                                 x=xs[k], w_branch=w_b_l, w_out=w_o_l, gt1=grp(l, 2, b), g=row(g_ffn[l]), sc=grp(l, 4, b),
                                 sh=grp(l, 3, b), w_r=np.ascontiguousarray(np.asarray(w_router[l], f32)), b_r=row(b_router[l]),
                                 ident=ident) for k, (b, h) in enumerate(cores)])
        del ra, rb
        xs = [rc[k]['xo'] for k in range(8)]
        n2T = np.ascontiguousarray(np.concatenate([rc[k]['n2'] for k in range(8)], 0).T)
        rw_all = np.concatenate([rc[k]['rw'] for k in range(8)], 0)
        del rc
        rd = run_spmd(ncD, [dict(n2T=n2T, rw=np.ascontiguousarray(rw_all[:, 4 * k:4 * k + 4]),
                                 w1=np.ascontiguousarray(np.asarray(w_exp1[l][4 * k:4 * k + 4], f32)),
                                 b1=np.ascontiguousarray(np.asarray(b_exp1[l][4 * k:4 * k + 4], f32)),
                                 w2=np.ascontiguousarray(np.asarray(w_exp2[l][4 * k:4 * k + 4], f32)),
                                 b2=np.ascontiguousarray(np.asarray(b_exp2[l][4 * k:4 * k + 4], f32)), ident=ident) for k in range(8)])
        del n2T
        rf = run_spmd(ncF, [dict(x=xs[k], parts=np.ascontiguousarray(np.stack([rd[cc]['y'][k * NT:(k + 1) * NT] for cc in range(8)], 0)),
                                 gt2=grp(l, 5, b), gfin=row(g_final)) for k, (b, h) in enumerate(cores)])
        del rd
        xs = [rf[k]['xo'] for k in range(8)]
        fo = [rf[k]['fo'] for k in range(8)]
    out = np.zeros((B, S, D), f32)
    for k, (b, h) in enumerate(cores):
        out[b, 2048 * h:2048 * h + 2048] = fo[k][128:]
    return out
```

```python
import contextlib
import numpy as np
import concourse.bass as bass
import concourse.mybir as mybir
from concourse.bass_utils import run_bass_kernel_spmd

F32 = mybir.dt.float32
BF16 = mybir.dt.bfloat16
ALU = mybir.AluOpType
AF = mybir.ActivationFunctionType
AX = mybir.AxisListType

NCORES = 8
SEM_G = 8192
NDMA = 40
NDMA_SW = 8


class AS:
    def __init__(self, nc, stack, prefix="", prev=None, pool=None):
        self.nc = nc
        self.stack = stack
        self.prefix = prefix
        self.prev = prev
        self.done_sem = None
        self.go_sem = None
        self.pool = pool if pool is not None else {'d': None, 'c': {}}
        self.streams = {k: [] for k in ('pe', 'act', 'dve', 'pool', 'sp')}
        self.count = {k: 0 for k in self.streams}
        self.waited = {k: {} for k in self.streams}
        self.last_w = {}
        self.readers = {}
        self.dma_i = 0
        self.dma_sw = 0
        self.csem = {k: [] for k in self.streams}
        if self.pool['d'] is None:
            self.pool['d'] = [stack.enter_context(nc.semaphore(f"dq{i}")) for i in range(NDMA + NDMA_SW)]
        self.dsem = self.pool['d']
        self.dma_final = {}

    def _deps(self, reads, writes):
        toks = {}

        def add(t):
            k = (t[0], t[1])
            if toks.get(k, 0) < t[2]:
                toks[k] = t[2]
        for b in reads:
            if b in self.last_w:
                add(self.last_w[b])
        for b in writes:
            if b in self.last_w:
                add(self.last_w[b])
            for k, v in self.readers.get(b, {}).items():
                add((k[0], k[1], v))
        return toks

    def _emit_waits(self, eng, toks):
        for k, val in toks.items():
            if k[0] == 'c' and k[1] == 'pe' and eng == 'pe':
                continue
            if self.waited[eng].get(k, 0) >= val:
                continue
            self.waited[eng][k] = val
            self.streams[eng].append(('wait', k, val))

    def _record(self, tok, reads, writes):
        k = (tok[0], tok[1])
        for b in reads:
            r = self.readers.setdefault(b, {})
            if r.get(k, 0) < tok[2]:
                r[k] = tok[2]
        for b in writes:
            self.last_w[b] = tok
            self.readers[b] = {}

    def op(self, eng, fn, reads=(), writes=()):
        toks = self._deps(reads, writes)
        self._emit_waits(eng, toks)
        self.count[eng] += 1
        idx = self.count[eng]
        self.streams[eng].append(('op', fn, idx))
        self._record(('c', eng, idx), reads, writes)

    def dma(self, eng, out, in_, reads=(), writes=(), **kw):
        if eng == 'pool':
            i = self.dma_sw
            self.dma_sw += 1
            s = NDMA + i % NDMA_SW
            prev = 16 * (i // NDMA_SW)
        else:
            i = self.dma_i
            self.dma_i += 1
            s = i % NDMA
            prev = 16 * (i // NDMA)
        toks = self._deps(reads, writes)
        if prev > 0:
            k = ('d', s)
            if toks.get(k, 0) < prev:
                toks[k] = prev
        self._emit_waits(eng, toks)
        self.streams[eng].append(('dma', out, in_, s, kw))
        self.dma_final[s] = prev + 16
        self._record(('d', s, prev + 16), reads, writes)

    def _sem_for(self, k, val):
        if k[0] == 'd':
            return self.dsem[k[1]], val
        eng = k[1]
        g = (val - 1) // SEM_G
        return self.csem[eng][g], (val - 1) % SEM_G + 1

    def finish(self):
        nc = self.nc
        for s, v in self.dma_final.items():
            self.streams['sp'].append(('wait', ('d', s), v))
        for eng in self.streams:
            if eng != 'sp' and self.count[eng] > 0:
                self.streams['sp'].append(('wait', ('c', eng), self.count[eng]))
        self.done_sem = self.stack.enter_context(nc.semaphore(f"{self.prefix}done"))
        self.streams['sp'].append(('done',))
        if self.prev is not None:
            self.go_sem = self.stack.enter_context(nc.semaphore(f"{self.prefix}go"))
        reuse = list(self.pool['d']) + [x for v in self.pool['c'].values() for x in v] if self.prev is not None else []
        for eng in self.streams:
            ng = max((self.count[eng] + SEM_G - 1) // SEM_G, 1)
            have = self.pool['c'].setdefault(eng, [])
            while len(have) < ng:
                have.append(self.stack.enter_context(nc.semaphore(f"c_{eng}{len(have)}")))
            self.csem[eng] = have
        engmap = {'pe': 'tensor', 'act': 'scalar', 'dve': 'vector', 'pool': 'gpsimd', 'sp': 'sync'}
        with nc.Block() as block:
            for eng, attr in engmap.items():
                stream = self.streams[eng]
                if not stream and self.prev is None:
                    continue

                def body(e, stream=stream, eng=eng):
                    if self.prev is not None:
                        e.wait_ge(self.prev, 1)
                        if eng == 'sp':
                            for sm in reuse:
                                e.sem_clear(sm)
                            e.nop().then_inc(self.go_sem, 1)
                        else:
                            e.wait_ge(self.go_sem, 1)
                    for it in stream:
                        if it[0] == 'done':
                            e.nop().then_inc(self.done_sem, 1)
                        elif it[0] == 'wait':
                            sem, val = self._sem_for(it[1], it[2])
                            e.wait_ge(sem, val)
                        elif it[0] == 'op':
                            idx = it[2]
                            sem = self.csem[eng][(idx - 1) // SEM_G]
                            it[1](e).then_inc(sem, 1)
                        else:
                            _, out, in_, s, kw = it
                            e.dma_start(out=out, in_=in_, **kw).then_inc(self.dsem[s], 16)
                getattr(block, attr)(body)


def new_nc():
    return bass.Bass("TRN2", target_bir_lowering=False)


def run_spmd(nc, in_maps):
    res = run_bass_kernel_spmd(nc, in_maps, core_ids=list(range(len(in_maps))))
    return res.results


def build_mod(ncol):
    nc = new_nc()
    cT = nc.dram_tensor("cT", [2048, 8], F32, kind="ExternalInput").ap()
    w = nc.dram_tensor("w", [2048, ncol], F32, kind="ExternalInput").ap()
    b = nc.dram_tensor("b", [1, ncol], F32, kind="ExternalInput").ap()
    o = nc.dram_tensor("o", [8, ncol], F32, kind="ExternalOutput").ap()
    NT = ncol // 512
    with contextlib.ExitStack() as st:
        a = AS(nc, st)
        sb = lambda name, shape, dt: st.enter_context(nc.sbuf_tensor(name, shape, dt))
        ct = sb("ct", [128, 16, 8], F32)
        cs = sb("cs", [128, 16, 8], F32)
        wt = [sb(f"wt{i}", [128, 16, 512], F32) for i in range(2)]
        bt = sb("bt", [8, ncol], F32)
        ot = sb("ot", [8, ncol], F32)
        ps = [st.enter_context(nc.psum_tensor(f"ps{i}", [128, 512], F32)) for i in range(2)]
        a.dma('sp', ct[:], cT.rearrange("(k p) c -> p k c", p=128), writes=['ct'])
        a.dma('sp', bt[:], b.broadcast_to([8, ncol]), writes=['bt'])
        a.op('act', lambda e: e.activation(out=cs[:], in_=ct[:], func=AF.Silu), reads=['ct'], writes=['cs'])
        for t in range(NT):
            wb = wt[t % 2]
            a.dma('sp' if t % 2 == 0 else 'act', wb[:], w[:, t * 512:(t + 1) * 512].rearrange("(k p) n -> p k n", p=128),
                  writes=[f'wt{t % 2}'])
            p = ps[t % 2]
            for k in range(16):
                a.op('pe', lambda e, k=k, p=p, wb=wb: e.matmul(p[0:8, :], lhsT=cs[:, k, :], rhs=wb[:, k, :],
                                                               start=(k == 0), stop=(k == 15)),
                     reads=['cs', f'wt{t % 2}'], writes=[f'ps{t % 2}'])
            a.op('dve', lambda e, p=p, t=t: e.tensor_tensor(out=ot[:, t * 512:(t + 1) * 512], in0=p[0:8, :],
                                                            in1=bt[:, t * 512:(t + 1) * 512], op=ALU.add),
                 reads=[f'ps{t % 2}', 'bt'], writes=['ot'])
        a.dma('sp', o, ot[:], reads=['ot'], writes=['o'])
        a.finish()
    return nc


class Prog:
    def __init__(self):
        self.nc = new_nc()
        self.semst = contextlib.ExitStack()
        self.prev = None
        self.nphase = 0
        self.pool = {'d': None, 'c': {}}


class Ctx:
    def __init__(self, prog=None, bind=None):
        self.prog = prog if prog is not None else Prog()
        self.standalone = prog is None
        self.nc = self.prog.nc
        self.st = contextlib.ExitStack()
        self.pfx = f"p{self.prog.nphase}_"
        self.prog.nphase += 1
        self.a = AS(self.nc, self.prog.semst, prefix=self.pfx, prev=self.prog.prev, pool=self.prog.pool)
        self.bind = bind or {}
        self.nps = 0

    def din(self, name, shape, dt=F32):
        if name in self.bind:
            return self.bind[name]
        return self.nc.dram_tensor(name, list(shape), dt, kind="ExternalInput").ap()

    def dout(self, name, shape, dt=F32):
        if name in self.bind:
            return self.bind[name]
        return self.nc.dram_tensor(name, list(shape), dt, kind="ExternalOutput").ap()

    def dscratch(self, name, shape, dt=F32):
        return self.nc.dram_tensor(self.pfx + name, list(shape), dt).ap()

    def sb(self, name, shape, dt=F32):
        return self.st.enter_context(self.nc.sbuf_tensor(self.pfx + name, list(shape), dt))

    def ps(self, name):
        self.nps += 1
        assert self.nps <= 8
        return self.st.enter_context(self.nc.psum_tensor(self.pfx + name, [128, 512], F32))

    def done(self):
        self.a.finish()
        self.prog.prev = self.a.done_sem
        self.st.close()
        if self.standalone:
            self.prog.semst.close()
        return self.nc


class Rot:
    def __init__(self, items):
        self.items = items
        self.i = 0

    def next(self):
        it = self.items[self.i % len(self.items)]
        self.i += 1
        return it


def rope_ops(a, eng, x, C, S, t1, t2, out, H, Wd, keys):
    kx, kC, kS, k1, k2, ko = keys
    hw = Wd // 4
    xv = x.rearrange("p (h a f w) -> p h a f w", h=H, a=2, f=2, w=hw)
    t2v = t2.rearrange("p (h a f w) -> p h a f w", h=H, a=2, f=2, w=hw)
    Sv = S.rearrange("p (a f w) -> p a f w", a=2, f=2, w=hw)
    x3 = x.rearrange("p (h d) -> p h d", h=H)
    t13 = t1.rearrange("p (h d) -> p h d", h=H)
    Cb = C.unsqueeze(1).broadcast_to([128, H, Wd])
    a.op(eng, lambda e: e.tensor_tensor(out=t13, in0=x3, in1=Cb, op=ALU.mult), reads=[kx, kC], writes=[k1])
    for f in range(2):
        Sb = Sv[:, :, f, :].unsqueeze(1).broadcast_to([128, H, 2, hw])
        a.op(eng, lambda e, f=f, Sb=Sb: e.tensor_tensor(out=t2v[:, :, :, f, :], in0=xv[:, :, :, 1 - f, :], in1=Sb,
                                                        op=ALU.mult), reads=[kx, kS], writes=[k2])
    a.op(eng, lambda e: e.tensor_tensor(out=out, in0=t1, in1=t2, op=ALU.add), reads=[k1, k2], writes=[ko])


A_COLS = [
    ('dq', 0, 512, 'dq'), ('dkv', 512, 512, 'dkv'), ('kr', 1024, 64, 'kr'),
    ('qb0', 1088, 512, 'qb'), ('qb1', 1600, 512, 'qb'), ('kb', 2112, 256, 'kb'), ('vb', 2368, 256, 'copy'),
    ('qc0', 2624, 512, 'copy'), ('qc1', 3136, 512, 'copy'), ('kc0', 3648, 512, 'copy'), ('kc1', 4160, 512, 'copy'),
    ('vc0', 4672, 512, 'copy'), ('vc1', 5184, 512, 'copy'),
] + [(f'gt{i}', 5696 + 512 * i, 512, 'sig') for i in range(12)]
A_OUT = {'qa': 1536, 'kva': 2048, 'kpe': 64, 'qb': 1024, 'kb': 256, 'vb': 256, 'qc': 1024, 'kc': 1024, 'vc': 1024,
         'gate': 6144}
A_DEST = {'vb': ('vb', 0), 'qc0': ('qc', 0), 'qc1': ('qc', 512), 'kc0': ('kc', 0), 'kc1': ('kc', 512),
          'vc0': ('vc', 0), 'vc1': ('vc', 512), 'qb0': ('qb', 0), 'qb1': ('qb', 512), 'kb': ('kb', 0), 'kr': ('kpe', 0)}
EPS = 1e-6


def build_A(NT, g0_tiles, ctx=None):
    c = ctx or Ctx()
    a = c.a
    nc = c.nc
    NTT = NT // 128
    x = c.din("x", [NT, 2048])
    g = c.din("g", [1, 2048])
    sc = c.din("sc", [2, 2048])
    sh = c.din("sh", [2, 2048])
    w_in = c.din("w_in", [2048, 11840])
    g_q = c.din("g_q", [1, 512])
    g_kv = c.din("g_kv", [1, 512])
    w_uq = c.din("w_uq", [512, 1536])
    w_ukv = c.din("w_ukv", [512, 2048])
    g_qn = c.din("g_qn", [1, 128])
    g_kn = c.din("g_kn", [1, 128])
    ident = c.din("ident", [128, 128])
    ropeb = c.din("ropeb", [NT, 256])
    ropea = c.din("ropea", [NT, 128])
    outs = {k: c.dout(k, [NT, w], BF16) for k, w in A_OUT.items()}

    idf = c.sb("idf", [128, 128])
    idb = c.sb("idb", [128, 128], BF16)
    At = c.sb("At", [128, 2048])
    St = c.sb("St", [128, 2048])
    gt = c.sb("gt", [128, 2048])
    gq = c.sb("gq", [128, 512])
    gkv = c.sb("gkv", [128, 512])
    gqn = c.sb("gqn", [128, 128])
    gkn = c.sb("gkn", [128, 128])
    nT = c.sb("nT", [128, 16, NT], BF16)
    wuq = c.sb("wuq", [128, 4, 1536], BF16)
    wukv = c.sb("wukv", [128, 4, 2048], BF16)
    xt = Rot([(c.sb(f"xt{i}", [128, 2048]), f"xt{i}") for i in range(1)])
    nb = Rot([(c.sb(f"nb{i}", [128, 2048], BF16), f"nb{i}") for i in range(1)])
    scr = c.sb("scr", [128, 2048])
    small = Rot([(c.sb(f"sm{i}", [128, 16]), f"sm{i}") for i in range(4)])
    wt = Rot([(c.sb(f"wt{i}", [128, 16, 512], BF16), f"wt{i}") for i in range(2)])
    ob = Rot([(c.sb(f"ob{i}", [128, 512], BF16), f"ob{i}") for i in range(3)])
    w1 = Rot([(c.sb(f"w1_{i}", [128, 1024]), f"w1_{i}") for i in range(2)])
    w2 = c.sb("w2", [128, 1024])
    w3 = c.sb("w3", [128, 1024])
    ynb = c.sb("ynb", [128, 512], BF16)
    ynT = c.sb("ynT", [128, 4, 128], BF16)
    qab = c.sb("qab", [128, 2048], BF16)
    rb = Rot([(c.sb(f"rb{i}", [128, 256]), f"rb{i}") for i in range(2)])
    ra = Rot([(c.sb(f"ra{i}", [128, 128]), f"ra{i}") for i in range(2)])
    pst = Rot([(c.ps(f"pst{i}"), f"pst{i}") for i in range(2)])
    psm = Rot([(c.ps(f"psm{i}"), f"psm{i}") for i in range(3)])
    psu = Rot([(c.ps(f"psu{i}"), f"psu{i}") for i in range(3)])

    epsb = c.sb("epsb", [128, 1])
    a.op('dve', lambda e: e.memset(epsb[:], EPS), writes=['epsb'])
    a.dma('sp', idf[:], ident, writes=['idf'])
    a.op('dve', lambda e: e.tensor_copy(out=idb[:], in_=idf[:]), reads=['idf'], writes=['idb'])
    a.dma('sp', gt[:], g.broadcast_to([128, 2048]), writes=['gt'])
    a.dma('sp', gq[:], g_q.broadcast_to([128, 512]), writes=['gq'])
    a.dma('sp', gkv[:], g_kv.broadcast_to([128, 512]), writes=['gkv'])
    a.dma('sp', gqn[:], g_qn.broadcast_to([128, 128]), writes=['gqn'])
    a.dma('sp', gkn[:], g_kn.broadcast_to([128, 128]), writes=['gkn'])
    a.dma('pool', wuq[:], w_uq.rearrange("(k p) n -> p k n", p=128), writes=['wuq'])
    a.dma('pool', wukv[:], w_ukv.rearrange("(k p) n -> p k n", p=128), writes=['wukv'])

    def load_group(gi):
        a.dma('sp', At[:], sc[gi:gi + 1, :].broadcast_to([128, 2048]), writes=['At'])
        a.dma('sp', St[:], sh[gi:gi + 1, :].broadcast_to([128, 2048]), writes=['St'])
        a.op('dve', lambda e: e.scalar_tensor_tensor(out=At[:], in0=At[:], scalar=1.0, in1=gt[:], op0=ALU.add,
                                                     op1=ALU.mult), reads=['At', 'gt'], writes=['At'])

    def rstd_from(ssap, sskey, n, outap, outkey):
        a.op('act', lambda e: e.activation(out=outap, in_=ssap, func=AF.Sqrt, bias=epsb[:, 0:1], scale=1.0 / n),
             reads=[sskey, 'epsb'], writes=[outkey])
        a.op('dve', lambda e: e.reciprocal(out=outap, in_=outap), reads=[outkey], writes=[outkey])

    for tt in range(NTT):
        if tt == 0:
            load_group(0)
        elif tt == g0_tiles:
            load_group(1)
        xtile, xk = xt.next()
        a.dma('sp', xtile[:], x[tt * 128:(tt + 1) * 128, :], writes=[xk])
        sm, smk = small.next()
        a.op('dve', lambda e, sm=sm: e.memset(sm[:], 0.0), writes=[smk])
        a.op('act', lambda e, xtile=xtile, sm=sm: e.activation(out=scr[:], in_=xtile[:], func=AF.Square,
                                                               accum_out=sm[:, 0:1]),
             reads=[xk, smk], writes=['scr', smk])
        rstd_from(sm[:, 0:1], smk, 2048, sm[:, 1:2], smk)
        a.op('dve', lambda e, xtile=xtile, sm=sm: e.scalar_tensor_tensor(out=xtile[:], in0=xtile[:], scalar=sm[:, 1:2],
                                                                         in1=At[:], op0=ALU.mult, op1=ALU.mult),
             reads=[xk, smk, 'At'], writes=[xk])
        nbt, nbk = nb.next()
        a.op('pool', lambda e, xtile=xtile, nbt=nbt: e.tensor_tensor(out=nbt[:], in0=xtile[:], in1=St[:], op=ALU.add),
             reads=[xk, 'St'], writes=[nbk])
        for q4 in range(4):
            p, pk = pst.next()
            for j in range(4):
                kc = q4 * 4 + j
                a.op('pe', lambda e, p=p, j=j, kc=kc, nbt=nbt: e.matmul(p[:, j * 128:(j + 1) * 128],
                                                                          lhsT=nbt[:, kc * 128:(kc + 1) * 128],
                                                                          rhs=idb[:], start=True, stop=True),
                     reads=[nbk, 'idb'], writes=[pk])
            eng = 'act' if q4 % 2 == 0 else 'dve'
            dst = nT[:, q4 * 4:(q4 + 1) * 4, tt * 128:(tt + 1) * 128]
            src = p[:].rearrange("p (j t) -> p j t", j=4)
            if eng == 'act':
                a.op('act', lambda e, dst=dst, src=src: e.copy(out=dst, in_=src), reads=[pk], writes=[('nT', tt)])
            else:
                a.op('dve', lambda e, dst=dst, src=src: e.tensor_copy(out=dst, in_=src), reads=[pk], writes=[('nT', tt)])

    def store(src_tile, src_key, name, col0, width, tt):
        a.dma('act', outs[name][tt * 128:(tt + 1) * 128, col0:col0 + width], src_tile[:, 0:width],
              reads=[src_key], writes=[('out', name, col0, tt)])

    def headnorm_rope(p, pk, cw, gtile, gkey, H, tt, name, col0):
        rbt, rbk = rb.next()
        a.dma('sp', rbt[:], ropeb[tt * 128:(tt + 1) * 128, :], writes=[rbk])
        xa, xak = w1.next()
        a.op('act', lambda e: e.copy(out=xa[:, 0:cw], in_=p[:, 0:cw]), reads=[pk], writes=[xak])
        a.op('pool', lambda e: e.tensor_tensor(out=w2[:, 0:cw], in0=xa[:, 0:cw], in1=xa[:, 0:cw], op=ALU.mult),
             reads=[xak], writes=['w2'])
        sm, smk = small.next()
        a.op('dve', lambda e: e.tensor_reduce(out=sm[:, 0:H], in_=w2[:, 0:cw].rearrange("p (h d) -> p h d", h=H),
                                              axis=AX.X, op=ALU.add), reads=['w2'], writes=[smk])
        rstd_from(sm[:, 0:H], smk, 128, sm[:, 8:8 + H], smk)
        x3 = xa[:, 0:cw].rearrange("p (h d) -> p h d", h=H)
        a.op('dve', lambda e: e.tensor_tensor(out=x3, in0=x3, in1=sm[:, 8:8 + H].unsqueeze(2).broadcast_to([128, H, 128]),
                                              op=ALU.mult), reads=[xak, smk], writes=[xak])
        a.op('dve', lambda e: e.tensor_tensor(out=x3, in0=x3, in1=gtile[:].unsqueeze(1).broadcast_to([128, H, 128]),
                                              op=ALU.mult), reads=[xak, gkey], writes=[xak])
        obt, obk = ob.next()
        rope_ops(a, 'dve', xa[:, 0:cw], rbt[:, 0:128], rbt[:, 128:256], w2[:, 0:cw], w3[:, 0:cw], obt[:, 0:cw], H, 128,
                 (xak, rbk, rbk, 'w2', 'w3', obk))
        store(obt, obk, name, col0, cw, tt)

    def upproj(p, pk, gtile, gkey, wres, wkey, nout, oname, tt, do_rope):
        sm, smk = small.next()
        a.op('dve', lambda e: e.memset(sm[:], 0.0), writes=[smk])
        a.op('act', lambda e: e.activation(out=w2[:, 0:512], in_=p[:, 0:512], func=AF.Square, accum_out=sm[:, 0:1]),
             reads=[pk, smk], writes=['w2', smk])
        rstd_from(sm[:, 0:1], smk, 512, sm[:, 1:2], smk)
        a.op('dve', lambda e: e.scalar_tensor_tensor(out=ynb[:], in0=p[:, 0:512], scalar=sm[:, 1:2], in1=gtile[:],
                                                     op0=ALU.mult, op1=ALU.mult), reads=[pk, smk, gkey], writes=['ynb'])
        pt, ptk = pst.next()
        for j in range(4):
            a.op('pe', lambda e, j=j: e.matmul(pt[:, j * 128:(j + 1) * 128], lhsT=ynb[:, j * 128:(j + 1) * 128],
                                               rhs=idb[:], start=True, stop=True), reads=['ynb', 'idb'], writes=[ptk])
        a.op('act', lambda e: e.copy(out=ynT[:], in_=pt[:].rearrange("p (j t) -> p j t", j=4)), reads=[ptk],
             writes=['ynT'])
        for n0 in range(0, nout, 512):
            pu, puk = psu.next()
            for k in range(4):
                a.op('pe', lambda e, k=k, n0=n0, pu=pu: e.matmul(pu[:, 0:512], lhsT=ynT[:, k, :], rhs=wres[:, k, n0:n0 + 512],
                                                          start=(k == 0), stop=(k == 3)), reads=['ynT', wkey], writes=[puk])
            if do_rope:
                a.op('act', lambda e, n0=n0, pu=pu: e.copy(out=w3[:, 0:512], in_=pu[:, 0:512]), reads=[puk], writes=['w3'])
                a.op('pool', lambda e, n0=n0: e.tensor_copy(out=scr[:, n0:n0 + 512], in_=w3[:, 0:512]), reads=['w3'],
                     writes=['scr'])
            else:
                eng = 'act' if (n0 // 512) % 2 == 0 else 'dve'
                if eng == 'act':
                    a.op('act', lambda e, n0=n0, pu=pu: e.copy(out=qab[:, n0:n0 + 512], in_=pu[:, 0:512]), reads=[puk],
                         writes=['qab'])
                else:
                    a.op('dve', lambda e, n0=n0, pu=pu: e.tensor_copy(out=qab[:, n0:n0 + 512], in_=pu[:, 0:512]), reads=[puk],
                         writes=['qab'])
        if do_rope:
            rat, rak = ra.next()
            a.dma('sp', rat[:], ropea[tt * 128:(tt + 1) * 128, :], writes=[rak])
            q3 = scr[:, 0:1536].rearrange("p (h d) -> p h d", h=8)
            a.op('dve', lambda e: e.tensor_copy(out=w2[:, 0:512].rearrange("p (h d) -> p h d", h=8), in_=q3[:, :, 128:192]),
                 reads=['scr'], writes=['w2'])
            xa, xak = w1.next()
            rope_ops(a, 'dve', w2[:, 0:512], rat[:, 0:64], rat[:, 64:128], w3[:, 0:512], w3[:, 512:1024], xa[:, 0:512], 8, 64,
                     ('w2', rak, rak, 'w3', 'w3', xak))
            qv = qab[:, 0:1536].rearrange("p (h d) -> p h d", h=8)
            a.op('act', lambda e: e.copy(out=qv[:, :, 0:128], in_=q3[:, :, 0:128]), reads=['scr'], writes=['qab'])
            a.op('dve', lambda e: e.tensor_copy(out=qv[:, :, 128:192], in_=xa[:, 0:512].rearrange("p (h d) -> p h d", h=8)),
                 reads=[xak], writes=['qab'])
        a.dma('act', outs[oname][tt * 128:(tt + 1) * 128, :], qab[:, 0:nout], reads=['qab'], writes=[('out', oname, tt)])

    for (cname, c0, cw, kind) in A_COLS:
        wtile, wk = wt.next()
        a.dma('pool', wtile[:, :, 0:cw], w_in[:, c0:c0 + cw].rearrange("(k p) n -> p k n", p=128), writes=[wk])
        for tt in range(NTT):
            p, pk = psm.next()
            for k in range(16):
                a.op('pe', lambda e, p=p, k=k, tt=tt, wtile=wtile: e.matmul(p[:, 0:cw], lhsT=nT[:, k, tt * 128:(tt + 1) * 128],
                                                                             rhs=wtile[:, k, 0:cw], start=(k == 0),
                                                                             stop=(k == 15)),
                     reads=[('nT', tt), wk], writes=[pk])
            if kind in ('copy', 'sig'):
                obt, obk = ob.next()
                if kind == 'sig':
                    a.op('act', lambda e, p=p, obt=obt: e.activation(out=obt[:, 0:cw], in_=p[:, 0:cw], func=AF.Sigmoid),
                         reads=[pk], writes=[obk])
                    store(obt, obk, 'gate', c0 - 5696, cw, tt)
                else:
                    a.op('dve', lambda e, p=p, obt=obt: e.tensor_copy(out=obt[:, 0:cw], in_=p[:, 0:cw]), reads=[pk],
                         writes=[obk])
                    dn, dc = A_DEST[cname]
                    store(obt, obk, dn, dc, cw, tt)
            elif kind == 'qb':
                dn, dc = A_DEST[cname]
                headnorm_rope(p, pk, cw, gqn, 'gqn', 4, tt, dn, dc)
            elif kind == 'kb':
                headnorm_rope(p, pk, cw, gkn, 'gkn', 2, tt, 'kb', 0)
            elif kind == 'kr':
                rat, rak = ra.next()
                a.dma('sp', rat[:], ropea[tt * 128:(tt + 1) * 128, :], writes=[rak])
                xa, xak = w1.next()
                a.op('act', lambda e, p=p, xa=xa: e.copy(out=xa[:, 0:64], in_=p[:, 0:64]), reads=[pk], writes=[xak])
                obt, obk = ob.next()
                rope_ops(a, 'dve', xa[:, 0:64], rat[:, 0:64], rat[:, 64:128], w2[:, 0:64], w3[:, 0:64], obt[:, 0:64], 1, 64,
                         (xak, rak, rak, 'w2', 'w3', obk))
                store(obt, obk, 'kpe', 0, 64, tt)
            elif kind == 'dq':
                upproj(p, pk, gq, 'gq', wuq, 'wuq', 1536, 'qa', tt, True)
            elif kind == 'dkv':
                upproj(p, pk, gkv, 'gkv', wukv, 'wukv', 2048, 'kva', tt, False)
    return c.done()


NQ = 2176
NK = 4352
NKL = 256 + 40 * 64
NA_SLOTS = 60


def na_row_chunks(i):
    if i < 4:
        cs, ce = i // 2, 5
    elif i >= 28:
        cs, ce = 14, (i + 7) // 2
    else:
        cs, ce = i // 2, (i + 7) // 2
    typ = i if i < 4 else (6 + i - 28 if i >= 28 else 4 + (i % 2))
    return typ, list(range(cs, ce + 1))


def build_B(nheads=8, nrows=32, do=('a', 'b', 'c'), ctx=None):
    c = ctx or Ctx()
    a = c.a
    qaT = c.din("qaT", [8, 192, NQ], BF16)
    kaT = c.din("kaT", [8, 128, NK], BF16)
    kpeT = c.din("kpeT", [64, NK], BF16)
    va = c.din("va", [NK, 1024], BF16)
    qbT = c.din("qbT", [8, 128, NQ], BF16)
    kbT = c.din("kbT", [2, 128, NK], BF16)
    vb = c.din("vb", [NK, 256], BF16)
    qcT = c.din("qcT", [8, 128, NQ], BF16)
    kcT = c.din("kcT", [8, 128, NKL], BF16)
    vc = c.din("vc", [NKL, 1024], BF16)
    nab = c.din("nab", [8, 128, NA_SLOTS * 64])
    outs = {k: c.dout(k, [1024, NQ], BF16) for k in ('oaT', 'obT', 'ocT')}

    ones = c.sb("ones", [128, 128], BF16)
    a.op('dve', lambda e: e.memset(ones[:], 1.0), writes=['ones'])
    kt = Rot([(c.sb(f"kt{i}", [128, NK], BF16), f"kt{i}") for i in range(2)])
    kpe = c.sb("kpe", [64, NK], BF16)
    ktb = c.sb("ktb", [128, NK], BF16)
    vtb = c.sb("vtb", [128, 34, 128], BF16)
    vt = Rot([(c.sb(f"vt{i}", [128, 34, 128], BF16), f"vt{i}") for i in range(2)])
    qt = Rot([(c.sb(f"qt{i}", [128, NQ], BF16), f"qt{i}") for i in range(2)])
    qr = Rot([(c.sb(f"qr{i}", [64, NQ], BF16), f"qr{i}") for i in range(2)])
    pt = Rot([(c.sb(f"pt{i}", [128, 512], BF16), f"pt{i}") for i in range(3)])
    ot = Rot([(c.sb(f"ot{i}", [128, NQ], BF16), f"ot{i}") for i in range(2)])
    rs = Rot([(c.sb(f"rs{i}", [128, 512]), f"rs{i}") for i in range(2)])
    accs = Rot([(c.sb(f"acc{i}", [128, 512]), f"acc{i}") for i in range(2)])
    accbs = Rot([(c.sb(f"accb{i}", [128, 512], BF16), f"accb{i}") for i in range(2)])
    ef = c.sb("ef", [128, NA_SLOTS * 64])
    eb = Rot([(c.sb(f"eb{i}", [128, NA_SLOTS * 64], BF16), f"eb{i}") for i in range(2)])
    pss = Rot([(c.ps(f"pss{i}"), f"pss{i}") for i in range(3)])
    pso = Rot([(c.ps(f"pso{i}"), f"pso{i}") for i in range(2)])
    psr = Rot([(c.ps(f"psr{i}"), f"psr{i}") for i in range(2)])
    a.dma('sp', kpe[:], kpeT, writes=['kpe'])

    units = []
    pre = []

    def add_unit(s1, s2):
        p = list(pre)
        pre.clear()

        def s1_all():
            for f in p:
                f()
            s1()
        units.append((s1_all, s2))

    def attend(qparts, kparts, vtile, vk, q0, n, kchunks, scale, otile, ok, etab=None, post=None):
        po, pok = pso.next()
        pr, prk = psr.next()
        nk = len(kchunks)

        def finish():
            rt, rk = rs.next()
            a.op('dve', lambda e: e.reciprocal(out=rt[:, 0:n], in_=pr[:, 0:n]), reads=[prk], writes=[rk])
            a.op('dve', lambda e: e.tensor_tensor(out=otile[:, q0:q0 + n], in0=po[:, 0:n], in1=rt[:, 0:n], op=ALU.mult),
                 reads=[pok, rk], writes=[ok])
            if post is not None:
                post()

        if etab is None:
            acc, acck = accs.next()
            accb, accbk = accbs.next()
            for ji, j in enumerate(kchunks):
                p, pk = pss.next()
                ptile, ptk = pt.next()

                def s1(p=p, pk=pk, ptile=ptile, ptk=ptk, j=j):
                    for pi, ((qtile, qk, nr), (ktile, kk, _)) in enumerate(zip(qparts, kparts)):
                        a.op('pe', lambda e, qtile=qtile, ktile=ktile, nr=nr, pi=pi: e.matmul(
                            p[:, 0:n], lhsT=ktile[0:nr, j * 128:(j + 1) * 128], rhs=qtile[0:nr, q0:q0 + n],
                            start=(pi == 0), stop=(pi == len(qparts) - 1)), reads=[qk, kk], writes=[pk])
                    a.op('act', lambda e: e.activation(out=ptile[:, 0:n], in_=p[:, 0:n], func=AF.Exp, scale=scale),
                         reads=[pk], writes=[ptk])

                def s2(ptile=ptile, ptk=ptk, j=j, ji=ji):
                    a.op('pe', lambda e: e.matmul(po[:, 0:n], lhsT=vtile[:, j, :], rhs=ptile[:, 0:n],
                                                  start=(ji == 0), stop=(ji == nk - 1)), reads=[vk, ptk], writes=[pok])
                    if ji == 0:
                        a.op('dve', lambda e: e.tensor_copy(out=acc[:, 0:n], in_=ptile[:, 0:n]), reads=[ptk], writes=[acck])
                    else:
                        a.op('dve', lambda e: e.tensor_tensor(out=acc[:, 0:n], in0=acc[:, 0:n], in1=ptile[:, 0:n], op=ALU.add),
                             reads=[ptk, acck], writes=[acck])
                    if ji == nk - 1:
                        a.op('dve', lambda e: e.tensor_copy(out=accb[:, 0:n], in_=acc[:, 0:n]), reads=[acck], writes=[accbk])
                        a.op('pe', lambda e: e.matmul(pr[:, 0:n], lhsT=ones[:], rhs=accb[:, 0:n], start=True, stop=True),
                             reads=['ones', accbk], writes=[prk])
                        finish()
                add_unit(s1, s2)
        else:
            etile, ek, slot0, nloc = etab
            p, pk = pss.next()
            ptile, ptk = pt.next()
            (qtile, qk, nr), (ktile, kk, _) = qparts[0], kparts[0]

            def s1():
                for ji, j in enumerate(kchunks):
                    a.op('pe', lambda e, j=j, ji=ji: e.matmul(p[:, ji * 64:(ji + 1) * 64], lhsT=ktile[0:nr, j * 128:(j + 1) * 128],
                                                              rhs=qtile[0:nr, q0:q0 + 64], start=True, stop=True),
                         reads=[qk, kk], writes=[pk])
                a.op('act', lambda e: e.activation(out=ptile[:, 0:nk * 64], in_=p[:, 0:nk * 64], func=AF.Exp, scale=scale),
                     reads=[pk], writes=[ptk])
                a.op('dve', lambda e: e.tensor_tensor(out=ptile[:, 0:nloc * 64], in0=ptile[:, 0:nloc * 64],
                                                      in1=etile[:, slot0 * 64:(slot0 + nloc) * 64], op=ALU.mult),
                     reads=[ptk, ek], writes=[ptk])

            def s2():
                for ji, j in enumerate(kchunks):
                    a.op('pe', lambda e, j=j, ji=ji: e.matmul(po[:, 0:64], lhsT=vtile[:, j, :], rhs=ptile[:, ji * 64:(ji + 1) * 64],
                                                              start=(ji == 0), stop=(ji == nk - 1)), reads=[vk, ptk], writes=[pok])
                for ji, j in enumerate(kchunks):
                    a.op('pe', lambda e, ji=ji: e.matmul(pr[:, 0:64], lhsT=ones[:], rhs=ptile[:, ji * 64:(ji + 1) * 64],
                                                         start=(ji == 0), stop=(ji == nk - 1)), reads=['ones', ptk], writes=[prk])
                finish()
            add_unit(s1, s2)

    def D(eng, out, in_, **kw):
        pre.append(lambda: a.dma(eng, out, in_, **kw))

    ALLK = list(range(34))
    for h in range(nheads):
        if 'a' in do:
            ktile, kk = kt.next()
            D('sp', ktile[:], kaT[h], writes=[kk])
            vtile, vk = vt.next()
            D('act', vtile[:], va[:, h * 128:(h + 1) * 128].rearrange("(c p) d -> p c d", p=128), writes=[vk])
            qtile, qk = qt.next()
            D('sp', qtile[:], qaT[h, 0:128, :], writes=[qk])
            qrt, qrk = qr.next()
            D('sp', qrt[:], qaT[h, 128:192, :], writes=[qrk])
            otile, ok = ot.next()
            qp = [(qtile, qk, 128), (qrt, qrk, 64)]
            kp = [(ktile, kk, 128), (kpe, 'kpe', 64)]
            attend(qp, kp, vtile, vk, 0, 128, [0, 1], 192 ** -0.5, otile, ok)
            for t in range(4):
                attend(qp, kp, vtile, vk, 128 + 512 * t, 512, ALLK, 192 ** -0.5, otile, ok,
                       post=(lambda otile=otile, ok=ok, h=h: a.dma('act', outs['oaT'][h * 128:(h + 1) * 128, :], otile[:], reads=[ok],
                                                                    writes=[('oa', h)])) if t == 3 else None)
        if 'b' in do:
            if h % 4 == 0:
                kbtile, kbk = ktb, 'ktb'
                D('sp', kbtile[:], kbT[h // 4], writes=[kbk])
                vbtile, vbk = vtb, 'vtb'
                D('act', vbtile[:], vb[:, (h // 4) * 128:(h // 4 + 1) * 128].rearrange("(c p) d -> p c d", p=128),
                      writes=[vbk])
            qtile, qk = qt.next()
            D('sp', qtile[:], qbT[h], writes=[qk])
            otile, ok = ot.next()
            qp = [(qtile, qk, 128)]
            kp = [(kbtile, kbk, 128)]
            attend(qp, kp, vbtile, vbk, 0, 128, [0, 1], 128 ** -0.5, otile, ok)
            for t in range(4):
                attend(qp, kp, vbtile, vbk, 128 + 512 * t, 512, ALLK, 128 ** -0.5, otile, ok,
                       post=(lambda otile=otile, ok=ok, h=h: a.dma('act', outs['obT'][h * 128:(h + 1) * 128, :], otile[:], reads=[ok],
                                                                    writes=[('ob', h)])) if t == 3 else None)
        if 'c' in do:
            ktile, kk = kt.next()
            D('sp', ktile[:, 0:NKL], kcT[h], writes=[kk])
            vtile, vk = vt.next()
            D('act', vtile[:, 0:22, :], vc[:, h * 128:(h + 1) * 128].rearrange("(c p) d -> p c d", p=128), writes=[vk])
            qtile, qk = qt.next()
            D('sp', qtile[:], qcT[h], writes=[qk])
            D('sp', ef[:], nab[h], writes=['ef'])
            etile, ek = eb.next()
            pre.append(lambda etile=etile, ek=ek: a.op('act', lambda e: e.activation(out=etile[:], in_=ef[:], func=AF.Exp), reads=['ef'], writes=[ek]))
            otile, ok = ot.next()
            qp = [(qtile, qk, 128)]
            kp = [(ktile, kk, 128)]
            attend(qp, kp, vtile, vk, 0, 128, [0, 1], 128 ** -0.5, otile, ok)
            for i in range(nrows):
                typ, chunks = na_row_chunks(i)
                kch = [2 + cc for cc in chunks] + [0, 1]
                attend(qp, kp, vtile, vk, 128 + 64 * i, 64, kch, 128 ** -0.5, otile, ok,
                       etab=(etile, ek, typ * 6, len(chunks)),
                       post=(lambda otile=otile, ok=ok, h=h: a.dma('act', outs['ocT'][h * 128:(h + 1) * 128, :], otile[:], reads=[ok],
                                                                    writes=[('oc', h)])) if i == nrows - 1 else None)
    for i, (s1, s2) in enumerate(units):
        s1()
        if i > 0:
            units[i - 1][1]()
    units[-1][1]()
    return c.done()


def na_bias_table(rpb_l, half):
    tab = np.full((8, NA_SLOTS, 128, 64), -30000.0, np.float32)
    rep = {0: 0, 1: 1, 2: 2, 3: 3, 4: 4, 5: 5, 6: 28, 7: 29, 8: 30, 9: 31}
    cq = np.arange(64)
    c0 = np.clip(cq - 8, 0, 48)
    ck = np.arange(64)
    colvalid = (ck[:, None] >= c0[None, :]) & (ck[:, None] < c0[None, :] + 16)
    dc = np.clip(ck[:, None] - cq[None, :] + 15, 0, 30)
    for typ, i in rep.items():
        _, chunks = na_row_chunks(i)
        r = 32 * half + i
        r0 = min(max(r - 4, 0), 56)
        for j, cc in enumerate(chunks):
            for rl in range(2):
                rk = 2 * cc + rl + 32 * half - 4
                if rk < 0 or rk >= 64 or rk < r0 or rk >= r0 + 8:
                    continue
                vals = rpb_l[:, rk - r + 7][:, dc]
                blk = tab[:, typ * 6 + j, rl * 64:(rl + 1) * 64, :]
                blk[:, colvalid] = vals[:, colvalid]
    return np.ascontiguousarray(tab.transpose(0, 2, 1, 3).reshape(8, 128, NA_SLOTS * 64))


def build_C(NT, g0_tiles, ctx=None):
    c = ctx or Ctx()
    a = c.a
    NTT = NT // 128
    oT = c.din("oT", [3, 1024, NT], BF16)
    gate = c.din("gate", [NT, 6144], BF16)
    x = c.din("x", [NT, 2048])
    w_branch = c.din("w_branch", [3, 1024, 2048])
    w_out = c.din("w_out", [2048, 2048])
    gt1 = c.din("gt1", [2, 2048])
    g = c.din("g", [1, 2048])
    sc = c.din("sc", [2, 2048])
    sh = c.din("sh", [2, 2048])
    w_r = c.din("w_r", [2048, 32])
    b_r = c.din("b_r", [1, 32])
    ident = c.din("ident", [128, 128])
    xo = c.dout("xo", [NT, 2048])
    n2o = c.dout("n2", [NT, 2048], BF16)
    rwo = c.dout("rw", [NT, 32])
    mTd = c.dscratch("mTd", [2048, NT], BF16)

    idf = c.sb("idf", [128, 128])
    idb = c.sb("idb", [128, 128], BF16)
    epsb = c.sb("epsb", [128, 1])
    wbig = c.sb("wbig", [128, 16 * 2048], BF16)
    gtile = Rot([(c.sb(f"gtl{i}", [128, 3, 1024], BF16), f"gtl{i}") for i in range(2)])
    otile = Rot([(c.sb(f"otl{i}", [128, 24, 128], BF16), f"otl{i}") for i in range(2)])
    S1 = c.sb("S1", [128, 2048])
    S2 = c.sb("S2", [128, 2048])
    S3 = c.sb("S3", [128, 2048])
    S4 = c.sb("S4", [128, 2048])
    mb = c.sb("mb", [128, 1024], BF16)
    mT = Rot([(c.sb(f"mT{i}", [128, 16, 128], BF16), f"mT{i}") for i in range(2)])
    G1 = c.sb("G1", [128, 2048])
    A2 = c.sb("A2", [128, 2048])
    Sh2 = c.sb("Sh2", [128, 2048])
    gf = c.sb("gf", [128, 2048])
    n2b = c.sb("n2b", [128, 2048], BF16)
    n2T = c.sb("n2T", [128, 16, 128])
    wr = c.sb("wr", [128, 16, 32])
    br = c.sb("br", [128, 32])
    small = Rot([(c.sb(f"sm{i}", [128, 48]), f"sm{i}") for i in range(3)])
    lg = c.sb("lg", [128, 32])
    ee = c.sb("ee", [128, 32])
    psm = Rot([(c.ps(f"psm{i}"), f"psm{i}") for i in range(4)])
    pst = Rot([(c.ps(f"pst{i}"), f"pst{i}") for i in range(2)])
    psl = c.ps("psl")

    a.op('dve', lambda e: e.memset(epsb[:], EPS), writes=['epsb'])
    a.dma('sp', idf[:], ident, writes=['idf'])
    a.op('dve', lambda e: e.tensor_copy(out=idb[:], in_=idf[:]), reads=['idf'], writes=['idb'])
    a.dma('sp', gf[:], g.broadcast_to([128, 2048]), writes=['gf'])
    a.dma('sp', wr[:], w_r.rearrange("(k p) n -> p k n", p=128), writes=['wr'])
    a.dma('sp', br[:], b_r.broadcast_to([128, 32]), writes=['br'])

    for half in range(2):
        wv = wbig[:, 0:24 * 1024].rearrange("p (k n) -> p k n", k=24)
        for i in range(3):
            a.dma('pool', wv[:, i * 8:(i + 1) * 8, :],
                  w_branch[i, :, half * 1024:(half + 1) * 1024].rearrange("(k p) n -> p k n", p=128), writes=['wbig'])
        for tt in range(NTT):
            ot_, otk = otile.next()
            a.dma('sp', ot_[:].rearrange("p (i k) t -> p i k t", i=3),
                  oT[:, :, tt * 128:(tt + 1) * 128].rearrange("i (k p) t -> p i k t", p=128), writes=[otk])
            gt_, gtk = gtile.next()
            a.dma('act', gt_[:], gate[tt * 128:(tt + 1) * 128, :].rearrange("t (i n) -> t i n", i=3)[:, :, half * 1024:(half + 1) * 1024],
                  writes=[gtk])
            for n0 in range(0, 1024, 512):
                for i in range(3):
                    p, pk = psm.next()
                    for k in range(8):
                        a.op('pe', lambda e, p=p, k=k, i=i, n0=n0, ot_=ot_: e.matmul(p[:, :], lhsT=ot_[:, i * 8 + k, :],
                                                                                     rhs=wv[:, i * 8 + k, n0:n0 + 512],
                                                                                     start=(k == 0), stop=(k == 7)),
                             reads=[otk, 'wbig'], writes=[pk])
                    if i == 0:
                        a.op('dve', lambda e, p=p, n0=n0, gt_=gt_: e.tensor_tensor(out=S1[:, n0:n0 + 512], in0=p[:, :],
                                                                                   in1=gt_[:, 0, n0:n0 + 512], op=ALU.mult),
                             reads=[pk, gtk], writes=['S1'])
                    else:
                        a.op('dve', lambda e, p=p, n0=n0, i=i, gt_=gt_: e.tensor_tensor(out=S2[:, n0:n0 + 512], in0=p[:, :],
                                                                                        in1=gt_[:, i, n0:n0 + 512], op=ALU.mult),
                             reads=[pk, gtk], writes=['S2'])
                        if i == 1:
                            a.op('pool', lambda e, n0=n0: e.tensor_tensor(out=S1[:, n0:n0 + 512], in0=S1[:, n0:n0 + 512],
                                                                          in1=S2[:, n0:n0 + 512], op=ALU.add),
                                 reads=['S1', 'S2'], writes=['S1'])
                        else:
                            a.op('pool', lambda e, n0=n0: e.tensor_tensor(out=mb[:, n0:n0 + 512], in0=S1[:, n0:n0 + 512],
                                                                          in1=S2[:, n0:n0 + 512], op=ALU.add),
                                 reads=['S1', 'S2'], writes=['mb'])
            mt_, mtk = mT.next()
            for q4 in range(2):
                p, pk = pst.next()
                for j in range(4):
                    kc = q4 * 4 + j
                    a.op('pe', lambda e, p=p, j=j, kc=kc: e.matmul(p[:, j * 128:(j + 1) * 128], lhsT=mb[:, kc * 128:(kc + 1) * 128],
                                                                   rhs=idb[:], start=True, stop=True), reads=['mb', 'idb'], writes=[pk])
                a.op('act', lambda e, p=p, q4=q4, mt_=mt_: e.copy(out=mt_[:, q4 * 4:(q4 + 1) * 4, :],
                                                                  in_=p[:].rearrange("p (j t) -> p j t", j=4)), reads=[pk], writes=[mtk])
            a.dma('sp', mTd[half * 1024:(half + 1) * 1024, tt * 128:(tt + 1) * 128].rearrange("(k p) t -> p k t", p=128),
                  mt_[:, 0:8, :], reads=[mtk], writes=[('mTd', tt, half)])

    wv2 = wbig[:].rearrange("p (k n) -> p k n", k=16)
    a.dma('pool', wv2[:, 0:8, :], w_out[0:1024, :].rearrange("(k p) n -> p k n", p=128), writes=['wbig'])
    a.dma('pool', wv2[:, 8:16, :], w_out[1024:2048, :].rearrange("(k p) n -> p k n", p=128), writes=['wbig'])

    def load_group(gi):
        a.dma('sp', G1[:], gt1[gi:gi + 1, :].broadcast_to([128, 2048]), writes=['G1'])
        a.dma('sp', A2[:], sc[gi:gi + 1, :].broadcast_to([128, 2048]), writes=['A2'])
        a.dma('sp', Sh2[:], sh[gi:gi + 1, :].broadcast_to([128, 2048]), writes=['Sh2'])
        a.op('dve', lambda e: e.scalar_tensor_tensor(out=A2[:], in0=A2[:], scalar=1.0, in1=gf[:], op0=ALU.add,
                                                     op1=ALU.mult), reads=['A2', 'gf'], writes=['A2'])

    for tt in range(NTT):
        if tt == 0:
            load_group(0)
        elif tt == g0_tiles:
            load_group(1)
        mt_, mtk = mT.next()
        a.dma('sp', mt_[:], mTd[:, tt * 128:(tt + 1) * 128].rearrange("(k p) t -> p k t", p=128),
              reads=[('mTd', tt, 0), ('mTd', tt, 1)], writes=[mtk])
        a.dma('act', S1[:], x[tt * 128:(tt + 1) * 128, :], writes=['S1'])
        for n0 in range(0, 2048, 512):
            p, pk = psm.next()
            for k in range(16):
                a.op('pe', lambda e, p=p, k=k, n0=n0, mt_=mt_: e.matmul(p[:, :], lhsT=mt_[:, k, :], rhs=wv2[:, k, n0:n0 + 512],
                                                                        start=(k == 0), stop=(k == 15)),
                     reads=[mtk, 'wbig'], writes=[pk])
            a.op('dve', lambda e, p=p, n0=n0: e.tensor_tensor(out=S2[:, n0:n0 + 512], in0=p[:, :], in1=G1[:, n0:n0 + 512],
                                                              op=ALU.mult), reads=[pk, 'G1'], writes=['S2'])
        a.op('pool', lambda e: e.tensor_tensor(out=S1[:], in0=S1[:], in1=S2[:], op=ALU.add), reads=['S1', 'S2'], writes=['S1'])
        a.dma('sp', xo[tt * 128:(tt + 1) * 128, :], S1[:], reads=['S1'], writes=[('xo', tt)])
        sm, smk = small.next()
        a.op('dve', lambda e, sm=sm: e.memset(sm[:], 0.0), writes=[smk])
        a.op('act', lambda e, sm=sm: e.activation(out=S4[:], in_=S1[:], func=AF.Square, accum_out=sm[:, 0:1]),
             reads=['S1', smk], writes=['S4', smk])
        a.op('act', lambda e, sm=sm: e.activation(out=sm[:, 1:2], in_=sm[:, 0:1], func=AF.Sqrt, bias=epsb[:, 0:1], scale=1.0 / 2048),
             reads=[smk, 'epsb'], writes=[smk])
        a.op('dve', lambda e, sm=sm: e.reciprocal(out=sm[:, 1:2], in_=sm[:, 1:2]), reads=[smk], writes=[smk])
        a.op('dve', lambda e, sm=sm: e.scalar_tensor_tensor(out=S3[:], in0=S1[:], scalar=sm[:, 1:2], in1=A2[:], op0=ALU.mult,
                                                            op1=ALU.mult), reads=['S1', smk, 'A2'], writes=['S3'])
        a.op('pool', lambda e: e.tensor_tensor(out=S3[:], in0=S3[:], in1=Sh2[:], op=ALU.add), reads=['S3', 'Sh2'], writes=['S3'])
        a.op('act', lambda e: e.copy(out=n2b[:], in_=S3[:]), reads=['S3'], writes=['n2b'])
        a.dma('act', n2o[tt * 128:(tt + 1) * 128, :], n2b[:], reads=['n2b'], writes=[('n2o', tt)])
        for q4 in range(4):
            p, pk = pst.next()
            for j in range(4):
                kc = q4 * 4 + j
                a.op('pe', lambda e, p=p, j=j, kc=kc: e.matmul(p[:, j * 128:(j + 1) * 128], lhsT=S3[:, kc * 128:(kc + 1) * 128],
                                                               rhs=idf[:], start=True, stop=True), reads=['S3', 'idf'], writes=[pk])
            a.op('dve' if q4 % 2 else 'act',
                 (lambda e, p=p, q4=q4: e.tensor_copy(out=n2T[:, q4 * 4:(q4 + 1) * 4, :], in_=p[:].rearrange("p (j t) -> p j t", j=4)))
                 if q4 % 2 else
                 (lambda e, p=p, q4=q4: e.copy(out=n2T[:, q4 * 4:(q4 + 1) * 4, :], in_=p[:].rearrange("p (j t) -> p j t", j=4))),
                 reads=[pk], writes=['n2T'])
        for k in range(16):
            a.op('pe', lambda e, k=k: e.matmul(psl[:, 0:32], lhsT=n2T[:, k, :], rhs=wr[:, k, :], start=(k == 0), stop=(k == 15)),
                 reads=['n2T', 'wr'], writes=['psl'])
        a.op('dve', lambda e: e.tensor_tensor(out=lg[:], in0=psl[:, 0:32], in1=br[:], op=ALU.add), reads=['psl', 'br'], writes=['lg'])
        sm, smk = small.next()
        a.op('dve', lambda e, sm=sm: e.max(out=sm[:, 0:8], in_=lg[:]), reads=['lg'], writes=[smk])
        a.op('dve', lambda e, sm=sm: e.tensor_scalar(out=sm[:, 8:9], in0=sm[:, 0:1], scalar1=-1.0, scalar2=None, op0=ALU.mult),
             reads=[smk], writes=[smk])
        a.op('act', lambda e, sm=sm: e.activation(out=ee[:], in_=lg[:], func=AF.Exp, bias=sm[:, 8:9], scale=1.0),
             reads=['lg', smk], writes=['ee'])
        a.op('dve', lambda e, sm=sm: e.tensor_scalar(out=lg[:], in0=lg[:], scalar1=sm[:, 3:4], scalar2=None, op0=ALU.is_ge),
             reads=['lg', smk], writes=['lg'])
        a.op('dve', lambda e: e.tensor_tensor(out=ee[:], in0=ee[:], in1=lg[:], op=ALU.mult), reads=['ee', 'lg'], writes=['ee'])
        a.op('dve', lambda e, sm=sm: e.tensor_reduce(out=sm[:, 9:10], in_=ee[:], axis=AX.X, op=ALU.add), reads=['ee'], writes=[smk])
        a.op('dve', lambda e, sm=sm: e.reciprocal(out=sm[:, 9:10], in_=sm[:, 9:10]), reads=[smk], writes=[smk])
        a.op('dve', lambda e, sm=sm: e.tensor_scalar(out=sm[:, 16:48], in0=ee[:], scalar1=sm[:, 9:10], scalar2=None, op0=ALU.mult),
             reads=['ee', smk], writes=[smk])
        a.dma('sp', rwo[tt * 128:(tt + 1) * 128, :], sm[:, 16:48], reads=[smk], writes=[('rwo', tt)])
    return c.done()


def build_D(NTOK, NE=4, TB=1024):
    c = Ctx()
    a = c.a
    n2T = c.din("n2T", [2048, NTOK], BF16)
    rw = c.din("rw", [NTOK, NE])
    w1 = c.din("w1", [NE, 2048, 4096])
    b1 = c.din("b1", [NE, 4096])
    w2 = c.din("w2", [NE, 2048, 2048])
    b2 = c.din("b2", [NE, 2048])
    ident = c.din("ident", [128, 128])
    yo = c.dout("y", [NTOK, 2048], BF16)
    NTT = TB // 128

    idf = c.sb("idf", [128, 128])
    idb = c.sb("idb", [128, 128], BF16)
    a.dma('sp', idf[:], ident, writes=['idf'])
    a.op('dve', lambda e: e.tensor_copy(out=idb[:], in_=idf[:]), reads=['idf'], writes=['idb'])
    nt = Rot([(c.sb(f"nt{i}", [128, 16, TB], BF16), f"nt{i}") for i in range(1 if TB > 512 else 2)])
    yacc = c.sb("yacc", [128, NTT, 2048])
    aT = c.sb("aT", [128, 16, TB], BF16)
    wt = Rot([(c.sb(f"wt{i}", [128, 16, 512], BF16), f"wt{i}") for i in range(2)])
    bt = Rot([(c.sb(f"bt{i}", [128, 512]), f"bt{i}") for i in range(2)])
    rwt = Rot([(c.sb(f"rwt{i}", [128, NTT, NE]), f"rwt{i}") for i in range(2)])
    hS = Rot([(c.sb(f"hS{i}", [128, 512]), f"hS{i}") for i in range(2)])
    hG = Rot([(c.sb(f"hG{i}", [128, 256]), f"hG{i}") for i in range(2)])
    hL = Rot([(c.sb(f"hL{i}", [128, 256]), f"hL{i}") for i in range(2)])
    hZ = Rot([(c.sb(f"hZ{i}", [128, 256]), f"hZ{i}") for i in range(2)])
    hA = Rot([(c.sb(f"hA{i}", [128, 256], BF16), f"hA{i}") for i in range(2)])
    stg = Rot([(c.sb(f"stg{i}", [128, 2048], BF16), f"stg{i}") for i in range(2)])
    psm = Rot([(c.ps(f"psm{i}"), f"psm{i}") for i in range(4)])
    pst = Rot([(c.ps(f"pst{i}"), f"pst{i}") for i in range(2)])

    for blk in range(NTOK // TB):
        t0 = blk * TB
        ntile, ntk = nt.next()
        a.dma('sp', ntile[:], n2T[:, t0:t0 + TB].rearrange("(k p) t -> p k t", p=128), writes=[ntk])
        rwtile, rwk = rwt.next()
        a.dma('sp', rwtile[:], rw[t0:t0 + TB, :].rearrange("(j p) e -> p j e", p=128), writes=[rwk])
        for ex in range(NE):
            for ct in range(8):
                wtile, wk = wt.next()
                a.dma('pool', wtile[:], w1[ex, :, ct * 512:(ct + 1) * 512].rearrange("(k p) n -> p k n", p=128), writes=[wk])
                btile, bk = bt.next()
                a.dma('act', btile[:], b1[ex:ex + 1, ct * 512:(ct + 1) * 512].broadcast_to([128, 512]), writes=[bk])
                for tt in range(NTT):
                    p, pk = psm.next()
                    for k in range(16):
                        a.op('pe', lambda e, p=p, k=k, tt=tt, ntile=ntile, wtile=wtile: e.matmul(
                            p[:, :], lhsT=ntile[:, k, tt * 128:(tt + 1) * 128], rhs=wtile[:, k, :], start=(k == 0), stop=(k == 15)),
                            reads=[ntk, wk], writes=[pk])
                    S, Sk = hS.next()
                    G, Gk = hG.next()
                    L, Lk = hL.next()
                    Z, Zk = hZ.next()
                    A_, Ak = hA.next()
                    a.op('dve', lambda e, p=p, S=S, btile=btile: e.tensor_tensor(out=S[:], in0=p[:, :], in1=btile[:], op=ALU.add),
                         reads=[pk, bk], writes=[Sk])
                    Sv = S[:].rearrange("p (n two) -> p n two", two=2)
                    a.op('dve', lambda e, Sv=Sv, G=G: e.tensor_scalar(out=G[:], in0=Sv[:, :, 0], scalar1=7.0, scalar2=None, op0=ALU.min),
                         reads=[Sk], writes=[Gk])
                    a.op('act', lambda e, G=G, Z=Z: e.activation(out=Z[:], in_=G[:], func=AF.Sigmoid, scale=1.702),
                         reads=[Gk], writes=[Zk])
                    a.op('dve', lambda e, Sv=Sv, L=L: e.tensor_scalar(out=L[:], in0=Sv[:, :, 1], scalar1=7.0, scalar2=-7.0, op0=ALU.min,
                                                                      op1=ALU.max), reads=[Sk], writes=[Lk])
                    a.op('dve', lambda e, L=L, G=G: e.scalar_tensor_tensor(out=L[:], in0=L[:], scalar=1.0, in1=G[:], op0=ALU.add,
                                                                           op1=ALU.mult), reads=[Lk, Gk], writes=[Lk])
                    a.op('dve', lambda e, L=L, Z=Z, A_=A_: e.tensor_tensor(out=A_[:], in0=L[:], in1=Z[:], op=ALU.mult),
                         reads=[Lk, Zk], writes=[Ak])
                    pt_, ptk = pst.next()
                    for j in range(2):
                        a.op('pe', lambda e, pt_=pt_, j=j, A_=A_: e.matmul(pt_[:, j * 128:(j + 1) * 128], lhsT=A_[:, j * 128:(j + 1) * 128],
                                                                            rhs=idb[:], start=True, stop=True), reads=[Ak, 'idb'], writes=[ptk])
                    a.op('act', lambda e, pt_=pt_, ct=ct, tt=tt: e.copy(out=aT[:, ct * 2:ct * 2 + 2, tt * 128:(tt + 1) * 128],
                                                                        in_=pt_[:, 0:256].rearrange("p (j t) -> p j t", j=2)),
                         reads=[ptk], writes=[('aT', tt)])
            for ct in range(4):
                wtile, wk = wt.next()
                a.dma('pool', wtile[:], w2[ex, :, ct * 512:(ct + 1) * 512].rearrange("(k p) n -> p k n", p=128), writes=[wk])
                btile, bk = bt.next()
                a.dma('act', btile[:], b2[ex:ex + 1, ct * 512:(ct + 1) * 512].broadcast_to([128, 512]), writes=[bk])
                for tt in range(NTT):
                    p, pk = psm.next()
                    for k in range(16):
                        a.op('pe', lambda e, p=p, k=k, tt=tt, wtile=wtile: e.matmul(
                            p[:, :], lhsT=aT[:, k, tt * 128:(tt + 1) * 128], rhs=wtile[:, k, :], start=(k == 0), stop=(k == 15)),
                            reads=[('aT', tt), wk], writes=[pk])
                    S, Sk = hS.next()
                    a.op('dve', lambda e, p=p, S=S, btile=btile: e.tensor_tensor(out=S[:], in0=p[:, :], in1=btile[:], op=ALU.add),
                         reads=[pk, bk], writes=[Sk])
                    ys = yacc[:, tt, ct * 512:(ct + 1) * 512]
                    if ex == 0:
                        a.op('dve', lambda e, S=S, ys=ys, tt=tt, rwtile=rwtile: e.tensor_scalar(
                            out=ys, in0=S[:], scalar1=rwtile[:, tt, 0:1], scalar2=None, op0=ALU.mult),
                            reads=[Sk, rwk], writes=[('yacc', tt, ct)])
                    else:
                        a.op('dve', lambda e, S=S, ys=ys, tt=tt, ex=ex, rwtile=rwtile: e.scalar_tensor_tensor(
                            out=ys, in0=S[:], scalar=rwtile[:, tt, ex:ex + 1], in1=ys, op0=ALU.mult, op1=ALU.add),
                            reads=[Sk, rwk, ('yacc', tt, ct)], writes=[('yacc', tt, ct)])
        for tt in range(NTT):
            st_, stk = stg.next()
            a.op('act', lambda e, st_=st_, tt=tt: e.copy(out=st_[:], in_=yacc[:, tt, :]), reads=[('yacc', tt, ct) for ct in range(4)],
                 writes=[stk])
            a.dma('sp', yo[t0 + tt * 128:t0 + (tt + 1) * 128, :], st_[:], reads=[stk], writes=[('yo', blk, tt)])
    return c.done()


def build_F(NT, g0_tiles, NP=8, ctx=None):
    c = ctx or Ctx()
    a = c.a
    x = c.din("x", [NT, 2048])
    parts = c.din("parts", [NP, NT, 2048], BF16)
    gt2 = c.din("gt2", [2, 2048])
    gfin = c.din("gfin", [1, 2048])
    xo = c.dout("xo", [NT, 2048])
    fo = c.dout("fo", [NT, 2048])
    epsb = c.sb("epsb", [128, 1])
    a.op('dve', lambda e: e.memset(epsb[:], EPS), writes=['epsb'])
    G2 = c.sb("G2", [128, 2048])
    GF = c.sb("GF", [128, 2048])
    a.dma('sp', GF[:], gfin.broadcast_to([128, 2048]), writes=['GF'])
    xt = Rot([(c.sb(f"xt{i}", [128, 2048]), f"xt{i}") for i in range(2)])
    pt = Rot([(c.sb(f"pt{i}", [128, NP, 2048], BF16), f"pt{i}") for i in range(2)])
    acc = c.sb("acc", [128, 2048])
    scr = c.sb("scr", [128, 2048])
    fo_t = Rot([(c.sb(f"fo{i}", [128, 2048]), f"fo{i}") for i in range(2)])
    small = Rot([(c.sb(f"sm{i}", [128, 4]), f"sm{i}") for i in range(2)])
    for tt in range(NT // 128):
        if tt == 0 or tt == g0_tiles:
            gi = 0 if tt == 0 else 1
            a.dma('sp', G2[:], gt2[gi:gi + 1, :].broadcast_to([128, 2048]), writes=['G2'])
        xtile, xk = xt.next()
        a.dma('sp', xtile[:], x[tt * 128:(tt + 1) * 128, :], writes=[xk])
        ptile, pk = pt.next()
        a.dma('act', ptile[:], parts[:, tt * 128:(tt + 1) * 128, :].rearrange("c t d -> t c d"), writes=[pk])
        a.op('dve', lambda e, ptile=ptile: e.tensor_tensor(out=acc[:], in0=ptile[:, 0, :], in1=ptile[:, 1, :], op=ALU.add),
             reads=[pk], writes=['acc'])
        for j in range(2, NP):
            a.op('dve', lambda e, ptile=ptile, j=j: e.tensor_tensor(out=acc[:], in0=acc[:], in1=ptile[:, j, :], op=ALU.add),
                 reads=[pk, 'acc'], writes=['acc'])
        a.op('pool', lambda e: e.tensor_tensor(out=acc[:], in0=acc[:], in1=G2[:], op=ALU.mult), reads=['acc', 'G2'], writes=['acc'])
        a.op('pool', lambda e, xtile=xtile: e.tensor_tensor(out=xtile[:], in0=xtile[:], in1=acc[:], op=ALU.add),
             reads=[xk, 'acc'], writes=[xk])
        a.dma('sp', xo[tt * 128:(tt + 1) * 128, :], xtile[:], reads=[xk], writes=[('xo', tt)])
        sm, smk = small.next()
        a.op('dve', lambda e, sm=sm: e.memset(sm[:], 0.0), writes=[smk])
        a.op('act', lambda e, sm=sm, xtile=xtile: e.activation(out=scr[:], in_=xtile[:], func=AF.Square, accum_out=sm[:, 0:1]),
             reads=[xk, smk], writes=['scr', smk])
        a.op('act', lambda e, sm=sm: e.activation(out=sm[:, 1:2], in_=sm[:, 0:1], func=AF.Sqrt, bias=epsb[:, 0:1], scale=1.0 / 2048),
             reads=[smk, 'epsb'], writes=[smk])
        a.op('dve', lambda e, sm=sm: e.reciprocal(out=sm[:, 1:2], in_=sm[:, 1:2]), reads=[smk], writes=[smk])
        ft, fk = fo_t.next()
        a.op('dve', lambda e, sm=sm, xtile=xtile, ft=ft: e.scalar_tensor_tensor(out=ft[:], in0=xtile[:], scalar=sm[:, 1:2], in1=GF[:],
                                                                                op0=ALU.mult, op1=ALU.mult),
             reads=[xk, smk, 'GF'], writes=[fk])
        a.dma('act', fo[tt * 128:(tt + 1) * 128, :], ft[:], reads=[fk], writes=[('fo', tt)])
    return c.done()


CAP = 320
CHUNKS = [(0, 128), (128, 128), (256, 64)]


def build_D2(NTOK, NE=4, GPB=4):
    c = Ctx()
    a = c.a
    NTILE = NTOK // 128
    NG = NTILE // 8
    assert NG * 8 == NTILE
    n2 = c.din("n2", [NTOK, 2048], BF16)
    rw = c.din("rw", [NTOK, NE])
    w1 = c.din("w1", [NE, 2048, 4096])
    b1 = c.din("b1", [NE, 4096])
    w2 = c.din("w2", [NE, 2048, 2048])
    b2 = c.din("b2", [NE, 2048])
    ident = c.din("ident", [128, 128])
    utri = c.din("utri", [128, 128])
    iota_d = c.din("iota", [128, CAP])
    yo = c.dout("y", [NTOK, 2048], BF16)
    y2d = c.dscratch("y2d", [NE, NG * CAP, 2048], BF16)
    NCH = len(CHUNKS)
    MAXSL = GPB * CAP

    idf = c.sb("idf", [128, 128])
    idb = c.sb("idb", [128, 128], BF16)
    ub = c.sb("ub", [128, 128], BF16)
    onesb = c.sb("onesb", [128, 128], BF16)
    iot = c.sb("iot", [128, CAP])
    a.dma('sp', idf[:], ident, writes=['idf'])
    a.op('dve', lambda e: e.tensor_copy(out=idb[:], in_=idf[:]), reads=['idf'], writes=['idb'])
    a.dma('sp', idf[:], utri, reads=['idf'], writes=['idf'])
    a.op('dve', lambda e: e.tensor_copy(out=ub[:], in_=idf[:]), reads=['idf'], writes=['ub'])
    a.op('dve', lambda e: e.memset(onesb[:], 1.0), writes=['onesb'])
    a.dma('sp', iot[:], iota_d, writes=['iot'])
    rwt = c.sb("rwt", [128, NTILE, NE])
    mt = c.sb("mt", [128, NTILE, NE])
    mbf = c.sb("mbf", [128, NTILE, NE], BF16)
    gpos = c.sb("gpos", [128, NTILE, NE])
    XT = c.sb("XT", [128, 16, MAXSL], BF16)
    aT = c.sb("aT", [128, max(16 * MAXSL, NE * NCH * 2048)], BF16)
    aTv = aT[:, 0:16 * MAXSL].rearrange("p (k s) -> p k s", k=16)
    Y2v = aT[:, 0:NE * NCH * 2048].rearrange("p (q d) -> p q d", d=2048)
    n2g = c.sb("n2g", [128, 8, 2048], BF16)
    selg = c.sb("selg", [128, 8, CAP], BF16)
    wt = Rot([(c.sb(f"wt{i}", [128, 16, 512], BF16), f"wt{i}") for i in range(2)])
    bt = Rot([(c.sb(f"bt{i}", [128, 512]), f"bt{i}") for i in range(2)])
    hS = Rot([(c.sb(f"hS{i}", [128, 512]), f"hS{i}") for i in range(2)])
    hG = Rot([(c.sb(f"hG{i}", [128, 256]), f"hG{i}") for i in range(2)])
    hL = Rot([(c.sb(f"hL{i}", [128, 256]), f"hL{i}") for i in range(2)])
    hZ = Rot([(c.sb(f"hZ{i}", [128, 256]), f"hZ{i}") for i in range(2)])
    hA = Rot([(c.sb(f"hA{i}", [128, 256], BF16), f"hA{i}") for i in range(2)])
    y2s = Rot([(c.sb(f"y2s{i}", [128, 512], BF16), f"y2s{i}") for i in range(3)])
    selw = Rot([(c.sb(f"selw{i}", [128, CAP], BF16), f"selw{i}") for i in range(2)])
    selT = Rot([(c.sb(f"selT{i}", [128, NE, NCH, 128], BF16), f"selT{i}") for i in range(2)])
    stg = Rot([(c.sb(f"stg{i}", [128, 2048], BF16), f"stg{i}") for i in range(1)])
    psm = Rot([(c.ps(f"psm{i}"), f"psm{i}") for i in range(4)])
    pst = Rot([(c.ps(f"pst{i}"), f"pst{i}") for i in range(2)])

    a.dma('sp', rwt[:], rw.rearrange("(j p) e -> p j e", p=128), writes=['rwt'])
    a.op('dve', lambda e: e.tensor_scalar(out=mt[:], in0=rwt[:], scalar1=0.0, scalar2=None, op0=ALU.is_gt), reads=['rwt'], writes=['mt'])
    a.op('dve', lambda e: e.tensor_copy(out=mbf[:], in_=mt[:]), reads=['mt'], writes=['mbf'])
    for g in range(NG):
        p, pk = pst.next()
        for j in range(8):
            for i in range(j + 1):
                a.op('pe', lambda e, p=p, j=j, i=i, g=g: e.matmul(p[:, j * NE:(j + 1) * NE], lhsT=(ub[:] if i == j else onesb[:]),
                                                                 rhs=mbf[:, g * 8 + i, :], start=(i == 0), stop=(i == j)),
                     reads=['ub', 'onesb', 'mbf'], writes=[pk])
        a.op('act', lambda e, p=p, g=g: e.copy(out=gpos[:, g * 8:(g + 1) * 8, :], in_=p[:, 0:8 * NE].rearrange("p (j e) -> p j e", j=8)),
             reads=[pk], writes=['gpos'])

    nblk = -(-NG // GPB)
    sizes = [NG // nblk + (1 if i < NG % nblk else 0) for i in range(nblk)]
    blocks, b0 = [], 0
    for sz in sizes:
        blocks.append(list(range(b0, b0 + sz)))
        b0 += sz
    for groups in blocks:
        nsl = len(groups) * CAP
        stiles = [(s0, min(128, nsl - s0)) for s0 in range(0, nsl, 128)]
        for ex in range(NE):
            for gi, g in enumerate(groups):
                a.dma('sp', n2g[:], n2[g * 1024:(g + 1) * 1024, :].rearrange("(j p) d -> p j d", p=128), writes=['n2g'])
                for j in range(8):
                    tj = g * 8 + j
                    a.op('dve', lambda e, j=j, tj=tj, ex=ex: e.tensor_scalar(out=selg[:, j, :], in0=iot[:], scalar1=gpos[:, tj, ex:ex + 1],
                                                                             scalar2=mt[:, tj, ex:ex + 1], op0=ALU.is_equal, op1=ALU.mult),
                         reads=['iot', 'gpos', 'mt'], writes=[('selg', j)])
                for dcg in range(4):
                    ps4 = [psm.next() for _ in range(4)]
                    for j in range(8):
                        for q in range(4):
                            p, pk = ps4[q]
                            dc = dcg * 4 + q
                            a.op('pe', lambda e, p=p, j=j, dc=dc: e.matmul(p[:, 0:CAP], lhsT=n2g[:, j, dc * 128:(dc + 1) * 128], rhs=selg[:, j, :],
                                                                          start=(j == 0), stop=(j == 7)),
                                 reads=['n2g', ('selg', j)], writes=[pk])
                    for q in range(4):
                        p, pk = ps4[q]
                        dc = dcg * 4 + q
                        dst = XT[:, dc, gi * CAP:(gi + 1) * CAP]
                        if q % 2 == 0:
                            a.op('act', lambda e, p=p, dst=dst: e.copy(out=dst, in_=p[:, 0:CAP]), reads=[pk], writes=[('XT', gi)])
                        else:
                            a.op('dve', lambda e, p=p, dst=dst: e.tensor_copy(out=dst, in_=p[:, 0:CAP]), reads=[pk], writes=[('XT', gi)])
            for ct in range(8):
                wtile, wk = wt.next()
                a.dma('pool', wtile[:], w1[ex, :, ct * 512:(ct + 1) * 512].rearrange("(k p) n -> p k n", p=128), writes=[wk])
                btile, bk = bt.next()
                a.dma('act', btile[:], b1[ex:ex + 1, ct * 512:(ct + 1) * 512].broadcast_to([128, 512]), writes=[bk])
                for (s0, sr) in stiles:
                    xkeys = [('XT', gq) for gq in range(s0 // CAP, (s0 + sr - 1) // CAP + 1)]
                    p, pk = psm.next()
                    for k in range(16):
                        a.op('pe', lambda e, p=p, k=k, s0=s0, sr=sr, wtile=wtile: e.matmul(
                            p[0:sr, :], lhsT=XT[:, k, s0:s0 + sr], rhs=wtile[:, k, :], start=(k == 0), stop=(k == 15)),
                            reads=xkeys + [wk], writes=[pk])
                    S, Sk = hS.next()
                    G, Gk = hG.next()
                    L, Lk = hL.next()
                    Z, Zk = hZ.next()
                    A_, Ak = hA.next()
                    a.op('dve', lambda e, p=p, S=S, btile=btile, sr=sr: e.tensor_tensor(out=S[0:sr, :], in0=p[0:sr, :], in1=btile[0:sr, :], op=ALU.add),
                         reads=[pk, bk], writes=[Sk])
                    Sv = S[0:sr, :].rearrange("p (n two) -> p n two", two=2)
                    a.op('dve', lambda e, Sv=Sv, G=G, sr=sr: e.tensor_scalar(out=G[0:sr, :], in0=Sv[:, :, 0], scalar1=7.0, scalar2=None, op0=ALU.min),
                         reads=[Sk], writes=[Gk])
                    a.op('act', lambda e, G=G, Z=Z, sr=sr: e.activation(out=Z[0:sr, :], in_=G[0:sr, :], func=AF.Sigmoid, scale=1.702),
                         reads=[Gk], writes=[Zk])
                    a.op('dve', lambda e, Sv=Sv, L=L, sr=sr: e.tensor_scalar(out=L[0:sr, :], in0=Sv[:, :, 1], scalar1=7.0, scalar2=-7.0, op0=ALU.min,
                                                                      op1=ALU.max), reads=[Sk], writes=[Lk])
                    a.op('dve', lambda e, L=L, G=G, sr=sr: e.scalar_tensor_tensor(out=L[0:sr, :], in0=L[0:sr, :], scalar=1.0, in1=G[0:sr, :], op0=ALU.add,
                                                                           op1=ALU.mult), reads=[Lk, Gk], writes=[Lk])
                    a.op('dve', lambda e, L=L, Z=Z, A_=A_, sr=sr: e.tensor_tensor(out=A_[0:sr, :], in0=L[0:sr, :], in1=Z[0:sr, :], op=ALU.mult),
                         reads=[Lk, Zk], writes=[Ak])
                    pt_, ptk = pst.next()
                    for j in range(2):
                        a.op('pe', lambda e, pt_=pt_, j=j, A_=A_, sr=sr: e.matmul(pt_[:, j * 128:j * 128 + sr], lhsT=A_[0:sr, j * 128:(j + 1) * 128],
                                                                                   rhs=idb[0:sr, 0:sr], start=True, stop=True), reads=[Ak, 'idb'], writes=[ptk])
                    a.op('act', lambda e, pt_=pt_, ct=ct, s0=s0, sr=sr: e.copy(out=aTv[:, ct * 2:ct * 2 + 2, s0:s0 + sr],
                                                                               in_=pt_[:, 0:256].rearrange("p (j t) -> p j t", j=2)[:, :, 0:sr]),
                         reads=[ptk], writes=['aT'])
            for ct in range(4):
                wtile, wk = wt.next()
                a.dma('pool', wtile[:], w2[ex, :, ct * 512:(ct + 1) * 512].rearrange("(k p) n -> p k n", p=128), writes=[wk])
                btile, bk = bt.next()
                a.dma('act', btile[:], b2[ex:ex + 1, ct * 512:(ct + 1) * 512].broadcast_to([128, 512]), writes=[bk])
                for (s0, sr) in stiles:
                    p, pk = psm.next()
                    for k in range(16):
                        a.op('pe', lambda e, p=p, k=k, s0=s0, sr=sr, wtile=wtile: e.matmul(
                            p[0:sr, :], lhsT=aTv[:, k, s0:s0 + sr], rhs=wtile[:, k, :], start=(k == 0), stop=(k == 15)),
                            reads=['aT', wk], writes=[pk])
                    ys, ysk = y2s.next()
                    a.op('dve', lambda e, p=p, ys=ys, btile=btile, sr=sr: e.tensor_tensor(out=ys[0:sr, :], in0=p[0:sr, :], in1=btile[0:sr, :], op=ALU.add),
                         reads=[pk, bk], writes=[ysk])
                    srow = groups[0] * CAP + s0
                    a.dma('sp', y2d[ex, srow:srow + sr, ct * 512:(ct + 1) * 512], ys[0:sr, :], reads=[ysk],
                          writes=[('y2d', ex, gq) for gq in range(srow // CAP, (srow + sr - 1) // CAP + 1)])
        for gi, g in enumerate(groups):
            for ex in range(NE):
                for cc, (u0, ur) in enumerate(CHUNKS):
                    a.dma('sp', Y2v[0:ur, ex * NCH + cc, :], y2d[ex, g * CAP + u0:g * CAP + u0 + ur, :],
                          reads=[('y2d', ex, g)], writes=['aT'])
            for j in range(8):
                tj = g * 8 + j
                sT, sTk = selT.next()
                for ex in range(NE):
                    sw, swk = selw.next()
                    a.op('dve', lambda e, sw=sw, tj=tj, ex=ex: e.tensor_scalar(out=sw[:], in0=iot[:], scalar1=gpos[:, tj, ex:ex + 1],
                                                                               scalar2=rwt[:, tj, ex:ex + 1], op0=ALU.is_equal, op1=ALU.mult),
                         reads=['iot', 'gpos', 'rwt'], writes=[swk])
                    pt_, ptk = pst.next()
                    for cc, (u0, ur) in enumerate(CHUNKS):
                        a.op('pe', lambda e, pt_=pt_, cc=cc, sw=sw, u0=u0, ur=ur: e.matmul(pt_[0:ur, cc * 128:(cc + 1) * 128], lhsT=sw[:, u0:u0 + ur],
                                                                                            rhs=idb[:], start=True, stop=True), reads=[swk, 'idb'], writes=[ptk])
                    a.op('act', lambda e, pt_=pt_, sT=sT, ex=ex: e.copy(out=sT[:, ex, 0:2, :], in_=pt_[:, 0:256].rearrange("p (c t) -> p c t", c=2)),
                         reads=[ptk], writes=[sTk])
                    a.op('act', lambda e, pt_=pt_, sT=sT, ex=ex: e.copy(out=sT[0:64, ex, 2, :], in_=pt_[0:64, 256:384]),
                         reads=[ptk], writes=[sTk])
                st_, stk = stg.next()
                for dt in range(4):
                    p, pk = psm.next()
                    n = 0
                    for ex in range(NE):
                        for cc, (u0, ur) in enumerate(CHUNKS):
                            a.op('pe', lambda e, p=p, sT=sT, ex=ex, cc=cc, dt=dt, n=n, ur=ur: e.matmul(
                                p[:, :], lhsT=sT[0:ur, ex, cc, :], rhs=Y2v[0:ur, ex * NCH + cc, dt * 512:(dt + 1) * 512],
                                start=(n == 0), stop=(n == NE * NCH - 1)), reads=[sTk, 'aT'], writes=[pk])
                            n += 1
                    if dt % 2 == 0:
                        a.op('act', lambda e, p=p, st_=st_, dt=dt: e.copy(out=st_[:, dt * 512:(dt + 1) * 512], in_=p[:, :]), reads=[pk], writes=[stk])
                    else:
                        a.op('dve', lambda e, p=p, st_=st_, dt=dt: e.tensor_copy(out=st_[:, dt * 512:(dt + 1) * 512], in_=p[:, :]), reads=[pk], writes=[stk])
                a.dma('act', yo[tj * 128:(tj + 1) * 128, :], st_[:], reads=[stk], writes=[('yo', tj)])
    return c.done()


def build_BC(NT):
    prog = Prog()
    oT = prog.nc.dram_tensor("oT_s", [3, 1024, NT], BF16).ap()
    build_B(ctx=Ctx(prog, bind={'oaT': oT[0], 'obT': oT[1], 'ocT': oT[2]}))
    build_C(NT, 1, ctx=Ctx(prog, bind={'oT': oT}))
    prog.semst.close()
    return prog.nc


def build_FA(NT):
    prog = Prog()
    cF = Ctx(prog)
    xo = prog.nc.dram_tensor("xo", [NT, 2048], F32, kind="ExternalOutput").ap()
    cF.bind = {'xo': xo}
    build_F(NT, 1, ctx=cF)
    build_A(NT, 1, ctx=Ctx(prog, bind={'x': xo}))
    prog.semst.close()
    return prog.nc


def _rope_table(pos_r, pos_c, dim):
    half = dim // 2
    fr = (10000.0 ** (-np.arange(0, half, 2, dtype=np.float32) / np.float32(half))).astype(np.float32)
    ar = pos_r[:, None].astype(np.float32) * fr
    ac = pos_c[:, None].astype(np.float32) * fr
    C = np.concatenate([np.cos(ar), np.cos(ar), np.cos(ac), np.cos(ac)], 1)
    S = np.concatenate([-np.sin(ar), np.sin(ar), -np.sin(ac), np.sin(ac)], 1)
    return np.ascontiguousarray(np.concatenate([C, S], 1).astype(np.float32))


_PROGS = {}


def _prog(name, fn):
    if name not in _PROGS:
        _PROGS[name] = fn()
    return _PROGS[name]


def kernel(x, c, ctx, c_ctx, w_mod, b_mod, g_mix, w_in, g_q_a, w_uq, g_kv_a, w_ukv, g_qn, g_kn,
           rpb, w_branch, w_out, g_ffn, w_router, b_router, w_exp1, b_exp1, w_exp2, b_exp2, g_final):
    f32 = np.float32
    x = np.asarray(x, f32)
    ctx = np.asarray(ctx, f32)
    B, S, D = x.shape
    NT = 2176
    ident = np.eye(128, dtype=f32)
    cores = [(b, h) for b in range(4) for h in range(2)]

    cT = np.zeros((2048, 8), f32)
    cT[:, 0:4] = np.asarray(c, f32).T
    cT[:, 4] = np.asarray(c_ctx, f32)
    w_all = np.concatenate([np.asarray(w_mod[0]), np.asarray(w_mod[1])], axis=1)
    b_all = np.concatenate([np.asarray(b_mod[0]), np.asarray(b_mod[1])], axis=0)[None, :]
    ncm = _prog('M', lambda: build_mod(3072))
    rm = run_spmd(ncm, [dict(cT=cT, w=np.ascontiguousarray(w_all[:, 3072 * k:3072 * (k + 1)]),
                             b=np.ascontiguousarray(b_all[:, 3072 * k:3072 * (k + 1)])) for k in range(8)])
    mod_all = np.concatenate([rm[k]["o"] for k in range(8)], axis=1)
    del w_all

    def grp(l, j, b):
        m = mod_all[:, l * 12288 + j * 2048: l * 12288 + (j + 1) * 2048]
        return np.ascontiguousarray(np.stack([m[4], m[b]], 0))

    xs = [np.ascontiguousarray(np.concatenate([ctx[b, 128 * h:128 * h + 128], x[b, 2048 * h:2048 * h + 2048]], 0)) for b, h in cores]
    ropeb, ropea = [], []
    for b, h in cores:
        t = np.arange(2048 * h, 2048 * h + 2048)
        pr = np.concatenate([np.zeros(128), t // 64]).astype(f32)
        pc = np.concatenate([np.zeros(128), t % 64]).astype(f32)
        ropeb.append(_rope_table(pr, pc, 128))
        ropea.append(_rope_table(pr, pc, 64))

    ncA = _prog('A', lambda: build_A(NT, 1))
    ncBC = _prog('BC', lambda: build_BC(NT))
    ncD = _prog('D', lambda: build_D2(8 * NT))
    ncFA = _prog('FA', lambda: build_FA(NT))
    ncF = _prog('F', lambda: build_F(NT, 1))
    row = lambda v: np.ascontiguousarray(np.asarray(v, f32)[None, :])
    utri = np.triu(np.ones((128, 128), f32), 1)
    iota = np.ascontiguousarray(np.tile(np.arange(CAP, dtype=f32), (128, 1)))
    parts = None
    for l in range(2):
        w_in_l = np.ascontiguousarray(np.asarray(w_in[l], f32))
        w_uq_l = np.ascontiguousarray(np.asarray(w_uq[l], f32))
        w_ukv_l = np.ascontiguousarray(np.asarray(w_ukv[l], f32))
        inA = [dict(g=row(g_mix[l]), sc=grp(l, 1, b), sh=grp(l, 0, b), w_in=w_in_l, g_q=row(g_q_a[l]),
                    g_kv=row(g_kv_a[l]), w_uq=w_uq_l, w_ukv=w_ukv_l, g_qn=row(g_qn[l]), g_kn=row(g_kn[l]),
                    ident=ident, ropeb=ropeb[k], ropea=ropea[k]) for k, (b, h) in enumerate(cores)]
        if l == 0:
            for k in range(8):
                inA[k]['x'] = xs[k]
            ra = run_spmd(ncA, inA)
        else:
            for k, (b, h) in enumerate(cores):
                inA[k].update(x=xs[k], parts=parts[k], gt2=grp(l - 1, 5, b), gfin=row(g_final))
            ra = run_spmd(ncFA, inA)
            xs = [ra[k]['xo'] for k in range(8)]
            parts = None
        del w_in_l, inA
        inB = []
        rpb_l = np.asarray(rpb[l], f32)
        w_b_l = np.ascontiguousarray(np.asarray(w_branch[l], f32))
        w_o_l = np.ascontiguousarray(np.asarray(w_out[l], f32))
        for k, (b, h) in enumerate(cores):
            k0, k1 = 2 * b, 2 * b + 1

            def full(name):
                return np.concatenate([ra[k0][name][:128], ra[k1][name][:128], ra[k0][name][128:], ra[k1][name][128:]], 0)
            kva = full('kva').reshape(NK, 8, 256)
            lrows = np.arange(40) + 32 * h - 4
            ltok = np.concatenate([np.arange(256)] + [256 + r * 64 + np.arange(64) if 0 <= r < 64 else np.full(64, -1) for r in lrows])
            msk = ltok >= 0

            def takek(v):
                o = np.zeros((len(ltok),) + v.shape[1:], v.dtype)
                o[msk] = v[ltok[msk]]
                return o
            kc_f, vc_f = full('kc'), full('vc')
            inB.append(dict(
                qaT=np.ascontiguousarray(ra[k]['qa'].reshape(NQ, 8, 192).transpose(1, 2, 0)),
                kaT=np.ascontiguousarray(kva[:, :, :128].transpose(1, 2, 0)),
                kpeT=np.ascontiguousarray(full('kpe').T),
                va=np.ascontiguousarray(kva[:, :, 128:].reshape(NK, 1024)),
                qbT=np.ascontiguousarray(ra[k]['qb'].reshape(NQ, 8, 128).transpose(1, 2, 0)),
                kbT=np.ascontiguousarray(full('kb').reshape(NK, 2, 128).transpose(1, 2, 0)),
                vb=np.ascontiguousarray(full('vb')),
                qcT=np.ascontiguousarray(ra[k]['qc'].reshape(NQ, 8, 128).transpose(1, 2, 0)),
                kcT=np.ascontiguousarray(takek(kc_f).reshape(NKL, 8, 128).transpose(1, 2, 0)),
                vc=np.ascontiguousarray(takek(vc_f)),
                nab=na_bias_table(rpb_l, h),
                gate=ra[k]['gate'], x=xs[k], w_branch=w_b_l, w_out=w_o_l, gt1=grp(l, 2, b), g=row(g_ffn[l]), sc=grp(l, 4, b),
                sh=grp(l, 3, b), w_r=np.ascontiguousarray(np.asarray(w_router[l], f32)), b_r=row(b_router[l]), ident=ident))
        rc = run_spmd(ncBC, inB)
        del inB, ra
        xs = [rc[k]['xo'] for k in range(8)]
        n2m = np.ascontiguousarray(np.stack([rc[k]['n2'] for k in range(8)], 0).reshape(8, 128, 17, 2048).transpose(2, 1, 0, 3).reshape(8 * NT, 2048))
        rw_all = np.stack([rc[k]['rw'] for k in range(8)], 0).reshape(8, 128, 17, 32).transpose(2, 1, 0, 3).reshape(8 * NT, 32)
        del rc
        rd = run_spmd(ncD, [dict(n2=n2m, rw=np.ascontiguousarray(rw_all[:, 4 * k:4 * k + 4]),
                                 w1=np.ascontiguousarray(np.asarray(w_exp1[l][4 * k:4 * k + 4], f32)),
                                 b1=np.ascontiguousarray(np.asarray(b_exp1[l][4 * k:4 * k + 4], f32)),
                                 w2=np.ascontiguousarray(np.asarray(w_exp2[l][4 * k:4 * k + 4], f32)),
                                 b2=np.ascontiguousarray(np.asarray(b_exp2[l][4 * k:4 * k + 4], f32)), ident=ident,
                                 utri=utri, iota=iota) for k in range(8)])
        del n2m
        parts = [np.ascontiguousarray(np.stack([rd[cc]['y'].reshape(17, 128, 8, 2048)[:, :, k, :].transpose(1, 0, 2).reshape(NT, 2048) for cc in range(8)], 0)) for k in range(8)]
        del rd
    rf = run_spmd(ncF, [dict(x=xs[k], parts=parts[k], gt2=grp(1, 5, b), gfin=row(g_final)) for k, (b, h) in enumerate(cores)])
    fo = [rf[k]['fo'] for k in range(8)]
    out = np.zeros((B, S, D), f32)
    for k, (b, h) in enumerate(cores):
        out[b, 2048 * h:2048 * h + 2048] = fo[k][128:]
    return out
```

```python
import contextlib
import numpy as np
import concourse.bass as bass
import concourse.mybir as mybir
from concourse.bass_utils import run_bass_kernel_spmd

F32 = mybir.dt.float32
BF16 = mybir.dt.bfloat16
ALU = mybir.AluOpType
AF = mybir.ActivationFunctionType
AX = mybir.AxisListType

NCORES = 8
SEM_G = 8192
NDMA = 40
NDMA_SW = 8


class AS:
    def __init__(self, nc, stack, prefix="", prev=None, pool=None):
        self.nc = nc
        self.stack = stack
        self.prefix = prefix
        self.prev = prev
        self.done_sem = None
        self.go_sem = None
        self.pool = pool if pool is not None else {'d': None, 'c': {}}
        self.streams = {k: [] for k in ('pe', 'act', 'dve', 'pool', 'sp')}
        self.count = {k: 0 for k in self.streams}
        self.waited = {k: {} for k in self.streams}
        self.last_w = {}
        self.readers = {}
        self.dma_i = 0
        self.dma_sw = 0
        self.csem = {k: [] for k in self.streams}
        if self.pool['d'] is None:
            self.pool['d'] = [stack.enter_context(nc.semaphore(f"dq{i}")) for i in range(NDMA + NDMA_SW)]
        self.dsem = self.pool['d']
        self.dma_final = {}

    def _deps(self, reads, writes):
        toks = {}

        def add(t):
            k = (t[0], t[1])
            if toks.get(k, 0) < t[2]:
                toks[k] = t[2]
        for b in reads:
            if b in self.last_w:
                add(self.last_w[b])
        for b in writes:
            if b in self.last_w:
                add(self.last_w[b])
            for k, v in self.readers.get(b, {}).items():
                add((k[0], k[1], v))
        return toks

    def _emit_waits(self, eng, toks):
        for k, val in toks.items():
            if k[0] == 'c' and k[1] == 'pe' and eng == 'pe':
                continue
            if self.waited[eng].get(k, 0) >= val:
                continue
            self.waited[eng][k] = val
            self.streams[eng].append(('wait', k, val))

    def _record(self, tok, reads, writes):
        k = (tok[0], tok[1])
        for b in reads:
            r = self.readers.setdefault(b, {})
            if r.get(k, 0) < tok[2]:
                r[k] = tok[2]
        for b in writes:
            self.last_w[b] = tok
            self.readers[b] = {}

    def op(self, eng, fn, reads=(), writes=()):
        toks = self._deps(reads, writes)
        self._emit_waits(eng, toks)
        self.count[eng] += 1
        idx = self.count[eng]
        self.streams[eng].append(('op', fn, idx))
        self._record(('c', eng, idx), reads, writes)

    def dma(self, eng, out, in_, reads=(), writes=(), **kw):
        if eng == 'pool':
            i = self.dma_sw
            self.dma_sw += 1
            s = NDMA + i % NDMA_SW
            prev = 16 * (i // NDMA_SW)
        else:
            i = self.dma_i
            self.dma_i += 1
            s = i % NDMA
            prev = 16 * (i // NDMA)
        toks = self._deps(reads, writes)
        if prev > 0:
            k = ('d', s)
            if toks.get(k, 0) < prev:
                toks[k] = prev
        self._emit_waits(eng, toks)
        self.streams[eng].append(('dma', out, in_, s, kw))
        self.dma_final[s] = prev + 16
        self._record(('d', s, prev + 16), reads, writes)

    def _sem_for(self, k, val):
        if k[0] == 'd':
            return self.dsem[k[1]], val
        eng = k[1]
        g = (val - 1) // SEM_G
        return self.csem[eng][g], (val - 1) % SEM_G + 1

    def finish(self):
        nc = self.nc
        for s, v in self.dma_final.items():
            self.streams['sp'].append(('wait', ('d', s), v))
        for eng in self.streams:
            if eng != 'sp' and self.count[eng] > 0:
                self.streams['sp'].append(('wait', ('c', eng), self.count[eng]))
        self.done_sem = self.stack.enter_context(nc.semaphore(f"{self.prefix}done"))
        self.streams['sp'].append(('done',))
        if self.prev is not None:
            self.go_sem = self.stack.enter_context(nc.semaphore(f"{self.prefix}go"))
        reuse = list(self.pool['d']) + [x for v in self.pool['c'].values() for x in v] if self.prev is not None else []
        for eng in self.streams:
            ng = max((self.count[eng] + SEM_G - 1) // SEM_G, 1)
            have = self.pool['c'].setdefault(eng, [])
            while len(have) < ng:
                have.append(self.stack.enter_context(nc.semaphore(f"c_{eng}{len(have)}")))
            self.csem[eng] = have
        engmap = {'pe': 'tensor', 'act': 'scalar', 'dve': 'vector', 'pool': 'gpsimd', 'sp': 'sync'}
        with nc.Block() as block:
            for eng, attr in engmap.items():
                stream = self.streams[eng]
                if not stream and self.prev is None:
                    continue

                def body(e, stream=stream, eng=eng):
                    if self.prev is not None:
                        e.wait_ge(self.prev, 1)
                        if eng == 'sp':
                            for sm in reuse:
                                e.sem_clear(sm)
                            e.nop().then_inc(self.go_sem, 1)
                        else:
                            e.wait_ge(self.go_sem, 1)
                    for it in stream:
                        if it[0] == 'done':
                            e.nop().then_inc(self.done_sem, 1)
                        elif it[0] == 'wait':
                            sem, val = self._sem_for(it[1], it[2])
                            e.wait_ge(sem, val)
                        elif it[0] == 'op':
                            idx = it[2]
                            sem = self.csem[eng][(idx - 1) // SEM_G]
                            it[1](e).then_inc(sem, 1)
                        else:
                            _, out, in_, s, kw = it
                            e.dma_start(out=out, in_=in_, **kw).then_inc(self.dsem[s], 16)
                getattr(block, attr)(body)


def new_nc():
    return bass.Bass("TRN2", target_bir_lowering=False)


def run_spmd(nc, in_maps):
    res = run_bass_kernel_spmd(nc, in_maps, core_ids=list(range(len(in_maps))))
    return res.results


def build_mod(ncol):
    nc = new_nc()
    cT = nc.dram_tensor("cT", [2048, 8], F32, kind="ExternalInput").ap()
    w = nc.dram_tensor("w", [2048, ncol], F32, kind="ExternalInput").ap()
    b = nc.dram_tensor("b", [1, ncol], F32, kind="ExternalInput").ap()
    o = nc.dram_tensor("o", [8, ncol], F32, kind="ExternalOutput").ap()
    NT = ncol // 512
    with contextlib.ExitStack() as st:
        a = AS(nc, st)
        sb = lambda name, shape, dt: st.enter_context(nc.sbuf_tensor(name, shape, dt))
        ct = sb("ct", [128, 16, 8], F32)
        cs = sb("cs", [128, 16, 8], F32)
        wt = [sb(f"wt{i}", [128, 16, 512], F32) for i in range(2)]
        bt = sb("bt", [8, ncol], F32)
        ot = sb("ot", [8, ncol], F32)
        ps = [st.enter_context(nc.psum_tensor(f"ps{i}", [128, 512], F32)) for i in range(2)]
        a.dma('sp', ct[:], cT.rearrange("(k p) c -> p k c", p=128), writes=['ct'])
        a.dma('sp', bt[:], b.broadcast_to([8, ncol]), writes=['bt'])
        a.op('act', lambda e: e.activation(out=cs[:], in_=ct[:], func=AF.Silu), reads=['ct'], writes=['cs'])
        for t in range(NT):
            wb = wt[t % 2]
            a.dma('sp' if t % 2 == 0 else 'act', wb[:], w[:, t * 512:(t + 1) * 512].rearrange("(k p) n -> p k n", p=128),
                  writes=[f'wt{t % 2}'])
            p = ps[t % 2]
            for k in range(16):
                a.op('pe', lambda e, k=k, p=p, wb=wb: e.matmul(p[0:8, :], lhsT=cs[:, k, :], rhs=wb[:, k, :],
                                                               start=(k == 0), stop=(k == 15)),
                     reads=['cs', f'wt{t % 2}'], writes=[f'ps{t % 2}'])
            a.op('dve', lambda e, p=p, t=t: e.tensor_tensor(out=ot[:, t * 512:(t + 1) * 512], in0=p[0:8, :],
                                                            in1=bt[:, t * 512:(t + 1) * 512], op=ALU.add),
                 reads=[f'ps{t % 2}', 'bt'], writes=['ot'])
        a.dma('sp', o, ot[:], reads=['ot'], writes=['o'])
        a.finish()
    return nc


class Prog:
    def __init__(self):
        self.nc = new_nc()
        self.semst = contextlib.ExitStack()
        self.prev = None
        self.nphase = 0
        self.pool = {'d': None, 'c': {}}


class Ctx:
    def __init__(self, prog=None, bind=None):
        self.prog = prog if prog is not None else Prog()
        self.standalone = prog is None
        self.nc = self.prog.nc
        self.st = contextlib.ExitStack()
        self.pfx = f"p{self.prog.nphase}_"
        self.prog.nphase += 1
        self.a = AS(self.nc, self.prog.semst, prefix=self.pfx, prev=self.prog.prev, pool=self.prog.pool)
        self.bind = bind or {}
        self.nps = 0

    def din(self, name, shape, dt=F32):
        if name in self.bind:
            return self.bind[name]
        return self.nc.dram_tensor(name, list(shape), dt, kind="ExternalInput").ap()

    def dout(self, name, shape, dt=F32):
        if name in self.bind:
            return self.bind[name]
        return self.nc.dram_tensor(name, list(shape), dt, kind="ExternalOutput").ap()

    def dscratch(self, name, shape, dt=F32):
        return self.nc.dram_tensor(self.pfx + name, list(shape), dt).ap()

    def sb(self, name, shape, dt=F32):
        return self.st.enter_context(self.nc.sbuf_tensor(self.pfx + name, list(shape), dt))

    def ps(self, name):
        self.nps += 1
        assert self.nps <= 8
        return self.st.enter_context(self.nc.psum_tensor(self.pfx + name, [128, 512], F32))

    def done(self):
        self.a.finish()
        self.prog.prev = self.a.done_sem
        self.st.close()
        if self.standalone:
            self.prog.semst.close()
        return self.nc


class Rot:
    def __init__(self, items):
        self.items = items
        self.i = 0

    def next(self):
        it = self.items[self.i % len(self.items)]
        self.i += 1
        return it


def rope_ops(a, eng, x, C, S, t1, t2, out, H, Wd, keys):
    kx, kC, kS, k1, k2, ko = keys
    hw = Wd // 4
    xv = x.rearrange("p (h a f w) -> p h a f w", h=H, a=2, f=2, w=hw)
    t2v = t2.rearrange("p (h a f w) -> p h a f w", h=H, a=2, f=2, w=hw)
    Sv = S.rearrange("p (a f w) -> p a f w", a=2, f=2, w=hw)
    x3 = x.rearrange("p (h d) -> p h d", h=H)
    t13 = t1.rearrange("p (h d) -> p h d", h=H)
    Cb = C.unsqueeze(1).broadcast_to([128, H, Wd])
    a.op(eng, lambda e: e.tensor_tensor(out=t13, in0=x3, in1=Cb, op=ALU.mult), reads=[kx, kC], writes=[k1])
    for f in range(2):
        Sb = Sv[:, :, f, :].unsqueeze(1).broadcast_to([128, H, 2, hw])
        a.op(eng, lambda e, f=f, Sb=Sb: e.tensor_tensor(out=t2v[:, :, :, f, :], in0=xv[:, :, :, 1 - f, :], in1=Sb,
                                                        op=ALU.mult), reads=[kx, kS], writes=[k2])
    a.op(eng, lambda e: e.tensor_tensor(out=out, in0=t1, in1=t2, op=ALU.add), reads=[k1, k2], writes=[ko])


A_COLS = [
    ('dq', 0, 512, 'dq'), ('dkv', 512, 512, 'dkv'), ('kr', 1024, 64, 'kr'),
    ('qb0', 1088, 512, 'qb'), ('qb1', 1600, 512, 'qb'), ('kb', 2112, 256, 'kb'), ('vb', 2368, 256, 'copy'),
    ('qc0', 2624, 512, 'copy'), ('qc1', 3136, 512, 'copy'), ('kc0', 3648, 512, 'copy'), ('kc1', 4160, 512, 'copy'),
    ('vc0', 4672, 512, 'copy'), ('vc1', 5184, 512, 'copy'),
] + [(f'gt{i}', 5696 + 512 * i, 512, 'sig') for i in range(12)]
A_OUT = {'qa': 1536, 'kva': 2048, 'kpe': 64, 'qb': 1024, 'kb': 256, 'vb': 256, 'qc': 1024, 'kc': 1024, 'vc': 1024,
         'gate': 6144}
A_DEST = {'vb': ('vb', 0), 'qc0': ('qc', 0), 'qc1': ('qc', 512), 'kc0': ('kc', 0), 'kc1': ('kc', 512),
          'vc0': ('vc', 0), 'vc1': ('vc', 512), 'qb0': ('qb', 0), 'qb1': ('qb', 512), 'kb': ('kb', 0), 'kr': ('kpe', 0)}
EPS = 1e-6


def build_A(NT, g0_tiles, ctx=None):
    c = ctx or Ctx()
    a = c.a
    nc = c.nc
    NTT = NT // 128
    x = c.din("x", [NT, 2048])
    g = c.din("g", [1, 2048])
    sc = c.din("sc", [2, 2048])
    sh = c.din("sh", [2, 2048])
    w_in = c.din("w_in", [2048, 11840])
    g_q = c.din("g_q", [1, 512])
    g_kv = c.din("g_kv", [1, 512])
    w_uq = c.din("w_uq", [512, 1536])
    w_ukv = c.din("w_ukv", [512, 2048])
    g_qn = c.din("g_qn", [1, 128])
    g_kn = c.din("g_kn", [1, 128])
    ident = c.din("ident", [128, 128])
    ropeb = c.din("ropeb", [NT, 256])
    ropea = c.din("ropea", [NT, 128])
    outs = {k: c.dout(k, [NT, w], BF16) for k, w in A_OUT.items()}

    idf = c.sb("idf", [128, 128])
    idb = c.sb("idb", [128, 128], BF16)
    At = c.sb("At", [128, 2048])
    St = c.sb("St", [128, 2048])
    gt = c.sb("gt", [128, 2048])
    gq = c.sb("gq", [128, 512])
    gkv = c.sb("gkv", [128, 512])
    gqn = c.sb("gqn", [128, 128])
    gkn = c.sb("gkn", [128, 128])
    nT = c.sb("nT", [128, 16, NT], BF16)
    wuq = c.sb("wuq", [128, 4, 1536], BF16)
    wukv = c.sb("wukv", [128, 4, 2048], BF16)
    xt = Rot([(c.sb(f"xt{i}", [128, 2048]), f"xt{i}") for i in range(1)])
    nb = Rot([(c.sb(f"nb{i}", [128, 2048], BF16), f"nb{i}") for i in range(1)])
    scr = c.sb("scr", [128, 2048])
    small = Rot([(c.sb(f"sm{i}", [128, 16]), f"sm{i}") for i in range(4)])
    wt = Rot([(c.sb(f"wt{i}", [128, 16, 512], BF16), f"wt{i}") for i in range(2)])
    ob = Rot([(c.sb(f"ob{i}", [128, 512], BF16), f"ob{i}") for i in range(3)])
    w1 = Rot([(c.sb(f"w1_{i}", [128, 1024]), f"w1_{i}") for i in range(2)])
    w2 = c.sb("w2", [128, 1024])
    w3 = c.sb("w3", [128, 1024])
    ynb = c.sb("ynb", [128, 512], BF16)
    ynT = c.sb("ynT", [128, 4, 128], BF16)
    qab = c.sb("qab", [128, 2048], BF16)
    rb = Rot([(c.sb(f"rb{i}", [128, 256]), f"rb{i}") for i in range(2)])
    ra = Rot([(c.sb(f"ra{i}", [128, 128]), f"ra{i}") for i in range(2)])
    pst = Rot([(c.ps(f"pst{i}"), f"pst{i}") for i in range(2)])
    psm = Rot([(c.ps(f"psm{i}"), f"psm{i}") for i in range(3)])
    psu = Rot([(c.ps(f"psu{i}"), f"psu{i}") for i in range(3)])

    epsb = c.sb("epsb", [128, 1])
    a.op('dve', lambda e: e.memset(epsb[:], EPS), writes=['epsb'])
    a.dma('sp', idf[:], ident, writes=['idf'])
    a.op('dve', lambda e: e.tensor_copy(out=idb[:], in_=idf[:]), reads=['idf'], writes=['idb'])
    a.dma('sp', gt[:], g.broadcast_to([128, 2048]), writes=['gt'])
    a.dma('sp', gq[:], g_q.broadcast_to([128, 512]), writes=['gq'])
    a.dma('sp', gkv[:], g_kv.broadcast_to([128, 512]), writes=['gkv'])
    a.dma('sp', gqn[:], g_qn.broadcast_to([128, 128]), writes=['gqn'])
    a.dma('sp', gkn[:], g_kn.broadcast_to([128, 128]), writes=['gkn'])
    a.dma('pool', wuq[:], w_uq.rearrange("(k p) n -> p k n", p=128), writes=['wuq'])
    a.dma('pool', wukv[:], w_ukv.rearrange("(k p) n -> p k n", p=128), writes=['wukv'])

    def load_group(gi):
        a.dma('sp', At[:], sc[gi:gi + 1, :].broadcast_to([128, 2048]), writes=['At'])
        a.dma('sp', St[:], sh[gi:gi + 1, :].broadcast_to([128, 2048]), writes=['St'])
        a.op('dve', lambda e: e.scalar_tensor_tensor(out=At[:], in0=At[:], scalar=1.0, in1=gt[:], op0=ALU.add,
                                                     op1=ALU.mult), reads=['At', 'gt'], writes=['At'])

    def rstd_from(ssap, sskey, n, outap, outkey):
        a.op('act', lambda e: e.activation(out=outap, in_=ssap, func=AF.Sqrt, bias=epsb[:, 0:1], scale=1.0 / n),
             reads=[sskey, 'epsb'], writes=[outkey])
        a.op('dve', lambda e: e.reciprocal(out=outap, in_=outap), reads=[outkey], writes=[outkey])

    for tt in range(NTT):
        if tt == 0:
            load_group(0)
        elif tt == g0_tiles:
            load_group(1)
        xtile, xk = xt.next()
        a.dma('sp', xtile[:], x[tt * 128:(tt + 1) * 128, :], writes=[xk])
        sm, smk = small.next()
        a.op('dve', lambda e, sm=sm: e.memset(sm[:], 0.0), writes=[smk])
        a.op('act', lambda e, xtile=xtile, sm=sm: e.activation(out=scr[:], in_=xtile[:], func=AF.Square,
                                                               accum_out=sm[:, 0:1]),
             reads=[xk, smk], writes=['scr', smk])
        rstd_from(sm[:, 0:1], smk, 2048, sm[:, 1:2], smk)
        a.op('dve', lambda e, xtile=xtile, sm=sm: e.scalar_tensor_tensor(out=xtile[:], in0=xtile[:], scalar=sm[:, 1:2],
                                                                         in1=At[:], op0=ALU.mult, op1=ALU.mult),
             reads=[xk, smk, 'At'], writes=[xk])
        nbt, nbk = nb.next()
        a.op('pool', lambda e, xtile=xtile, nbt=nbt: e.tensor_tensor(out=nbt[:], in0=xtile[:], in1=St[:], op=ALU.add),
             reads=[xk, 'St'], writes=[nbk])
        for q4 in range(4):
            p, pk = pst.next()
            for j in range(4):
                kc = q4 * 4 + j
                a.op('pe', lambda e, p=p, j=j, kc=kc, nbt=nbt: e.matmul(p[:, j * 128:(j + 1) * 128],
                                                                          lhsT=nbt[:, kc * 128:(kc + 1) * 128],
                                                                          rhs=idb[:], start=True, stop=True),
                     reads=[nbk, 'idb'], writes=[pk])
            eng = 'act' if q4 % 2 == 0 else 'dve'
            dst = nT[:, q4 * 4:(q4 + 1) * 4, tt * 128:(tt + 1) * 128]
            src = p[:].rearrange("p (j t) -> p j t", j=4)
            if eng == 'act':
                a.op('act', lambda e, dst=dst, src=src: e.copy(out=dst, in_=src), reads=[pk], writes=[('nT', tt)])
            else:
                a.op('dve', lambda e, dst=dst, src=src: e.tensor_copy(out=dst, in_=src), reads=[pk], writes=[('nT', tt)])

    def store(src_tile, src_key, name, col0, width, tt):
        a.dma('act', outs[name][tt * 128:(tt + 1) * 128, col0:col0 + width], src_tile[:, 0:width],
              reads=[src_key], writes=[('out', name, col0, tt)])

    def headnorm_rope(p, pk, cw, gtile, gkey, H, tt, name, col0):
        rbt, rbk = rb.next()
        a.dma('sp', rbt[:], ropeb[tt * 128:(tt + 1) * 128, :], writes=[rbk])
        xa, xak = w1.next()
        a.op('act', lambda e: e.copy(out=xa[:, 0:cw], in_=p[:, 0:cw]), reads=[pk], writes=[xak])
        a.op('pool', lambda e: e.tensor_tensor(out=w2[:, 0:cw], in0=xa[:, 0:cw], in1=xa[:, 0:cw], op=ALU.mult),
             reads=[xak], writes=['w2'])
        sm, smk = small.next()
        a.op('dve', lambda e: e.tensor_reduce(out=sm[:, 0:H], in_=w2[:, 0:cw].rearrange("p (h d) -> p h d", h=H),
                                              axis=AX.X, op=ALU.add), reads=['w2'], writes=[smk])
        rstd_from(sm[:, 0:H], smk, 128, sm[:, 8:8 + H], smk)
        x3 = xa[:, 0:cw].rearrange("p (h d) -> p h d", h=H)
        a.op('dve', lambda e: e.tensor_tensor(out=x3, in0=x3, in1=sm[:, 8:8 + H].unsqueeze(2).broadcast_to([128, H, 128]),
                                              op=ALU.mult), reads=[xak, smk], writes=[xak])
        a.op('dve', lambda e: e.tensor_tensor(out=x3, in0=x3, in1=gtile[:].unsqueeze(1).broadcast_to([128, H, 128]),
                                              op=ALU.mult), reads=[xak, gkey], writes=[xak])
        obt, obk = ob.next()
        rope_ops(a, 'dve', xa[:, 0:cw], rbt[:, 0:128], rbt[:, 128:256], w2[:, 0:cw], w3[:, 0:cw], obt[:, 0:cw], H, 128,
                 (xak, rbk, rbk, 'w2', 'w3', obk))
        store(obt, obk, name, col0, cw, tt)

    def upproj(p, pk, gtile, gkey, wres, wkey, nout, oname, tt, do_rope):
        sm, smk = small.next()
        a.op('dve', lambda e: e.memset(sm[:], 0.0), writes=[smk])
        a.op('act', lambda e: e.activation(out=w2[:, 0:512], in_=p[:, 0:512], func=AF.Square, accum_out=sm[:, 0:1]),
             reads=[pk, smk], writes=['w2', smk])
        rstd_from(sm[:, 0:1], smk, 512, sm[:, 1:2], smk)
        a.op('dve', lambda e: e.scalar_tensor_tensor(out=ynb[:], in0=p[:, 0:512], scalar=sm[:, 1:2], in1=gtile[:],
                                                     op0=ALU.mult, op1=ALU.mult), reads=[pk, smk, gkey], writes=['ynb'])
        pt, ptk = pst.next()
        for j in range(4):
            a.op('pe', lambda e, j=j: e.matmul(pt[:, j * 128:(j + 1) * 128], lhsT=ynb[:, j * 128:(j + 1) * 128],
                                               rhs=idb[:], start=True, stop=True), reads=['ynb', 'idb'], writes=[ptk])
        a.op('act', lambda e: e.copy(out=ynT[:], in_=pt[:].rearrange("p (j t) -> p j t", j=4)), reads=[ptk],
             writes=['ynT'])
        for n0 in range(0, nout, 512):
            pu, puk = psu.next()
            for k in range(4):
                a.op('pe', lambda e, k=k, n0=n0, pu=pu: e.matmul(pu[:, 0:512], lhsT=ynT[:, k, :], rhs=wres[:, k, n0:n0 + 512],
                                                          start=(k == 0), stop=(k == 3)), reads=['ynT', wkey], writes=[puk])
            if do_rope:
                a.op('act', lambda e, n0=n0, pu=pu: e.copy(out=w3[:, 0:512], in_=pu[:, 0:512]), reads=[puk], writes=['w3'])
                a.op('pool', lambda e, n0=n0: e.tensor_copy(out=scr[:, n0:n0 + 512], in_=w3[:, 0:512]), reads=['w3'],
                     writes=['scr'])
            else:
                eng = 'act' if (n0 // 512) % 2 == 0 else 'dve'
                if eng == 'act':
                    a.op('act', lambda e, n0=n0, pu=pu: e.copy(out=qab[:, n0:n0 + 512], in_=pu[:, 0:512]), reads=[puk],
                         writes=['qab'])
                else:
                    a.op('dve', lambda e, n0=n0, pu=pu: e.tensor_copy(out=qab[:, n0:n0 + 512], in_=pu[:, 0:512]), reads=[puk],
                         writes=['qab'])
        if do_rope:
            rat, rak = ra.next()
            a.dma('sp', rat[:], ropea[tt * 128:(tt + 1) * 128, :], writes=[rak])
            q3 = scr[:, 0:1536].rearrange("p (h d) -> p h d", h=8)
            a.op('dve', lambda e: e.tensor_copy(out=w2[:, 0:512].rearrange("p (h d) -> p h d", h=8), in_=q3[:, :, 128:192]),
                 reads=['scr'], writes=['w2'])
            xa, xak = w1.next()
            rope_ops(a, 'dve', w2[:, 0:512], rat[:, 0:64], rat[:, 64:128], w3[:, 0:512], w3[:, 512:1024], xa[:, 0:512], 8, 64,
                     ('w2', rak, rak, 'w3', 'w3', xak))
            qv = qab[:, 0:1536].rearrange("p (h d) -> p h d", h=8)
            a.op('act', lambda e: e.copy(out=qv[:, :, 0:128], in_=q3[:, :, 0:128]), reads=['scr'], writes=['qab'])
            a.op('dve', lambda e: e.tensor_copy(out=qv[:, :, 128:192], in_=xa[:, 0:512].rearrange("p (h d) -> p h d", h=8)),
                 reads=[xak], writes=['qab'])
        a.dma('act', outs[oname][tt * 128:(tt + 1) * 128, :], qab[:, 0:nout], reads=['qab'], writes=[('out', oname, tt)])

    for (cname, c0, cw, kind) in A_COLS:
        wtile, wk = wt.next()
        a.dma('pool', wtile[:, :, 0:cw], w_in[:, c0:c0 + cw].rearrange("(k p) n -> p k n", p=128), writes=[wk])
        for tt in range(NTT):
            p, pk = psm.next()
            for k in range(16):
                a.op('pe', lambda e, p=p, k=k, tt=tt, wtile=wtile: e.matmul(p[:, 0:cw], lhsT=nT[:, k, tt * 128:(tt + 1) * 128],
                                                                             rhs=wtile[:, k, 0:cw], start=(k == 0),
                                                                             stop=(k == 15)),
                     reads=[('nT', tt), wk], writes=[pk])
            if kind in ('copy', 'sig'):
                obt, obk = ob.next()
                if kind == 'sig':
                    a.op('act', lambda e, p=p, obt=obt: e.activation(out=obt[:, 0:cw], in_=p[:, 0:cw], func=AF.Sigmoid),
                         reads=[pk], writes=[obk])
                    store(obt, obk, 'gate', c0 - 5696, cw, tt)
                else:
                    a.op('dve', lambda e, p=p, obt=obt: e.tensor_copy(out=obt[:, 0:cw], in_=p[:, 0:cw]), reads=[pk],
                         writes=[obk])
                    dn, dc = A_DEST[cname]
                    store(obt, obk, dn, dc, cw, tt)
            elif kind == 'qb':
                dn, dc = A_DEST[cname]
                headnorm_rope(p, pk, cw, gqn, 'gqn', 4, tt, dn, dc)
            elif kind == 'kb':
                headnorm_rope(p, pk, cw, gkn, 'gkn', 2, tt, 'kb', 0)
            elif kind == 'kr':
                rat, rak = ra.next()
                a.dma('sp', rat[:], ropea[tt * 128:(tt + 1) * 128, :], writes=[rak])
                xa, xak = w1.next()
                a.op('act', lambda e, p=p, xa=xa: e.copy(out=xa[:, 0:64], in_=p[:, 0:64]), reads=[pk], writes=[xak])
                obt, obk = ob.next()
                rope_ops(a, 'dve', xa[:, 0:64], rat[:, 0:64], rat[:, 64:128], w2[:, 0:64], w3[:, 0:64], obt[:, 0:64], 1, 64,
                         (xak, rak, rak, 'w2', 'w3', obk))
                store(obt, obk, 'kpe', 0, 64, tt)
            elif kind == 'dq':
                upproj(p, pk, gq, 'gq', wuq, 'wuq', 1536, 'qa', tt, True)
            elif kind == 'dkv':
                upproj(p, pk, gkv, 'gkv', wukv, 'wukv', 2048, 'kva', tt, False)
    return c.done()


NQ = 2176
NK = 4352
NKL = 256 + 40 * 64
NA_SLOTS = 60


def na_row_chunks(i):
    if i < 4:
        cs, ce = i // 2, 5
    elif i >= 28:
        cs, ce = 14, (i + 7) // 2
    else:
        cs, ce = i // 2, (i + 7) // 2
    typ = i if i < 4 else (6 + i - 28 if i >= 28 else 4 + (i % 2))
    return typ, list(range(cs, ce + 1))


def build_B(nheads=8, nrows=32, do=('a', 'b', 'c'), ctx=None):
    c = ctx or Ctx()
    a = c.a
    qaT = c.din("qaT", [8, 192, NQ], BF16)
    kaT = c.din("kaT", [8, 128, NK], BF16)
    kpeT = c.din("kpeT", [64, NK], BF16)
    va = c.din("va", [NK, 1024], BF16)
    qbT = c.din("qbT", [8, 128, NQ], BF16)
    kbT = c.din("kbT", [2, 128, NK], BF16)
    vb = c.din("vb", [NK, 256], BF16)
    qcT = c.din("qcT", [8, 128, NQ], BF16)
    kcT = c.din("kcT", [8, 128, NKL], BF16)
    vc = c.din("vc", [NKL, 1024], BF16)
    nab = c.din("nab", [8, 128, NA_SLOTS * 64])
    outs = {k: c.dout(k, [1024, NQ], BF16) for k in ('oaT', 'obT', 'ocT')}

    ones = c.sb("ones", [128, 128], BF16)
    a.op('dve', lambda e: e.memset(ones[:], 1.0), writes=['ones'])
    kt = Rot([(c.sb(f"kt{i}", [128, NK], BF16), f"kt{i}") for i in range(2)])
    kpe = c.sb("kpe", [64, NK], BF16)
    ktb = c.sb("ktb", [128, NK], BF16)
    vtb = c.sb("vtb", [128, 34, 128], BF16)
    vt = Rot([(c.sb(f"vt{i}", [128, 34, 128], BF16), f"vt{i}") for i in range(2)])
    qt = Rot([(c.sb(f"qt{i}", [128, NQ], BF16), f"qt{i}") for i in range(2)])
    qr = Rot([(c.sb(f"qr{i}", [64, NQ], BF16), f"qr{i}") for i in range(2)])
    pt = Rot([(c.sb(f"pt{i}", [128, 512], BF16), f"pt{i}") for i in range(3)])
    ot = Rot([(c.sb(f"ot{i}", [128, NQ], BF16), f"ot{i}") for i in range(2)])
    rs = Rot([(c.sb(f"rs{i}", [128, 512]), f"rs{i}") for i in range(2)])
    ef = c.sb("ef", [128, NA_SLOTS * 64])
    eb = Rot([(c.sb(f"eb{i}", [128, NA_SLOTS * 64], BF16), f"eb{i}") for i in range(2)])
    pss = Rot([(c.ps(f"pss{i}"), f"pss{i}") for i in range(3)])
    pso = Rot([(c.ps(f"pso{i}"), f"pso{i}") for i in range(2)])
    psr = Rot([(c.ps(f"psr{i}"), f"psr{i}") for i in range(2)])
    a.dma('sp', kpe[:], kpeT, writes=['kpe'])

    units = []
    pre = []

    def add_unit(s1, s2):
        p = list(pre)
        pre.clear()

        def s1_all():
            for f in p:
                f()
            s1()
        units.append((s1_all, s2))

    def attend(qparts, kparts, vtile, vk, q0, n, kchunks, scale, otile, ok, etab=None, post=None):
        po, pok = pso.next()
        pr, prk = psr.next()
        nk = len(kchunks)

        def finish():
            rt, rk = rs.next()
            a.op('dve', lambda e: e.reciprocal(out=rt[:, 0:n], in_=pr[:, 0:n]), reads=[prk], writes=[rk])
            a.op('dve', lambda e: e.tensor_tensor(out=otile[:, q0:q0 + n], in0=po[:, 0:n], in1=rt[:, 0:n], op=ALU.mult),
                 reads=[pok, rk], writes=[ok])
            if post is not None:
                post()

        if etab is None:
            for ji, j in enumerate(kchunks):
                p, pk = pss.next()
                ptile, ptk = pt.next()

                def s1(p=p, pk=pk, ptile=ptile, ptk=ptk, j=j):
                    for pi, ((qtile, qk, nr), (ktile, kk, _)) in enumerate(zip(qparts, kparts)):
                        a.op('pe', lambda e, qtile=qtile, ktile=ktile, nr=nr, pi=pi: e.matmul(
                            p[:, 0:n], lhsT=ktile[0:nr, j * 128:(j + 1) * 128], rhs=qtile[0:nr, q0:q0 + n],
                            start=(pi == 0), stop=(pi == len(qparts) - 1)), reads=[qk, kk], writes=[pk])
                    a.op('act', lambda e: e.activation(out=ptile[:, 0:n], in_=p[:, 0:n], func=AF.Exp, scale=scale),
                         reads=[pk], writes=[ptk])

                def s2(ptile=ptile, ptk=ptk, j=j, ji=ji):
                    a.op('pe', lambda e: e.matmul(po[:, 0:n], lhsT=vtile[:, j, :], rhs=ptile[:, 0:n],
                                                  start=(ji == 0), stop=(ji == nk - 1)), reads=[vk, ptk], writes=[pok])
                    a.op('pe', lambda e: e.matmul(pr[:, 0:n], lhsT=ones[:], rhs=ptile[:, 0:n],
                                                  start=(ji == 0), stop=(ji == nk - 1)), reads=['ones', ptk], writes=[prk])
                    if ji == nk - 1:
                        finish()
                add_unit(s1, s2)
        else:
            etile, ek, slot0, nloc = etab
            p, pk = pss.next()
            ptile, ptk = pt.next()
            (qtile, qk, nr), (ktile, kk, _) = qparts[0], kparts[0]

            def s1():
                for ji, j in enumerate(kchunks):
                    a.op('pe', lambda e, j=j, ji=ji: e.matmul(p[:, ji * 64:(ji + 1) * 64], lhsT=ktile[0:nr, j * 128:(j + 1) * 128],
                                                              rhs=qtile[0:nr, q0:q0 + 64], start=True, stop=True),
                         reads=[qk, kk], writes=[pk])
                a.op('act', lambda e: e.activation(out=ptile[:, 0:nk * 64], in_=p[:, 0:nk * 64], func=AF.Exp, scale=scale),
                     reads=[pk], writes=[ptk])
                a.op('dve', lambda e: e.tensor_tensor(out=ptile[:, 0:nloc * 64], in0=ptile[:, 0:nloc * 64],
                                                      in1=etile[:, slot0 * 64:(slot0 + nloc) * 64], op=ALU.mult),
                     reads=[ptk, ek], writes=[ptk])

            def s2():
                for ji, j in enumerate(kchunks):
                    a.op('pe', lambda e, j=j, ji=ji: e.matmul(po[:, 0:64], lhsT=vtile[:, j, :], rhs=ptile[:, ji * 64:(ji + 1) * 64],
                                                              start=(ji == 0), stop=(ji == nk - 1)), reads=[vk, ptk], writes=[pok])
                for ji, j in enumerate(kchunks):
                    a.op('pe', lambda e, ji=ji: e.matmul(pr[:, 0:64], lhsT=ones[:], rhs=ptile[:, ji * 64:(ji + 1) * 64],
                                                         start=(ji == 0), stop=(ji == nk - 1)), reads=['ones', ptk], writes=[prk])
                finish()
            add_unit(s1, s2)

    def D(eng, out, in_, **kw):
        pre.append(lambda: a.dma(eng, out, in_, **kw))

    ALLK = list(range(34))
    for h in range(nheads):
        if 'a' in do:
            ktile, kk = kt.next()
            D('sp', ktile[:], kaT[h], writes=[kk])
            vtile, vk = vt.next()
            D('act', vtile[:], va[:, h * 128:(h + 1) * 128].rearrange("(c p) d -> p c d", p=128), writes=[vk])
            qtile, qk = qt.next()
            D('sp', qtile[:], qaT[h, 0:128, :], writes=[qk])
            qrt, qrk = qr.next()
            D('sp', qrt[:], qaT[h, 128:192, :], writes=[qrk])
            otile, ok = ot.next()
            qp = [(qtile, qk, 128), (qrt, qrk, 64)]
            kp = [(ktile, kk, 128), (kpe, 'kpe', 64)]
            attend(qp, kp, vtile, vk, 0, 128, [0, 1], 192 ** -0.5, otile, ok)
            for t in range(4):
                attend(qp, kp, vtile, vk, 128 + 512 * t, 512, ALLK, 192 ** -0.5, otile, ok,
                       post=(lambda otile=otile, ok=ok, h=h: a.dma('act', outs['oaT'][h * 128:(h + 1) * 128, :], otile[:], reads=[ok],
                                                                    writes=[('oa', h)])) if t == 3 else None)
        if 'b' in do:
            if h % 4 == 0:
                kbtile, kbk = ktb, 'ktb'
                D('sp', kbtile[:], kbT[h // 4], writes=[kbk])
                vbtile, vbk = vtb, 'vtb'
                D('act', vbtile[:], vb[:, (h // 4) * 128:(h // 4 + 1) * 128].rearrange("(c p) d -> p c d", p=128),
                      writes=[vbk])
            qtile, qk = qt.next()
            D('sp', qtile[:], qbT[h], writes=[qk])
            otile, ok = ot.next()
            qp = [(qtile, qk, 128)]
            kp = [(kbtile, kbk, 128)]
            attend(qp, kp, vbtile, vbk, 0, 128, [0, 1], 128 ** -0.5, otile, ok)
            for t in range(4):
                attend(qp, kp, vbtile, vbk, 128 + 512 * t, 512, ALLK, 128 ** -0.5, otile, ok,
                       post=(lambda otile=otile, ok=ok, h=h: a.dma('act', outs['obT'][h * 128:(h + 1) * 128, :], otile[:], reads=[ok],
                                                                    writes=[('ob', h)])) if t == 3 else None)
        if 'c' in do:
            ktile, kk = kt.next()
            D('sp', ktile[:, 0:NKL], kcT[h], writes=[kk])
            vtile, vk = vt.next()
            D('act', vtile[:, 0:22, :], vc[:, h * 128:(h + 1) * 128].rearrange("(c p) d -> p c d", p=128), writes=[vk])
            qtile, qk = qt.next()
            D('sp', qtile[:], qcT[h], writes=[qk])
            D('sp', ef[:], nab[h], writes=['ef'])
            etile, ek = eb.next()
            pre.append(lambda etile=etile, ek=ek: a.op('act', lambda e: e.activation(out=etile[:], in_=ef[:], func=AF.Exp), reads=['ef'], writes=[ek]))
            otile, ok = ot.next()
            qp = [(qtile, qk, 128)]
            kp = [(ktile, kk, 128)]
            attend(qp, kp, vtile, vk, 0, 128, [0, 1], 128 ** -0.5, otile, ok)
            for i in range(nrows):
                typ, chunks = na_row_chunks(i)
                kch = [2 + cc for cc in chunks] + [0, 1]
                attend(qp, kp, vtile, vk, 128 + 64 * i, 64, kch, 128 ** -0.5, otile, ok,
                       etab=(etile, ek, typ * 6, len(chunks)),
                       post=(lambda otile=otile, ok=ok, h=h: a.dma('act', outs['ocT'][h * 128:(h + 1) * 128, :], otile[:], reads=[ok],
                                                                    writes=[('oc', h)])) if i == nrows - 1 else None)
    for i, (s1, s2) in enumerate(units):
        s1()
        if i > 0:
            units[i - 1][1]()
    units[-1][1]()
    return c.done()


def na_bias_table(rpb_l, half):
    tab = np.full((8, NA_SLOTS, 128, 64), -30000.0, np.float32)
    rep = {0: 0, 1: 1, 2: 2, 3: 3, 4: 4, 5: 5, 6: 28, 7: 29, 8: 30, 9: 31}
    cq = np.arange(64)
    c0 = np.clip(cq - 8, 0, 48)
    ck = np.arange(64)
    colvalid = (ck[:, None] >= c0[None, :]) & (ck[:, None] < c0[None, :] + 16)
    dc = np.clip(ck[:, None] - cq[None, :] + 15, 0, 30)
    for typ, i in rep.items():
        _, chunks = na_row_chunks(i)
        r = 32 * half + i
        r0 = min(max(r - 4, 0), 56)
        for j, cc in enumerate(chunks):
            for rl in range(2):
                rk = 2 * cc + rl + 32 * half - 4
                if rk < 0 or rk >= 64 or rk < r0 or rk >= r0 + 8:
                    continue
                vals = rpb_l[:, rk - r + 7][:, dc]
                blk = tab[:, typ * 6 + j, rl * 64:(rl + 1) * 64, :]
                blk[:, colvalid] = vals[:, colvalid]
    return np.ascontiguousarray(tab.transpose(0, 2, 1, 3).reshape(8, 128, NA_SLOTS * 64))


def build_C(NT, g0_tiles, ctx=None):
    c = ctx or Ctx()
    a = c.a
    NTT = NT // 128
    oT = c.din("oT", [3, 1024, NT], BF16)
    gate = c.din("gate", [NT, 6144], BF16)
    x = c.din("x", [NT, 2048])
    w_branch = c.din("w_branch", [3, 1024, 2048])
    w_out = c.din("w_out", [2048, 2048])
    gt1 = c.din("gt1", [2, 2048])
    g = c.din("g", [1, 2048])
    sc = c.din("sc", [2, 2048])
    sh = c.din("sh", [2, 2048])
    w_r = c.din("w_r", [2048, 32])
    b_r = c.din("b_r", [1, 32])
    ident = c.din("ident", [128, 128])
    xo = c.dout("xo", [NT, 2048])
    n2o = c.dout("n2", [NT, 2048], BF16)
    rwo = c.dout("rw", [NT, 32])
    mTd = c.dscratch("mTd", [2048, NT], BF16)

    idf = c.sb("idf", [128, 128])
    idb = c.sb("idb", [128, 128], BF16)
    epsb = c.sb("epsb", [128, 1])
    wbig = c.sb("wbig", [128, 16 * 2048], BF16)
    gtile = Rot([(c.sb(f"gtl{i}", [128, 3, 1024], BF16), f"gtl{i}") for i in range(2)])
    otile = Rot([(c.sb(f"otl{i}", [128, 24, 128], BF16), f"otl{i}") for i in range(2)])
    S1 = c.sb("S1", [128, 2048])
    S2 = c.sb("S2", [128, 2048])
    S3 = c.sb("S3", [128, 2048])
    S4 = c.sb("S4", [128, 2048])
    mb = c.sb("mb", [128, 1024], BF16)
    mT = Rot([(c.sb(f"mT{i}", [128, 16, 128], BF16), f"mT{i}") for i in range(2)])
    G1 = c.sb("G1", [128, 2048])
    A2 = c.sb("A2", [128, 2048])
    Sh2 = c.sb("Sh2", [128, 2048])
    gf = c.sb("gf", [128, 2048])
    n2b = c.sb("n2b", [128, 2048], BF16)
    n2T = c.sb("n2T", [128, 16, 128])
    wr = c.sb("wr", [128, 16, 32])
    br = c.sb("br", [128, 32])
    small = Rot([(c.sb(f"sm{i}", [128, 48]), f"sm{i}") for i in range(3)])
    lg = c.sb("lg", [128, 32])
    ee = c.sb("ee", [128, 32])
    psm = Rot([(c.ps(f"psm{i}"), f"psm{i}") for i in range(4)])
    pst = Rot([(c.ps(f"pst{i}"), f"pst{i}") for i in range(2)])
    psl = c.ps("psl")

    a.op('dve', lambda e: e.memset(epsb[:], EPS), writes=['epsb'])
    a.dma('sp', idf[:], ident, writes=['idf'])
    a.op('dve', lambda e: e.tensor_copy(out=idb[:], in_=idf[:]), reads=['idf'], writes=['idb'])
    a.dma('sp', gf[:], g.broadcast_to([128, 2048]), writes=['gf'])
    a.dma('sp', wr[:], w_r.rearrange("(k p) n -> p k n", p=128), writes=['wr'])
    a.dma('sp', br[:], b_r.broadcast_to([128, 32]), writes=['br'])

    for half in range(2):
        wv = wbig[:, 0:24 * 1024].rearrange("p (k n) -> p k n", k=24)
        for i in range(3):
            a.dma('pool', wv[:, i * 8:(i + 1) * 8, :],
                  w_branch[i, :, half * 1024:(half + 1) * 1024].rearrange("(k p) n -> p k n", p=128), writes=['wbig'])
        for tt in range(NTT):
            ot_, otk = otile.next()
            a.dma('sp', ot_[:].rearrange("p (i k) t -> p i k t", i=3),
                  oT[:, :, tt * 128:(tt + 1) * 128].rearrange("i (k p) t -> p i k t", p=128), writes=[otk])
            gt_, gtk = gtile.next()
            a.dma('act', gt_[:], gate[tt * 128:(tt + 1) * 128, :].rearrange("t (i n) -> t i n", i=3)[:, :, half * 1024:(half + 1) * 1024],
                  writes=[gtk])
            for n0 in range(0, 1024, 512):
                for i in range(3):
                    p, pk = psm.next()
                    for k in range(8):
                        a.op('pe', lambda e, p=p, k=k, i=i, n0=n0, ot_=ot_: e.matmul(p[:, :], lhsT=ot_[:, i * 8 + k, :],
                                                                                     rhs=wv[:, i * 8 + k, n0:n0 + 512],
                                                                                     start=(k == 0), stop=(k == 7)),
                             reads=[otk, 'wbig'], writes=[pk])
                    if i == 0:
                        a.op('dve', lambda e, p=p, n0=n0, gt_=gt_: e.tensor_tensor(out=S1[:, n0:n0 + 512], in0=p[:, :],
                                                                                   in1=gt_[:, 0, n0:n0 + 512], op=ALU.mult),
                             reads=[pk, gtk], writes=['S1'])
                    else:
                        a.op('dve', lambda e, p=p, n0=n0, i=i, gt_=gt_: e.tensor_tensor(out=S2[:, n0:n0 + 512], in0=p[:, :],
                                                                                        in1=gt_[:, i, n0:n0 + 512], op=ALU.mult),
                             reads=[pk, gtk], writes=['S2'])
                        if i == 1:
                            a.op('pool', lambda e, n0=n0: e.tensor_tensor(out=S1[:, n0:n0 + 512], in0=S1[:, n0:n0 + 512],
                                                                          in1=S2[:, n0:n0 + 512], op=ALU.add),
                                 reads=['S1', 'S2'], writes=['S1'])
                        else:
                            a.op('pool', lambda e, n0=n0: e.tensor_tensor(out=mb[:, n0:n0 + 512], in0=S1[:, n0:n0 + 512],
                                                                          in1=S2[:, n0:n0 + 512], op=ALU.add),
                                 reads=['S1', 'S2'], writes=['mb'])
            mt_, mtk = mT.next()
            for q4 in range(2):
                p, pk = pst.next()
                for j in range(4):
                    kc = q4 * 4 + j
                    a.op('pe', lambda e, p=p, j=j, kc=kc: e.matmul(p[:, j * 128:(j + 1) * 128], lhsT=mb[:, kc * 128:(kc + 1) * 128],
                                                                   rhs=idb[:], start=True, stop=True), reads=['mb', 'idb'], writes=[pk])
                a.op('act', lambda e, p=p, q4=q4, mt_=mt_: e.copy(out=mt_[:, q4 * 4:(q4 + 1) * 4, :],
                                                                  in_=p[:].rearrange("p (j t) -> p j t", j=4)), reads=[pk], writes=[mtk])
            a.dma('sp', mTd[half * 1024:(half + 1) * 1024, tt * 128:(tt + 1) * 128].rearrange("(k p) t -> p k t", p=128),
                  mt_[:, 0:8, :], reads=[mtk], writes=[('mTd', tt, half)])

    wv2 = wbig[:].rearrange("p (k n) -> p k n", k=16)
    a.dma('pool', wv2[:, 0:8, :], w_out[0:1024, :].rearrange("(k p) n -> p k n", p=128), writes=['wbig'])
    a.dma('pool', wv2[:, 8:16, :], w_out[1024:2048, :].rearrange("(k p) n -> p k n", p=128), writes=['wbig'])

    def load_group(gi):
        a.dma('sp', G1[:], gt1[gi:gi + 1, :].broadcast_to([128, 2048]), writes=['G1'])
        a.dma('sp', A2[:], sc[gi:gi + 1, :].broadcast_to([128, 2048]), writes=['A2'])
        a.dma('sp', Sh2[:], sh[gi:gi + 1, :].broadcast_to([128, 2048]), writes=['Sh2'])
        a.op('dve', lambda e: e.scalar_tensor_tensor(out=A2[:], in0=A2[:], scalar=1.0, in1=gf[:], op0=ALU.add,
                                                     op1=ALU.mult), reads=['A2', 'gf'], writes=['A2'])

    for tt in range(NTT):
        if tt == 0:
            load_group(0)
        elif tt == g0_tiles:
            load_group(1)
        mt_, mtk = mT.next()
        a.dma('sp', mt_[:], mTd[:, tt * 128:(tt + 1) * 128].rearrange("(k p) t -> p k t", p=128),
              reads=[('mTd', tt, 0), ('mTd', tt, 1)], writes=[mtk])
        a.dma('act', S1[:], x[tt * 128:(tt + 1) * 128, :], writes=['S1'])
        for n0 in range(0, 2048, 512):
            p, pk = psm.next()
            for k in range(16):
                a.op('pe', lambda e, p=p, k=k, n0=n0, mt_=mt_: e.matmul(p[:, :], lhsT=mt_[:, k, :], rhs=wv2[:, k, n0:n0 + 512],
                                                                        start=(k == 0), stop=(k == 15)),
                     reads=[mtk, 'wbig'], writes=[pk])
            a.op('dve', lambda e, p=p, n0=n0: e.tensor_tensor(out=S2[:, n0:n0 + 512], in0=p[:, :], in1=G1[:, n0:n0 + 512],
                                                              op=ALU.mult), reads=[pk, 'G1'], writes=['S2'])
        a.op('pool', lambda e: e.tensor_tensor(out=S1[:], in0=S1[:], in1=S2[:], op=ALU.add), reads=['S1', 'S2'], writes=['S1'])
        a.dma('sp', xo[tt * 128:(tt + 1) * 128, :], S1[:], reads=['S1'], writes=[('xo', tt)])
        sm, smk = small.next()
        a.op('dve', lambda e, sm=sm: e.memset(sm[:], 0.0), writes=[smk])
        a.op('act', lambda e, sm=sm: e.activation(out=S4[:], in_=S1[:], func=AF.Square, accum_out=sm[:, 0:1]),
             reads=['S1', smk], writes=['S4', smk])
        a.op('act', lambda e, sm=sm: e.activation(out=sm[:, 1:2], in_=sm[:, 0:1], func=AF.Sqrt, bias=epsb[:, 0:1], scale=1.0 / 2048),
             reads=[smk, 'epsb'], writes=[smk])
        a.op('dve', lambda e, sm=sm: e.reciprocal(out=sm[:, 1:2], in_=sm[:, 1:2]), reads=[smk], writes=[smk])
        a.op('dve', lambda e, sm=sm: e.scalar_tensor_tensor(out=S3[:], in0=S1[:], scalar=sm[:, 1:2], in1=A2[:], op0=ALU.mult,
                                                            op1=ALU.mult), reads=['S1', smk, 'A2'], writes=['S3'])
        a.op('pool', lambda e: e.tensor_tensor(out=S3[:], in0=S3[:], in1=Sh2[:], op=ALU.add), reads=['S3', 'Sh2'], writes=['S3'])
        a.op('act', lambda e: e.copy(out=n2b[:], in_=S3[:]), reads=['S3'], writes=['n2b'])
        a.dma('act', n2o[tt * 128:(tt + 1) * 128, :], n2b[:], reads=['n2b'], writes=[('n2o', tt)])
        for q4 in range(4):
            p, pk = pst.next()
            for j in range(4):
                kc = q4 * 4 + j
                a.op('pe', lambda e, p=p, j=j, kc=kc: e.matmul(p[:, j * 128:(j + 1) * 128], lhsT=S3[:, kc * 128:(kc + 1) * 128],
                                                               rhs=idf[:], start=True, stop=True), reads=['S3', 'idf'], writes=[pk])
            a.op('dve' if q4 % 2 else 'act',
                 (lambda e, p=p, q4=q4: e.tensor_copy(out=n2T[:, q4 * 4:(q4 + 1) * 4, :], in_=p[:].rearrange("p (j t) -> p j t", j=4)))
                 if q4 % 2 else
                 (lambda e, p=p, q4=q4: e.copy(out=n2T[:, q4 * 4:(q4 + 1) * 4, :], in_=p[:].rearrange("p (j t) -> p j t", j=4))),
                 reads=[pk], writes=['n2T'])
        for k in range(16):
            a.op('pe', lambda e, k=k: e.matmul(psl[:, 0:32], lhsT=n2T[:, k, :], rhs=wr[:, k, :], start=(k == 0), stop=(k == 15)),
                 reads=['n2T', 'wr'], writes=['psl'])
        a.op('dve', lambda e: e.tensor_tensor(out=lg[:], in0=psl[:, 0:32], in1=br[:], op=ALU.add), reads=['psl', 'br'], writes=['lg'])
        sm, smk = small.next()
        a.op('dve', lambda e, sm=sm: e.max(out=sm[:, 0:8], in_=lg[:]), reads=['lg'], writes=[smk])
        a.op('dve', lambda e, sm=sm: e.tensor_scalar(out=sm[:, 8:9], in0=sm[:, 0:1], scalar1=-1.0, scalar2=None, op0=ALU.mult),
             reads=[smk], writes=[smk])
        a.op('act', lambda e, sm=sm: e.activation(out=ee[:], in_=lg[:], func=AF.Exp, bias=sm[:, 8:9], scale=1.0),
             reads=['lg', smk], writes=['ee'])
        a.op('dve', lambda e, sm=sm: e.tensor_scalar(out=lg[:], in0=lg[:], scalar1=sm[:, 3:4], scalar2=None, op0=ALU.is_ge),
             reads=['lg', smk], writes=['lg'])
        a.op('dve', lambda e: e.tensor_tensor(out=ee[:], in0=ee[:], in1=lg[:], op=ALU.mult), reads=['ee', 'lg'], writes=['ee'])
        a.op('dve', lambda e, sm=sm: e.tensor_reduce(out=sm[:, 9:10], in_=ee[:], axis=AX.X, op=ALU.add), reads=['ee'], writes=[smk])
        a.op('dve', lambda e, sm=sm: e.reciprocal(out=sm[:, 9:10], in_=sm[:, 9:10]), reads=[smk], writes=[smk])
        a.op('dve', lambda e, sm=sm: e.tensor_scalar(out=sm[:, 16:48], in0=ee[:], scalar1=sm[:, 9:10], scalar2=None, op0=ALU.mult),
             reads=['ee', smk], writes=[smk])
        a.dma('sp', rwo[tt * 128:(tt + 1) * 128, :], sm[:, 16:48], reads=[smk], writes=[('rwo', tt)])
    return c.done()


def build_D(NTOK, NE=4, TB=1024):
    c = Ctx()
    a = c.a
    n2T = c.din("n2T", [2048, NTOK], BF16)
    rw = c.din("rw", [NTOK, NE])
    w1 = c.din("w1", [NE, 2048, 4096])
    b1 = c.din("b1", [NE, 4096])
    w2 = c.din("w2", [NE, 2048, 2048])
    b2 = c.din("b2", [NE, 2048])
    ident = c.din("ident", [128, 128])
    yo = c.dout("y", [NTOK, 2048], BF16)
    NTT = TB // 128

    idf = c.sb("idf", [128, 128])
    idb = c.sb("idb", [128, 128], BF16)
    a.dma('sp', idf[:], ident, writes=['idf'])
    a.op('dve', lambda e: e.tensor_copy(out=idb[:], in_=idf[:]), reads=['idf'], writes=['idb'])
    nt = Rot([(c.sb(f"nt{i}", [128, 16, TB], BF16), f"nt{i}") for i in range(1 if TB > 512 else 2)])
    yacc = c.sb("yacc", [128, NTT, 2048])
    aT = c.sb("aT", [128, 16, TB], BF16)
    wt = Rot([(c.sb(f"wt{i}", [128, 16, 512], BF16), f"wt{i}") for i in range(2)])
    bt = Rot([(c.sb(f"bt{i}", [128, 512]), f"bt{i}") for i in range(2)])
    rwt = Rot([(c.sb(f"rwt{i}", [128, NTT, NE]), f"rwt{i}") for i in range(2)])
    hS = Rot([(c.sb(f"hS{i}", [128, 512]), f"hS{i}") for i in range(2)])
    hG = Rot([(c.sb(f"hG{i}", [128, 256]), f"hG{i}") for i in range(2)])
    hL = Rot([(c.sb(f"hL{i}", [128, 256]), f"hL{i}") for i in range(2)])
    hZ = Rot([(c.sb(f"hZ{i}", [128, 256]), f"hZ{i}") for i in range(2)])
    hA = Rot([(c.sb(f"hA{i}", [128, 256], BF16), f"hA{i}") for i in range(2)])
    stg = Rot([(c.sb(f"stg{i}", [128, 2048], BF16), f"stg{i}") for i in range(2)])
    psm = Rot([(c.ps(f"psm{i}"), f"psm{i}") for i in range(4)])
    pst = Rot([(c.ps(f"pst{i}"), f"pst{i}") for i in range(2)])

    for blk in range(NTOK // TB):
        t0 = blk * TB
        ntile, ntk = nt.next()
        a.dma('sp', ntile[:], n2T[:, t0:t0 + TB].rearrange("(k p) t -> p k t", p=128), writes=[ntk])
        rwtile, rwk = rwt.next()
        a.dma('sp', rwtile[:], rw[t0:t0 + TB, :].rearrange("(j p) e -> p j e", p=128), writes=[rwk])
        for ex in range(NE):
            for ct in range(8):
                wtile, wk = wt.next()
                a.dma('pool', wtile[:], w1[ex, :, ct * 512:(ct + 1) * 512].rearrange("(k p) n -> p k n", p=128), writes=[wk])
                btile, bk = bt.next()
                a.dma('act', btile[:], b1[ex:ex + 1, ct * 512:(ct + 1) * 512].broadcast_to([128, 512]), writes=[bk])
                for tt in range(NTT):
                    p, pk = psm.next()
                    for k in range(16):
                        a.op('pe', lambda e, p=p, k=k, tt=tt, ntile=ntile, wtile=wtile: e.matmul(
                            p[:, :], lhsT=ntile[:, k, tt * 128:(tt + 1) * 128], rhs=wtile[:, k, :], start=(k == 0), stop=(k == 15)),
                            reads=[ntk, wk], writes=[pk])
                    S, Sk = hS.next()
                    G, Gk = hG.next()
                    L, Lk = hL.next()
                    Z, Zk = hZ.next()
                    A_, Ak = hA.next()
                    a.op('dve', lambda e, p=p, S=S, btile=btile: e.tensor_tensor(out=S[:], in0=p[:, :], in1=btile[:], op=ALU.add),
                         reads=[pk, bk], writes=[Sk])
                    Sv = S[:].rearrange("p (n two) -> p n two", two=2)
                    a.op('dve', lambda e, Sv=Sv, G=G: e.tensor_scalar(out=G[:], in0=Sv[:, :, 0], scalar1=7.0, scalar2=None, op0=ALU.min),
                         reads=[Sk], writes=[Gk])
                    a.op('act', lambda e, G=G, Z=Z: e.activation(out=Z[:], in_=G[:], func=AF.Sigmoid, scale=1.702),
                         reads=[Gk], writes=[Zk])
                    a.op('dve', lambda e, Sv=Sv, L=L: e.tensor_scalar(out=L[:], in0=Sv[:, :, 1], scalar1=7.0, scalar2=-7.0, op0=ALU.min,
                                                                      op1=ALU.max), reads=[Sk], writes=[Lk])
                    a.op('dve', lambda e, L=L, G=G: e.scalar_tensor_tensor(out=L[:], in0=L[:], scalar=1.0, in1=G[:], op0=ALU.add,
                                                                           op1=ALU.mult), reads=[Lk, Gk], writes=[Lk])
                    a.op('dve', lambda e, L=L, Z=Z, A_=A_: e.tensor_tensor(out=A_[:], in0=L[:], in1=Z[:], op=ALU.mult),
                         reads=[Lk, Zk], writes=[Ak])
                    pt_, ptk = pst.next()
                    for j in range(2):
                        a.op('pe', lambda e, pt_=pt_, j=j, A_=A_: e.matmul(pt_[:, j * 128:(j + 1) * 128], lhsT=A_[:, j * 128:(j + 1) * 128],
                                                                            rhs=idb[:], start=True, stop=True), reads=[Ak, 'idb'], writes=[ptk])
                    a.op('act', lambda e, pt_=pt_, ct=ct, tt=tt: e.copy(out=aT[:, ct * 2:ct * 2 + 2, tt * 128:(tt + 1) * 128],
                                                                        in_=pt_[:, 0:256].rearrange("p (j t) -> p j t", j=2)),
                         reads=[ptk], writes=[('aT', tt)])
            for ct in range(4):
                wtile, wk = wt.next()
                a.dma('pool', wtile[:], w2[ex, :, ct * 512:(ct + 1) * 512].rearrange("(k p) n -> p k n", p=128), writes=[wk])
                btile, bk = bt.next()
                a.dma('act', btile[:], b2[ex:ex + 1, ct * 512:(ct + 1) * 512].broadcast_to([128, 512]), writes=[bk])
                for tt in range(NTT):
                    p, pk = psm.next()
                    for k in range(16):
                        a.op('pe', lambda e, p=p, k=k, tt=tt, wtile=wtile: e.matmul(
                            p[:, :], lhsT=aT[:, k, tt * 128:(tt + 1) * 128], rhs=wtile[:, k, :], start=(k == 0), stop=(k == 15)),
                            reads=[('aT', tt), wk], writes=[pk])
                    S, Sk = hS.next()
                    a.op('dve', lambda e, p=p, S=S, btile=btile: e.tensor_tensor(out=S[:], in0=p[:, :], in1=btile[:], op=ALU.add),
                         reads=[pk, bk], writes=[Sk])
                    ys = yacc[:, tt, ct * 512:(ct + 1) * 512]
                    if ex == 0:
                        a.op('dve', lambda e, S=S, ys=ys, tt=tt, rwtile=rwtile: e.tensor_scalar(
                            out=ys, in0=S[:], scalar1=rwtile[:, tt, 0:1], scalar2=None, op0=ALU.mult),
                            reads=[Sk, rwk], writes=[('yacc', tt, ct)])
                    else:
                        a.op('dve', lambda e, S=S, ys=ys, tt=tt, ex=ex, rwtile=rwtile: e.scalar_tensor_tensor(
                            out=ys, in0=S[:], scalar=rwtile[:, tt, ex:ex + 1], in1=ys, op0=ALU.mult, op1=ALU.add),
                            reads=[Sk, rwk, ('yacc', tt, ct)], writes=[('yacc', tt, ct)])
        for tt in range(NTT):
            st_, stk = stg.next()
            a.op('act', lambda e, st_=st_, tt=tt: e.copy(out=st_[:], in_=yacc[:, tt, :]), reads=[('yacc', tt, ct) for ct in range(4)],
                 writes=[stk])
            a.dma('sp', yo[t0 + tt * 128:t0 + (tt + 1) * 128, :], st_[:], reads=[stk], writes=[('yo', blk, tt)])
    return c.done()


def build_F(NT, g0_tiles, NP=8, ctx=None):
    c = ctx or Ctx()
    a = c.a
    x = c.din("x", [NT, 2048])
    parts = c.din("parts", [NP, NT, 2048], BF16)
    gt2 = c.din("gt2", [2, 2048])
    gfin = c.din("gfin", [1, 2048])
    xo = c.dout("xo", [NT, 2048])
    fo = c.dout("fo", [NT, 2048])
    epsb = c.sb("epsb", [128, 1])
    a.op('dve', lambda e: e.memset(epsb[:], EPS), writes=['epsb'])
    G2 = c.sb("G2", [128, 2048])
    GF = c.sb("GF", [128, 2048])
    a.dma('sp', GF[:], gfin.broadcast_to([128, 2048]), writes=['GF'])
    xt = Rot([(c.sb(f"xt{i}", [128, 2048]), f"xt{i}") for i in range(2)])
    pt = Rot([(c.sb(f"pt{i}", [128, NP, 2048], BF16), f"pt{i}") for i in range(2)])
    acc = c.sb("acc", [128, 2048])
    scr = c.sb("scr", [128, 2048])
    fo_t = Rot([(c.sb(f"fo{i}", [128, 2048]), f"fo{i}") for i in range(2)])
    small = Rot([(c.sb(f"sm{i}", [128, 4]), f"sm{i}") for i in range(2)])
    for tt in range(NT // 128):
        if tt == 0 or tt == g0_tiles:
            gi = 0 if tt == 0 else 1
            a.dma('sp', G2[:], gt2[gi:gi + 1, :].broadcast_to([128, 2048]), writes=['G2'])
        xtile, xk = xt.next()
        a.dma('sp', xtile[:], x[tt * 128:(tt + 1) * 128, :], writes=[xk])
        ptile, pk = pt.next()
        a.dma('act', ptile[:], parts[:, tt * 128:(tt + 1) * 128, :].rearrange("c t d -> t c d"), writes=[pk])
        a.op('dve', lambda e, ptile=ptile: e.tensor_tensor(out=acc[:], in0=ptile[:, 0, :], in1=ptile[:, 1, :], op=ALU.add),
             reads=[pk], writes=['acc'])
        for j in range(2, NP):
            a.op('dve', lambda e, ptile=ptile, j=j: e.tensor_tensor(out=acc[:], in0=acc[:], in1=ptile[:, j, :], op=ALU.add),
                 reads=[pk, 'acc'], writes=['acc'])
        a.op('pool', lambda e: e.tensor_tensor(out=acc[:], in0=acc[:], in1=G2[:], op=ALU.mult), reads=['acc', 'G2'], writes=['acc'])
        a.op('pool', lambda e, xtile=xtile: e.tensor_tensor(out=xtile[:], in0=xtile[:], in1=acc[:], op=ALU.add),
             reads=[xk, 'acc'], writes=[xk])
        a.dma('sp', xo[tt * 128:(tt + 1) * 128, :], xtile[:], reads=[xk], writes=[('xo', tt)])
        sm, smk = small.next()
        a.op('dve', lambda e, sm=sm: e.memset(sm[:], 0.0), writes=[smk])
        a.op('act', lambda e, sm=sm, xtile=xtile: e.activation(out=scr[:], in_=xtile[:], func=AF.Square, accum_out=sm[:, 0:1]),
             reads=[xk, smk], writes=['scr', smk])
        a.op('act', lambda e, sm=sm: e.activation(out=sm[:, 1:2], in_=sm[:, 0:1], func=AF.Sqrt, bias=epsb[:, 0:1], scale=1.0 / 2048),
             reads=[smk, 'epsb'], writes=[smk])
        a.op('dve', lambda e, sm=sm: e.reciprocal(out=sm[:, 1:2], in_=sm[:, 1:2]), reads=[smk], writes=[smk])
        ft, fk = fo_t.next()
        a.op('dve', lambda e, sm=sm, xtile=xtile, ft=ft: e.scalar_tensor_tensor(out=ft[:], in0=xtile[:], scalar=sm[:, 1:2], in1=GF[:],
                                                                                op0=ALU.mult, op1=ALU.mult),
             reads=[xk, smk, 'GF'], writes=[fk])
        a.dma('act', fo[tt * 128:(tt + 1) * 128, :], ft[:], reads=[fk], writes=[('fo', tt)])
    return c.done()


CAP = 384


def build_D2(NTOK, NE=4, GPB=4):
    c = Ctx()
    a = c.a
    NTILE = NTOK // 128
    NG = NTILE // 8
    assert NG * 8 == NTILE
    n2 = c.din("n2", [NTOK, 2048], BF16)
    rw = c.din("rw", [NTOK, NE])
    w1 = c.din("w1", [NE, 2048, 4096])
    b1 = c.din("b1", [NE, 4096])
    w2 = c.din("w2", [NE, 2048, 2048])
    b2 = c.din("b2", [NE, 2048])
    ident = c.din("ident", [128, 128])
    utri = c.din("utri", [128, 128])
    iota_d = c.din("iota", [128, CAP])
    yo = c.dout("y", [NTOK, 2048], BF16)
    y2d = c.dscratch("y2d", [NE, NG * CAP, 2048], BF16)
    NCH = CAP // 128
    MAXSL = GPB * CAP

    idf = c.sb("idf", [128, 128])
    idb = c.sb("idb", [128, 128], BF16)
    ub = c.sb("ub", [128, 128], BF16)
    onesb = c.sb("onesb", [128, 128], BF16)
    iot = c.sb("iot", [128, CAP])
    a.dma('sp', idf[:], ident, writes=['idf'])
    a.op('dve', lambda e: e.tensor_copy(out=idb[:], in_=idf[:]), reads=['idf'], writes=['idb'])
    a.dma('sp', idf[:], utri, reads=['idf'], writes=['idf'])
    a.op('dve', lambda e: e.tensor_copy(out=ub[:], in_=idf[:]), reads=['idf'], writes=['ub'])
    a.op('dve', lambda e: e.memset(onesb[:], 1.0), writes=['onesb'])
    a.dma('sp', iot[:], iota_d, writes=['iot'])
    rwt = c.sb("rwt", [128, NTILE, NE])
    mt = c.sb("mt", [128, NTILE, NE])
    mbf = c.sb("mbf", [128, NTILE, NE], BF16)
    gpos = c.sb("gpos", [128, NTILE, NE])
    XT = c.sb("XT", [128, 16, MAXSL], BF16)
    aT = c.sb("aT", [128, 16 * MAXSL], BF16)
    aTv = aT[:].rearrange("p (k s) -> p k s", k=16)
    Y2v = aT[:].rearrange("p (q d) -> p q d", d=2048)
    n2g = c.sb("n2g", [128, 8, 2048], BF16)
    selg = c.sb("selg", [128, 8, CAP], BF16)
    wt = Rot([(c.sb(f"wt{i}", [128, 16, 512], BF16), f"wt{i}") for i in range(2)])
    bt = Rot([(c.sb(f"bt{i}", [128, 512]), f"bt{i}") for i in range(2)])
    hS = Rot([(c.sb(f"hS{i}", [128, 512]), f"hS{i}") for i in range(2)])
    hG = Rot([(c.sb(f"hG{i}", [128, 256]), f"hG{i}") for i in range(2)])
    hL = Rot([(c.sb(f"hL{i}", [128, 256]), f"hL{i}") for i in range(2)])
    hZ = Rot([(c.sb(f"hZ{i}", [128, 256]), f"hZ{i}") for i in range(2)])
    hA = Rot([(c.sb(f"hA{i}", [128, 256], BF16), f"hA{i}") for i in range(2)])
    y2s = Rot([(c.sb(f"y2s{i}", [128, 512], BF16), f"y2s{i}") for i in range(3)])
    selw = Rot([(c.sb(f"selw{i}", [128, CAP], BF16), f"selw{i}") for i in range(2)])
    selT = Rot([(c.sb(f"selT{i}", [128, NE, NCH, 128], BF16), f"selT{i}") for i in range(2)])
    stg = Rot([(c.sb(f"stg{i}", [128, 2048], BF16), f"stg{i}") for i in range(1)])
    psm = Rot([(c.ps(f"psm{i}"), f"psm{i}") for i in range(4)])
    pst = Rot([(c.ps(f"pst{i}"), f"pst{i}") for i in range(2)])

    a.dma('sp', rwt[:], rw.rearrange("(j p) e -> p j e", p=128), writes=['rwt'])
    a.op('dve', lambda e: e.tensor_scalar(out=mt[:], in0=rwt[:], scalar1=0.0, scalar2=None, op0=ALU.is_gt), reads=['rwt'], writes=['mt'])
    a.op('dve', lambda e: e.tensor_copy(out=mbf[:], in_=mt[:]), reads=['mt'], writes=['mbf'])
    for g in range(NG):
        p, pk = pst.next()
        for j in range(8):
            for i in range(j + 1):
                a.op('pe', lambda e, p=p, j=j, i=i, g=g: e.matmul(p[:, j * NE:(j + 1) * NE], lhsT=(ub[:] if i == j else onesb[:]),
                                                                 rhs=mbf[:, g * 8 + i, :], start=(i == 0), stop=(i == j)),
                     reads=['ub', 'onesb', 'mbf'], writes=[pk])
        a.op('act', lambda e, p=p, g=g: e.copy(out=gpos[:, g * 8:(g + 1) * 8, :], in_=p[:, 0:8 * NE].rearrange("p (j e) -> p j e", j=8)),
             reads=[pk], writes=['gpos'])

    blocks = [list(range(b0, min(b0 + GPB, NG))) for b0 in range(0, NG, GPB)]
    for groups in blocks:
        nsl = len(groups) * CAP
        nst = nsl // 128
        for ex in range(NE):
            for gi, g in enumerate(groups):
                a.dma('act', n2g[:], n2[g * 1024:(g + 1) * 1024, :].rearrange("(j p) d -> p j d", p=128), writes=['n2g'])
                for j in range(8):
                    tj = g * 8 + j
                    a.op('dve', lambda e, j=j, tj=tj, ex=ex: e.tensor_scalar(out=selg[:, j, :], in0=iot[:], scalar1=gpos[:, tj, ex:ex + 1],
                                                                             scalar2=mt[:, tj, ex:ex + 1], op0=ALU.is_equal, op1=ALU.mult),
                         reads=['iot', 'gpos', 'mt'], writes=[('selg', j)])
                for dcg in range(4):
                    ps4 = [psm.next() for _ in range(4)]
                    for j in range(8):
                        for q in range(4):
                            p, pk = ps4[q]
                            dc = dcg * 4 + q
                            a.op('pe', lambda e, p=p, j=j, dc=dc: e.matmul(p[:, 0:CAP], lhsT=n2g[:, j, dc * 128:(dc + 1) * 128], rhs=selg[:, j, :],
                                                                          start=(j == 0), stop=(j == 7)),
                                 reads=['n2g', ('selg', j)], writes=[pk])
                    for q in range(4):
                        p, pk = ps4[q]
                        dc = dcg * 4 + q
                        dst = XT[:, dc, gi * CAP:(gi + 1) * CAP]
                        if q % 2 == 0:
                            a.op('act', lambda e, p=p, dst=dst: e.copy(out=dst, in_=p[:, 0:CAP]), reads=[pk], writes=[('XT', gi)])
                        else:
                            a.op('dve', lambda e, p=p, dst=dst: e.tensor_copy(out=dst, in_=p[:, 0:CAP]), reads=[pk], writes=[('XT', gi)])
            for ct in range(8):
                wtile, wk = wt.next()
                a.dma('pool', wtile[:], w1[ex, :, ct * 512:(ct + 1) * 512].rearrange("(k p) n -> p k n", p=128), writes=[wk])
                btile, bk = bt.next()
                a.dma('act', btile[:], b1[ex:ex + 1, ct * 512:(ct + 1) * 512].broadcast_to([128, 512]), writes=[bk])
                for st in range(nst):
                    gi = (st * 128) // CAP
                    p, pk = psm.next()
                    for k in range(16):
                        a.op('pe', lambda e, p=p, k=k, st=st, wtile=wtile: e.matmul(
                            p[:, :], lhsT=XT[:, k, st * 128:(st + 1) * 128], rhs=wtile[:, k, :], start=(k == 0), stop=(k == 15)),
                            reads=[('XT', gi), wk], writes=[pk])
                    S, Sk = hS.next()
                    G, Gk = hG.next()
                    L, Lk = hL.next()
                    Z, Zk = hZ.next()
                    A_, Ak = hA.next()
                    a.op('dve', lambda e, p=p, S=S, btile=btile: e.tensor_tensor(out=S[:], in0=p[:, :], in1=btile[:], op=ALU.add),
                         reads=[pk, bk], writes=[Sk])
                    Sv = S[:].rearrange("p (n two) -> p n two", two=2)
                    a.op('dve', lambda e, Sv=Sv, G=G: e.tensor_scalar(out=G[:], in0=Sv[:, :, 0], scalar1=7.0, scalar2=None, op0=ALU.min),
                         reads=[Sk], writes=[Gk])
                    a.op('act', lambda e, G=G, Z=Z: e.activation(out=Z[:], in_=G[:], func=AF.Sigmoid, scale=1.702),
                         reads=[Gk], writes=[Zk])
                    a.op('dve', lambda e, Sv=Sv, L=L: e.tensor_scalar(out=L[:], in0=Sv[:, :, 1], scalar1=7.0, scalar2=-7.0, op0=ALU.min,
                                                                      op1=ALU.max), reads=[Sk], writes=[Lk])
                    a.op('dve', lambda e, L=L, G=G: e.scalar_tensor_tensor(out=L[:], in0=L[:], scalar=1.0, in1=G[:], op0=ALU.add,
                                                                           op1=ALU.mult), reads=[Lk, Gk], writes=[Lk])
                    a.op('dve', lambda e, L=L, Z=Z, A_=A_: e.tensor_tensor(out=A_[:], in0=L[:], in1=Z[:], op=ALU.mult),
                         reads=[Lk, Zk], writes=[Ak])
                    pt_, ptk = pst.next()
                    for j in range(2):
                        a.op('pe', lambda e, pt_=pt_, j=j, A_=A_: e.matmul(pt_[:, j * 128:(j + 1) * 128], lhsT=A_[:, j * 128:(j + 1) * 128],
                                                                            rhs=idb[:], start=True, stop=True), reads=[Ak, 'idb'], writes=[ptk])
                    a.op('act', lambda e, pt_=pt_, ct=ct, st=st: e.copy(out=aTv[:, ct * 2:ct * 2 + 2, st * 128:(st + 1) * 128],
                                                                        in_=pt_[:, 0:256].rearrange("p (j t) -> p j t", j=2)),
                         reads=[ptk], writes=['aT'])
            for ct in range(4):
                wtile, wk = wt.next()
                a.dma('pool', wtile[:], w2[ex, :, ct * 512:(ct + 1) * 512].rearrange("(k p) n -> p k n", p=128), writes=[wk])
                btile, bk = bt.next()
                a.dma('act', btile[:], b2[ex:ex + 1, ct * 512:(ct + 1) * 512].broadcast_to([128, 512]), writes=[bk])
                for st in range(nst):
                    p, pk = psm.next()
                    for k in range(16):
                        a.op('pe', lambda e, p=p, k=k, st=st, wtile=wtile: e.matmul(
                            p[:, :], lhsT=aTv[:, k, st * 128:(st + 1) * 128], rhs=wtile[:, k, :], start=(k == 0), stop=(k == 15)),
                            reads=['aT', wk], writes=[pk])
                    ys, ysk = y2s.next()
                    a.op('dve', lambda e, p=p, ys=ys, btile=btile: e.tensor_tensor(out=ys[:], in0=p[:, :], in1=btile[:], op=ALU.add),
                         reads=[pk, bk], writes=[ysk])
                    srow = groups[0] * CAP + st * 128
                    a.dma('sp', y2d[ex, srow:srow + 128, ct * 512:(ct + 1) * 512], ys[:], reads=[ysk], writes=[('y2d', ex, srow // CAP)])
        for gi, g in enumerate(groups):
            for ex in range(NE):
                a.dma('sp', Y2v[:, ex * NCH:(ex + 1) * NCH, :], y2d[ex, g * CAP:(g + 1) * CAP, :].rearrange("(c p) d -> p c d", p=128),
                      reads=[('y2d', ex, g)], writes=['aT'])
            for j in range(8):
                tj = g * 8 + j
                sT, sTk = selT.next()
                for ex in range(NE):
                    sw, swk = selw.next()
                    a.op('dve', lambda e, sw=sw, tj=tj, ex=ex: e.tensor_scalar(out=sw[:], in0=iot[:], scalar1=gpos[:, tj, ex:ex + 1],
                                                                               scalar2=rwt[:, tj, ex:ex + 1], op0=ALU.is_equal, op1=ALU.mult),
                         reads=['iot', 'gpos', 'rwt'], writes=[swk])
                    pt_, ptk = pst.next()
                    for cc in range(NCH):
                        a.op('pe', lambda e, pt_=pt_, cc=cc, sw=sw: e.matmul(pt_[:, cc * 128:(cc + 1) * 128], lhsT=sw[:, cc * 128:(cc + 1) * 128],
                                                                              rhs=idb[:], start=True, stop=True), reads=[swk, 'idb'], writes=[ptk])
                    a.op('act', lambda e, pt_=pt_, sT=sT, ex=ex: e.copy(out=sT[:, ex, :, :], in_=pt_[:, 0:CAP].rearrange("p (c t) -> p c t", c=NCH)),
                         reads=[ptk], writes=[sTk])
                st_, stk = stg.next()
                for dt in range(4):
                    p, pk = psm.next()
                    n = 0
                    for ex in range(NE):
                        for cc in range(NCH):
                            a.op('pe', lambda e, p=p, sT=sT, ex=ex, cc=cc, dt=dt, n=n: e.matmul(
                                p[:, :], lhsT=sT[:, ex, cc, :], rhs=Y2v[:, ex * NCH + cc, dt * 512:(dt + 1) * 512],
                                start=(n == 0), stop=(n == NE * NCH - 1)), reads=[sTk, 'aT'], writes=[pk])
                            n += 1
                    if dt % 2 == 0:
                        a.op('act', lambda e, p=p, st_=st_, dt=dt: e.copy(out=st_[:, dt * 512:(dt + 1) * 512], in_=p[:, :]), reads=[pk], writes=[stk])
                    else:
                        a.op('dve', lambda e, p=p, st_=st_, dt=dt: e.tensor_copy(out=st_[:, dt * 512:(dt + 1) * 512], in_=p[:, :]), reads=[pk], writes=[stk])
                a.dma('act', yo[tj * 128:(tj + 1) * 128, :], st_[:], reads=[stk], writes=[('yo', tj)])
    return c.done()


def build_BC(NT):
    prog = Prog()
    oT = prog.nc.dram_tensor("oT_s", [3, 1024, NT], BF16).ap()
    build_B(ctx=Ctx(prog, bind={'oaT': oT[0], 'obT': oT[1], 'ocT': oT[2]}))
    build_C(NT, 1, ctx=Ctx(prog, bind={'oT': oT}))
    prog.semst.close()
    return prog.nc


def build_FA(NT):
    prog = Prog()
    cF = Ctx(prog)
    xo = prog.nc.dram_tensor("xo", [NT, 2048], F32, kind="ExternalOutput").ap()
    cF.bind = {'xo': xo}
    build_F(NT, 1, ctx=cF)
    build_A(NT, 1, ctx=Ctx(prog, bind={'x': xo}))
    prog.semst.close()
    return prog.nc


def _rope_table(pos_r, pos_c, dim):
    half = dim // 2
    fr = (10000.0 ** (-np.arange(0, half, 2, dtype=np.float32) / np.float32(half))).astype(np.float32)
    ar = pos_r[:, None].astype(np.float32) * fr
    ac = pos_c[:, None].astype(np.float32) * fr
    C = np.concatenate([np.cos(ar), np.cos(ar), np.cos(ac), np.cos(ac)], 1)
    S = np.concatenate([-np.sin(ar), np.sin(ar), -np.sin(ac), np.sin(ac)], 1)
    return np.ascontiguousarray(np.concatenate([C, S], 1).astype(np.float32))


_PROGS = {}


def _prog(name, fn):
    if name not in _PROGS:
        _PROGS[name] = fn()
    return _PROGS[name]


def kernel(x, c, ctx, c_ctx, w_mod, b_mod, g_mix, w_in, g_q_a, w_uq, g_kv_a, w_ukv, g_qn, g_kn,
           rpb, w_branch, w_out, g_ffn, w_router, b_router, w_exp1, b_exp1, w_exp2, b_exp2, g_final):
    f32 = np.float32
    x = np.asarray(x, f32)
    ctx = np.asarray(ctx, f32)
    B, S, D = x.shape
    NT = 2176
    ident = np.eye(128, dtype=f32)
    cores = [(b, h) for b in range(4) for h in range(2)]

    cT = np.zeros((2048, 8), f32)
    cT[:, 0:4] = np.asarray(c, f32).T
    cT[:, 4] = np.asarray(c_ctx, f32)
    w_all = np.concatenate([np.asarray(w_mod[0]), np.asarray(w_mod[1])], axis=1)
    b_all = np.concatenate([np.asarray(b_mod[0]), np.asarray(b_mod[1])], axis=0)[None, :]
    ncm = _prog('M', lambda: build_mod(3072))
    rm = run_spmd(ncm, [dict(cT=cT, w=np.ascontiguousarray(w_all[:, 3072 * k:3072 * (k + 1)]),
                             b=np.ascontiguousarray(b_all[:, 3072 * k:3072 * (k + 1)])) for k in range(8)])
    mod_all = np.concatenate([rm[k]["o"] for k in range(8)], axis=1)
    del w_all

    def grp(l, j, b):
        m = mod_all[:, l * 12288 + j * 2048: l * 12288 + (j + 1) * 2048]
        return np.ascontiguousarray(np.stack([m[4], m[b]], 0))

    xs = [np.ascontiguousarray(np.concatenate([ctx[b, 128 * h:128 * h + 128], x[b, 2048 * h:2048 * h + 2048]], 0)) for b, h in cores]
    ropeb, ropea = [], []
    for b, h in cores:
        t = np.arange(2048 * h, 2048 * h + 2048)
        pr = np.concatenate([np.zeros(128), t // 64]).astype(f32)
        pc = np.concatenate([np.zeros(128), t % 64]).astype(f32)
        ropeb.append(_rope_table(pr, pc, 128))
        ropea.append(_rope_table(pr, pc, 64))

    ncA = _prog('A', lambda: build_A(NT, 1))
    ncBC = _prog('BC', lambda: build_BC(NT))
    ncD = _prog('D', lambda: build_D2(8 * NT))
    ncFA = _prog('FA', lambda: build_FA(NT))
    ncF = _prog('F', lambda: build_F(NT, 1))
    row = lambda v: np.ascontiguousarray(np.asarray(v, f32)[None, :])
    utri = np.triu(np.ones((128, 128), f32), 1)
    iota = np.ascontiguousarray(np.tile(np.arange(CAP, dtype=f32), (128, 1)))
    parts = None
    for l in range(2):
        w_in_l = np.ascontiguousarray(np.asarray(w_in[l], f32))
        w_uq_l = np.ascontiguousarray(np.asarray(w_uq[l], f32))
        w_ukv_l = np.ascontiguousarray(np.asarray(w_ukv[l], f32))
        inA = [dict(g=row(g_mix[l]), sc=grp(l, 1, b), sh=grp(l, 0, b), w_in=w_in_l, g_q=row(g_q_a[l]),
                    g_kv=row(g_kv_a[l]), w_uq=w_uq_l, w_ukv=w_ukv_l, g_qn=row(g_qn[l]), g_kn=row(g_kn[l]),
                    ident=ident, ropeb=ropeb[k], ropea=ropea[k]) for k, (b, h) in enumerate(cores)]
        if l == 0:
            for k in range(8):
                inA[k]['x'] = xs[k]
            ra = run_spmd(ncA, inA)
        else:
            for k, (b, h) in enumerate(cores):
                inA[k].update(x=xs[k], parts=parts[k], gt2=grp(l - 1, 5, b), gfin=row(g_final))
            ra = run_spmd(ncFA, inA)
            xs = [ra[k]['xo'] for k in range(8)]
            parts = None
        del w_in_l, inA
        inB = []
        rpb_l = np.asarray(rpb[l], f32)
        w_b_l = np.ascontiguousarray(np.asarray(w_branch[l], f32))
        w_o_l = np.ascontiguousarray(np.asarray(w_out[l], f32))
        for k, (b, h) in enumerate(cores):
            k0, k1 = 2 * b, 2 * b + 1

            def full(name):
                return np.concatenate([ra[k0][name][:128], ra[k1][name][:128], ra[k0][name][128:], ra[k1][name][128:]], 0)
            kva = full('kva').reshape(NK, 8, 256)
            lrows = np.arange(40) + 32 * h - 4
            ltok = np.concatenate([np.arange(256)] + [256 + r * 64 + np.arange(64) if 0 <= r < 64 else np.full(64, -1) for r in lrows])
            msk = ltok >= 0

            def takek(v):
                o = np.zeros((len(ltok),) + v.shape[1:], v.dtype)
                o[msk] = v[ltok[msk]]
                return o
            kc_f, vc_f = full('kc'), full('vc')
            inB.append(dict(
                qaT=np.ascontiguousarray(ra[k]['qa'].reshape(NQ, 8, 192).transpose(1, 2, 0)),
                kaT=np.ascontiguousarray(kva[:, :, :128].transpose(1, 2, 0)),
                kpeT=np.ascontiguousarray(full('kpe').T),
                va=np.ascontiguousarray(kva[:, :, 128:].reshape(NK, 1024)),
                qbT=np.ascontiguousarray(ra[k]['qb'].reshape(NQ, 8, 128).transpose(1, 2, 0)),
                kbT=np.ascontiguousarray(full('kb').reshape(NK, 2, 128).transpose(1, 2, 0)),
                vb=np.ascontiguousarray(full('vb')),
                qcT=np.ascontiguousarray(ra[k]['qc'].reshape(NQ, 8, 128).transpose(1, 2, 0)),
                kcT=np.ascontiguousarray(takek(kc_f).reshape(NKL, 8, 128).transpose(1, 2, 0)),
                vc=np.ascontiguousarray(takek(vc_f)),
                nab=na_bias_table(rpb_l, h),
                gate=ra[k]['gate'], x=xs[k], w_branch=w_b_l, w_out=w_o_l, gt1=grp(l, 2, b), g=row(g_ffn[l]), sc=grp(l, 4, b),
                sh=grp(l, 3, b), w_r=np.ascontiguousarray(np.asarray(w_router[l], f32)), b_r=row(b_router[l]), ident=ident))
        rc = run_spmd(ncBC, inB)
        del inB, ra
        xs = [rc[k]['xo'] for k in range(8)]
        n2m = np.ascontiguousarray(np.stack([rc[k]['n2'] for k in range(8)], 0).reshape(8, 128, 17, 2048).transpose(2, 1, 0, 3).reshape(8 * NT, 2048))
        rw_all = np.stack([rc[k]['rw'] for k in range(8)], 0).reshape(8, 128, 17, 32).transpose(2, 1, 0, 3).reshape(8 * NT, 32)
        del rc
        rd = run_spmd(ncD, [dict(n2=n2m, rw=np.ascontiguousarray(rw_all[:, 4 * k:4 * k + 4]),
                                 w1=np.ascontiguousarray(np.asarray(w_exp1[l][4 * k:4 * k + 4], f32)),
                                 b1=np.ascontiguousarray(np.asarray(b_exp1[l][4 * k:4 * k + 4], f32)),
                                 w2=np.ascontiguousarray(np.asarray(w_exp2[l][4 * k:4 * k + 4], f32)),
                                 b2=np.ascontiguousarray(np.asarray(b_exp2[l][4 * k:4 * k + 4], f32)), ident=ident,
                                 utri=utri, iota=iota) for k in range(8)])
        del n2m
        parts = [np.ascontiguousarray(np.stack([rd[cc]['y'].reshape(17, 128, 8, 2048)[:, :, k, :].transpose(1, 0, 2).reshape(NT, 2048) for cc in range(8)], 0)) for k in range(8)]
        del rd
    rf = run_spmd(ncF, [dict(x=xs[k], parts=parts[k], gt2=grp(1, 5, b), gfin=row(g_final)) for k, (b, h) in enumerate(cores)])
    fo = [rf[k]['fo'] for k in range(8)]
    out = np.zeros((B, S, D), f32)
    for k, (b, h) in enumerate(cores):
        out[b, 2048 * h:2048 * h + 2048] = fo[k][128:]
    return out
```
